# Optimizing a Trainium2 kernel written in Bass

```python
import math
import jax
import jax.numpy as jnp
from jax import lax
import numpy as np

D_MODEL = 4096
BATCH = 2
SEQ = 4096
DEPTH = 2

GRID_W = 64
CTX_LEN = 256
NORM_EPS = 1e-6
N_MOD = 6
NEG_INF = -1e30

HEAD_DIM = 128
ATTN_HEADS = D_MODEL // 2 // HEAD_DIM
ATTN_KV_HEADS = ATTN_HEADS // 4
GQ = ATTN_HEADS // ATTN_KV_HEADS
WINDOW = 128
ATTN_BLOCK = 128
ROPE_THETA = 10000.0
ATTN_WIDTH = ATTN_HEADS * HEAD_DIM
KV_WIDTH = ATTN_KV_HEADS * HEAD_DIM

SGU_GROUPS = 16
SGU_WIDTH = D_MODEL // 2
SGU_GROUP_DIM = SGU_WIDTH // SGU_GROUPS
SGU_CHUNK = 128

EVEN_IN_SPLITS = (ATTN_WIDTH, ATTN_WIDTH + KV_WIDTH, ATTN_WIDTH + 2 * KV_WIDTH, ATTN_WIDTH + 2 * KV_WIDTH + SGU_WIDTH)
EVEN_IN_WIDTH = ATTN_WIDTH + 2 * KV_WIDTH + 2 * SGU_WIDTH
EVEN_OUT_WIDTH = ATTN_WIDTH + SGU_WIDTH

HYENA_WIDTH = D_MODEL
HYENA_SHORT = 3
HYENA_BANDS = 16
HYENA_EMB = 1 + 2 * HYENA_BANDS
HYENA_FILTER_HIDDEN = 64
HYENA_TARGET = 1e-2
HYENA_FAST_DECAY_PCT = 0.3
HYENA_SLOW_DECAY_PCT = 1.5

N_EXPERTS = 16
EXPERT_FF = D_MODEL // 4
EC_CAPACITY_FACTOR = 2

N_EVEN = (DEPTH + 1) // 2
N_ODD = DEPTH // 2

kernel_name = 'hybrid_window_gmlp_hyena_ec_moe_block'


def rms_norm(x, g):
    xf = x.astype(jnp.float32)
    y = xf * lax.rsqrt(jnp.mean(xf * xf, axis=-1, keepdims=True) + NORM_EPS)
    return (y * g.astype(jnp.float32)).astype(x.dtype)


def modulate(x, shift, scale):
    return x * (1.0 + scale) + shift


def grid_positions(L):
    rows = L // GRID_W
    row = jnp.repeat(jnp.arange(rows, dtype=jnp.int32), GRID_W)
    col = jnp.tile(jnp.arange(GRID_W, dtype=jnp.int32), rows)
    return row, col


def rope_axis(x, pos):
    d = x.shape[-1]
    inv = ROPE_THETA ** (-jnp.arange(0, d, 2, dtype=jnp.float32) / d)
    ang = pos.astype(jnp.float32)[:, None] * inv[None, :]
    cos = jnp.cos(ang)[None, :, None, :]
    sin = jnp.sin(ang)[None, :, None, :]
    xf = x.astype(jnp.float32)
    x1, x2 = xf[..., : d // 2], xf[..., d // 2:]
    return jnp.concatenate([x1 * cos - x2 * sin, x2 * cos + x1 * sin], axis=-1).astype(x.dtype)


def rope_2d(x, row, col):
    half = x.shape[-1] // 2
    return jnp.concatenate([rope_axis(x[..., :half], row), rope_axis(x[..., half:], col)], axis=-1)


def even_project(a, w_in):
    B, L = a.shape[0], a.shape[1]
    q, k, v, u, z = jnp.split(a @ w_in, EVEN_IN_SPLITS, axis=-1)
    q = q.reshape(B, L, ATTN_HEADS, HEAD_DIM)
    k = k.reshape(B, L, ATTN_KV_HEADS, HEAD_DIM)
    v = v.reshape(B, L, ATTN_KV_HEADS, HEAD_DIM)
    return q, k, v, u, z


def project_kv(a, w_in):
    B, L = a.shape[0], a.shape[1]
    kv = a @ w_in[:, ATTN_WIDTH:ATTN_WIDTH + 2 * KV_WIDTH]
    k, v = jnp.split(kv, 2, axis=-1)
    return k.reshape(B, L, ATTN_KV_HEADS, HEAD_DIM), v.reshape(B, L, ATTN_KV_HEADS, HEAD_DIM)


def _band(t, nb):
    B = t.shape[0]
    tb = t.reshape(B, nb, ATTN_BLOCK, ATTN_KV_HEADS, HEAD_DIM)
    tp = jnp.pad(tb, ((0, 0), (1, 1), (0, 0), (0, 0), (0, 0)))
    return jnp.concatenate([tp[:, :-2], tp[:, 1:-1], tp[:, 2:]], axis=2)


def windowed_gqa_with_context(q, k, v, kc, vc, sink):
    B, L = q.shape[0], q.shape[1]
    Lc = kc.shape[1]
    nb = L // ATTN_BLOCK
    scale = HEAD_DIM ** -0.5
    qb = q.reshape(B, nb, ATTN_BLOCK, ATTN_KV_HEADS, GQ, HEAD_DIM)
    kb, vb = _band(k, nb), _band(v, nb)
    s_loc = jnp.einsum('bnqhgd,bnkhd->bnhgqk', qb, kb).astype(jnp.float32) * scale
    q_off = jnp.arange(ATTN_BLOCK)
    k_off = jnp.arange(3 * ATTN_BLOCK) - ATTN_BLOCK
    k_abs = jnp.arange(nb)[:, None] * ATTN_BLOCK + k_off[None, :]
    valid = (jnp.abs(k_off[None, :] - q_off[:, None]) <= WINDOW)[None] & ((k_abs >= 0) & (k_abs < L))[:, None, :]
    s_loc = jnp.where(valid[None, :, None, None], s_loc, NEG_INF)
    s_ctx = jnp.einsum('bnqhgd,bchd->bnhgqc', qb, kc).astype(jnp.float32) * scale
    sink_col = jnp.broadcast_to(sink.astype(jnp.float32).reshape(ATTN_KV_HEADS, GQ)[None, None, :, :, None, None], s_loc.shape[:-1] + (1,))
    p = jax.nn.softmax(jnp.concatenate([s_loc, s_ctx, sink_col], axis=-1), axis=-1)
    n_loc = 3 * ATTN_BLOCK
    p_loc = p[..., :n_loc].astype(v.dtype)
    p_ctx = p[..., n_loc:n_loc + Lc].astype(v.dtype)
    o = jnp.einsum('bnhgqk,bnkhd->bnqhgd', p_loc, vb) + jnp.einsum('bnhgqc,bchd->bnqhgd', p_ctx, vc)
    return o.reshape(B, L, ATTN_WIDTH)


def context_gqa(qc, kc, vc, sink):
    B, Lc = qc.shape[0], qc.shape[1]
    qg = qc.reshape(B, Lc, ATTN_KV_HEADS, GQ, HEAD_DIM)
    s = jnp.einsum('bqhgd,bkhd->bhgqk', qg, kc).astype(jnp.float32) * HEAD_DIM ** -0.5
    sink_col = jnp.broadcast_to(sink.astype(jnp.float32).reshape(ATTN_KV_HEADS, GQ)[None, :, :, None, None], s.shape[:-1] + (1,))
    p = jax.nn.softmax(jnp.concatenate([s, sink_col], axis=-1), axis=-1)[..., :Lc]
    o = jnp.einsum('bhgqk,bkhd->bqhgd', p.astype(vc.dtype), vc)
    return o.reshape(B, Lc, ATTN_WIDTH)


def spatial_gating(u, z, g, w_s, b_s):
    B, L = u.shape[0], u.shape[1]
    u = jax.nn.gelu(u, approximate=False)
    z = jax.nn.gelu(z, approximate=False)
    zf = z.astype(jnp.float32).reshape(B, L, SGU_GROUPS, SGU_GROUP_DIM)
    mu = jnp.mean(zf, axis=-1, keepdims=True)
    var = jnp.mean(jnp.square(zf - mu), axis=-1, keepdims=True)
    zn = ((zf - mu) * lax.rsqrt(var + NORM_EPS) * g.astype(jnp.float32).reshape(SGU_GROUPS, SGU_GROUP_DIM)).astype(z.dtype)
    zc = zn.reshape(B, L // SGU_CHUNK, SGU_CHUNK, SGU_GROUPS, SGU_GROUP_DIM)
    mixed = jnp.einsum('gpq,bnqgc->bnpgc', w_s, zc) + jnp.transpose(b_s)[None, None, :, :, None]
    return u * mixed.reshape(B, L, SGU_WIDTH)


def attn_sgu_mixer(a_lat, a_ctx, row, col, w_in, sink, sgu_g, w_s, b_s, w_out, need_ctx_out):
    q, k, v, u, z = even_project(a_lat, w_in)
    q, k = rope_2d(q, row, col), rope_2d(k, row, col)
    if need_ctx_out:
        qc, kc, vc, uc, zc = even_project(a_ctx, w_in)
    else:
        kc, vc = project_kv(a_ctx, w_in)
    o = windowed_gqa_with_context(q, k, v, kc, vc, sink)
    y_lat = jnp.concatenate([o, spatial_gating(u, z, sgu_g, w_s, b_s)], axis=-1) @ w_out
    if not need_ctx_out:
        return y_lat, None
    oc = context_gqa(qc, kc, vc, sink)
    y_ctx = jnp.concatenate([oc, spatial_gating(uc, zc, sgu_g, w_s, b_s)], axis=-1) @ w_out
    return y_lat, y_ctx


def short_conv_centred(u, w, b):
    up = jnp.pad(u, ((0, 0), (1, 1), (0, 0)))
    return up[:, :-2] * w[0] + up[:, 1:-1] * w[1] + up[:, 2:] * w[2] + b


def hyena_filter(L, w1, b1, w2, b2, freq, w3):
    pos = jnp.arange(L, dtype=jnp.float32)
    t01 = pos / max(L - 1, 1)
    bands = jnp.linspace(1e-4, HYENA_BANDS - 1, HYENA_BANDS, dtype=jnp.float32)
    ang = (2.0 * math.pi / L) * pos[:, None] * bands[None, :]
    feats = jnp.concatenate([t01[:, None], jnp.cos(ang), -jnp.sin(ang)], axis=-1)
    hid = jnp.sin(freq[0] * (feats @ w1 + b1))
    hid = jnp.sin(freq[1] * (hid @ w2 + b2))
    h = (hid @ w3).astype(jnp.float32).reshape(L, 2, HYENA_WIDTH)
    deltas = jnp.abs(jnp.linspace(math.log(HYENA_TARGET) / HYENA_SLOW_DECAY_PCT, math.log(HYENA_TARGET) / HYENA_FAST_DECAY_PCT, HYENA_WIDTH, dtype=jnp.float32))
    h = h * jnp.exp(-t01[:, None] * deltas[None, :])[:, None, :]
    k = jnp.concatenate([h[:, 0], jnp.zeros((1, HYENA_WIDTH), jnp.float32), h[1:, 1][::-1]], axis=0)
    return k / jnp.sum(jnp.abs(k), axis=0, keepdims=True)


def hyena_mix(a, w_in, conv_w, conv_b, w1, b1, w2, b2, freq, w3, bias, w_out):
    L = a.shape[1]
    u = short_conv_centred(a @ w_in, conv_w, conv_b)
    x0, x1, v = jnp.split(u, 3, axis=-1)
    k = hyena_filter(L, w1, b1, w2, b2, freq, w3)
    z = (v * x1).astype(jnp.float32)
    zf = jnp.fft.rfft(z, n=2 * L, axis=1)
    kf = jnp.fft.rfft(k, n=2 * L, axis=0)
    y = jnp.fft.irfft(zf * kf[None], n=2 * L, axis=1)[:, :L] + z * bias.astype(jnp.float32)
    return (x0 * y.astype(a.dtype)) @ w_out


def expert_choice_moe(h, w_router, w_gate, w_up, w_down):
    B, L = h.shape[0], h.shape[1]
    cap = EC_CAPACITY_FACTOR * L // N_EXPERTS
    aff = jax.nn.softmax((h @ w_router).astype(jnp.float32), axis=-1)
    gates, idx = lax.top_k(jnp.swapaxes(aff, 1, 2), cap)
    bidx = jnp.arange(B)[:, None, None]
    xs = h[bidx, idx]
    act = jax.nn.silu(jnp.einsum('becd,edf->becf', xs, w_gate)) * jnp.einsum('becd,edf->becf', xs, w_up)
    ys = jnp.einsum('becf,efd->becd', act, w_down) * gates[..., None].astype(h.dtype)
    return jnp.zeros_like(h).at[bidx, idx].add(ys)


def setup_inputs(seed: int = 0) -> dict:
    key = jax.random.key(seed)
    ks = jax.random.split(key, 30)
    D = D_MODEL

    def nrm(k, shape, scale):
        return jax.random.normal(k, shape, jnp.float32) * scale

    return {
        'x': nrm(ks[0], (BATCH, SEQ, D), 1.0),
        'c': nrm(ks[1], (BATCH, D), 1.0),
        'ctx': nrm(ks[2], (BATCH, CTX_LEN, D), 1.0),
        'c_ctx': nrm(ks[3], (D,), 1.0),
        'ada_w': nrm(ks[4], (DEPTH, D, N_MOD * D), 0.5 * D ** -0.5),
        'ada_b': nrm(ks[5], (DEPTH, N_MOD * D), 0.01),
        'norm_mix_g': 1.0 + nrm(ks[6], (DEPTH, D), 0.1),
        'norm_ffn_g': 1.0 + nrm(ks[7], (DEPTH, D), 0.1),
        'attn_sgu_w_in': nrm(ks[8], (N_EVEN, D, EVEN_IN_WIDTH), D ** -0.5),
        'attn_sink': nrm(ks[9], (N_EVEN, ATTN_HEADS), 1.0),
        'sgu_norm_g': 1.0 + nrm(ks[10], (N_EVEN, SGU_WIDTH), 0.1),
        'sgu_w_s': nrm(ks[11], (N_EVEN, SGU_GROUPS, SGU_CHUNK, SGU_CHUNK), SGU_CHUNK ** -0.5),
        'sgu_b_s': 1.0 + nrm(ks[12], (N_EVEN, SGU_GROUPS, SGU_CHUNK), 0.1),
        'attn_sgu_w_out': nrm(ks[13], (N_EVEN, EVEN_OUT_WIDTH, D), EVEN_OUT_WIDTH ** -0.5),
        'hyena_w_in': nrm(ks[14], (N_ODD, D, 3 * HYENA_WIDTH), D ** -0.5),
        'hyena_conv_w': nrm(ks[15], (N_ODD, HYENA_SHORT, 3 * HYENA_WIDTH), HYENA_SHORT ** -0.5),
        'hyena_conv_b': nrm(ks[16], (N_ODD, 3 * HYENA_WIDTH), 0.01),
        'hyena_filt_w1': nrm(ks[17], (N_ODD, HYENA_EMB, HYENA_FILTER_HIDDEN), HYENA_EMB ** -0.5),
        'hyena_filt_b1': nrm(ks[18], (N_ODD, HYENA_FILTER_HIDDEN), 0.1),
        'hyena_filt_w2': nrm(ks[19], (N_ODD, HYENA_FILTER_HIDDEN, HYENA_FILTER_HIDDEN), HYENA_FILTER_HIDDEN ** -0.5),
        'hyena_filt_b2': nrm(ks[20], (N_ODD, HYENA_FILTER_HIDDEN), 0.1),
        'hyena_filt_freq': 1.0 + nrm(ks[21], (N_ODD, 2, HYENA_FILTER_HIDDEN), 0.1),
        'hyena_filt_w3': nrm(ks[22], (N_ODD, HYENA_FILTER_HIDDEN, 2 * HYENA_WIDTH), HYENA_FILTER_HIDDEN ** -0.5),
        'hyena_bias': nrm(ks[23], (N_ODD, HYENA_WIDTH), 0.1),
        'hyena_w_out': nrm(ks[24], (N_ODD, HYENA_WIDTH, D), HYENA_WIDTH ** -0.5),
        'router_w': nrm(ks[25], (DEPTH, D, N_EXPERTS), D ** -0.5),
        'expert_w_gate': nrm(ks[26], (DEPTH, N_EXPERTS, D, EXPERT_FF), D ** -0.5),
        'expert_w_up': nrm(ks[27], (DEPTH, N_EXPERTS, D, EXPERT_FF), D ** -0.5),
        'expert_w_down': nrm(ks[28], (DEPTH, N_EXPERTS, EXPERT_FF, D), EXPERT_FF ** -0.5),
        'final_norm_g': 1.0 + nrm(ks[29], (D,), 0.1),
    }


def reference(x, c, ctx, c_ctx, ada_w, ada_b, norm_mix_g, norm_ffn_g, attn_sgu_w_in, attn_sink, sgu_norm_g, sgu_w_s, sgu_b_s, attn_sgu_w_out, hyena_w_in, hyena_conv_w, hyena_conv_b, hyena_filt_w1, hyena_filt_b1, hyena_filt_w2, hyena_filt_b2, hyena_filt_freq, hyena_filt_w3, hyena_bias, hyena_w_out, router_w, expert_w_gate, expert_w_up, expert_w_down, final_norm_g):
    L = x.shape[1]
    row, col = grid_positions(L)
    s_c = jax.nn.silu(c)
    s_cc = jax.nn.silu(c_ctx)
    h_lat, h_ctx = x, ctx
    for layer in range(DEPTH):
        even = layer % 2 == 0
        i = layer // 2
        update_ctx = any(l % 2 == 0 for l in range(layer + 1, DEPTH))
        mod_lat = jnp.split((s_c @ ada_w[layer] + ada_b[layer])[:, None, :], N_MOD, axis=-1)
        a_lat = modulate(rms_norm(h_lat, norm_mix_g[layer]), mod_lat[0], mod_lat[1])
        if even or update_ctx:
            mod_ctx = jnp.split(s_cc @ ada_w[layer] + ada_b[layer], N_MOD, axis=-1)
            a_ctx = modulate(rms_norm(h_ctx, norm_mix_g[layer]), mod_ctx[0], mod_ctx[1])
        if even:
            y_lat, y_ctx = attn_sgu_mixer(a_lat, a_ctx, row, col, attn_sgu_w_in[i], attn_sink[i], sgu_norm_g[i], sgu_w_s[i], sgu_b_s[i], attn_sgu_w_out[i], update_ctx)
        else:
            hp = (hyena_w_in[i], hyena_conv_w[i], hyena_conv_b[i], hyena_filt_w1[i], hyena_filt_b1[i], hyena_filt_w2[i], hyena_filt_b2[i], hyena_filt_freq[i], hyena_filt_w3[i], hyena_bias[i], hyena_w_out[i])
            y_lat = hyena_mix(a_lat, *hp)
            y_ctx = hyena_mix(a_ctx, *hp) if update_ctx else None
        h_lat = h_lat + mod_lat[2] * y_lat
        b_lat = modulate(rms_norm(h_lat, norm_ffn_g[layer]), mod_lat[3], mod_lat[4])
        h_lat = h_lat + mod_lat[5] * expert_choice_moe(b_lat, router_w[layer], expert_w_gate[layer], expert_w_up[layer], expert_w_down[layer])
        if update_ctx:
            h_ctx = h_ctx + mod_ctx[2] * y_ctx
            b_ctx = modulate(rms_norm(h_ctx, norm_ffn_g[layer]), mod_ctx[3], mod_ctx[4])
            h_ctx = h_ctx + mod_ctx[5] * expert_choice_moe(b_ctx, router_w[layer], expert_w_gate[layer], expert_w_up[layer], expert_w_down[layer])
    return rms_norm(h_lat, final_norm_g)
```

```python
import math
import numpy as np
from contextlib import ExitStack
import ml_dtypes
import concourse.bass as bass
import concourse.mybir as mybir
from concourse.bass_utils import run_bass_kernel_spmd

F32 = mybir.dt.float32; BF16 = mybir.dt.bfloat16; I32 = mybir.dt.int32; U32 = mybir.dt.uint32
ALU = mybir.AluOpType; AF = mybir.ActivationFunctionType; AX = mybir.AxisListType
NPBF = ml_dtypes.bfloat16
NCORES = 8
D = 4096; L = 4096; B = 2


class Buf:
    def __init__(self, t, name):
        self.t = t; self.name = name; self.w = {}; self.r = {}; self.dsem = None; self.dcnt = 0

    def __getitem__(self, k):
        return self.t[k]

    def ap(self):
        return self.t.ap()


class Eng:
    def __init__(self, h, sem, name):
        self.h = h; self.sem = sem; self.cnt = 0; self.waited = {}; self.name = name


class KB:
    def __init__(self):
        self.nc = bass.Bass("TRN2", target_bir_lowering=False)
        self.es = ExitStack()
        nc = self.nc
        self.E = {}
        for nm, h in (("pe", nc.tensor), ("act", nc.scalar), ("dve", nc.vector), ("pool", nc.gpsimd), ("sp", nc.sync)):
            self.E[nm] = Eng(h, self.es.enter_context(nc.semaphore("s_" + nm)), nm)
        self.bufs = []
        self.same_engine_sync = True

    def _reg(self, b):
        self.bufs.append(b); return b

    def dram(self, name, shape, dtype, kind="Internal"):
        return self._reg(Buf(self.nc.dram_tensor(name, list(shape), dtype, kind=kind), name))

    def sbuf(self, name, shape, dtype):
        return self._reg(Buf(self.es.enter_context(self.nc.sbuf_tensor(name, list(shape), dtype)), name))

    def psum(self, name, shape, dtype=F32):
        b = self._reg(Buf(self.es.enter_context(self.nc.psum_tensor(name, list(shape), dtype)), name))
        b.psum = True
        return b

    def _waits(self, e, reads, writes):
        deps = {}
        for b in reads:
            for s, v in b.w.items(): deps[s] = max(deps.get(s, 0), v)
        for b in writes:
            for s, v in b.w.items(): deps[s] = max(deps.get(s, 0), v)
            for s, v in b.r.items(): deps[s] = max(deps.get(s, 0), v)
        for s, v in deps.items():
            if s is e.sem and (e.name == "pe" or not self.same_engine_sync):
                continue
            if e.waited.get(s, 0) < v:
                for o in self.E.values():
                    if o.sem is s:
                        assert v <= o.cnt, f"wait on not-yet-emitted signal: {e.name} waits {o.name}>={v} (cnt {o.cnt})"
                e.h.wait_ge(s, v); e.waited[s] = v

    def op(self, eng, fn, reads=(), writes=(), sig=True):
        e = self.E[eng]
        writes = list(writes) + [b for b in reads if getattr(b, "psum", False)]
        reads = [b for b in reads if not getattr(b, "psum", False)]
        self._waits(e, reads, writes)
        ins = fn(e.h)
        if sig:
            e.cnt += 1
            ins.then_inc(e.sem, 1)
        tok = e.cnt if sig else e.cnt + 1
        for b in writes: b.w[e.sem] = max(b.w.get(e.sem, 0), tok)
        for b in reads: b.r[e.sem] = max(b.r.get(e.sem, 0), tok)
        return ins

    def dma(self, eng, fn, reads=(), writes=(), sembuf=None):
        e = self.E[eng]
        self._waits(e, reads, writes)
        sb = sembuf if sembuf is not None else (writes[0] if writes else reads[0])
        if sb.dsem is None:
            sb.dsem = self.es.enter_context(self.nc.semaphore("d_" + sb.name))
        ins = fn(e.h)
        sb.dcnt += 16
        ins.then_inc(sb.dsem, 16)
        for b in writes: b.w[sb.dsem] = sb.dcnt
        for b in reads: b.r[sb.dsem] = sb.dcnt
        return ins

    def load(self, dst, dst_ap, src, src_ap, eng="sp"):
        return self.dma(eng, lambda h: h.dma_start(out=dst_ap, in_=src_ap), reads=[src], writes=[dst], sembuf=dst)

    def store(self, dst, dst_ap, src, src_ap, eng="sp"):
        return self.dma(eng, lambda h: h.dma_start(out=dst_ap, in_=src_ap), reads=[src], writes=[dst], sembuf=src)

    def mm(self, out, out_ap, lhs, lhs_ap, rhs, rhs_ap, start, stop, extra_reads=(), sig=None):
        return self.op("pe", lambda h: h.matmul(out_ap, lhs_ap, rhs_ap, start=start, stop=stop),
                       reads=[lhs, rhs] + list(extra_reads), writes=[out], sig=(stop if sig is None else sig))

    def finish(self):
        e = self.E["sp"]
        for b in self.bufs:
            if b.dsem is not None and e.waited.get(b.dsem, 0) < b.dcnt:
                e.h.wait_ge(b.dsem, b.dcnt); e.waited[b.dsem] = b.dcnt
        for nm, o in self.E.items():
            if o.cnt > 0 and e.waited.get(o.sem, 0) < o.cnt:
                e.h.wait_ge(o.sem, o.cnt)
        self.es.close()
        return self.nc


def run(nc, in_maps):
    res = run_bass_kernel_spmd(nc, in_maps, core_ids=list(range(NCORES)))
    return res.results


def build_A():
    kb = KB()
    w = kb.dram("ada_w", [2, 4096, 3072], F32, "ExternalInput")
    bia = kb.dram("ada_b", [2, 3072], F32, "ExternalInput")
    cv = kb.dram("cv", [128, 32, 3], F32, "ExternalInput")
    out = kb.dram("mods", [2, 3, 3072], F32, "ExternalOutput")
    s = kb.sbuf("s", [128, 32, 3], F32)
    bt = kb.sbuf("bt", [3, 2, 3072], F32)
    res = kb.sbuf("res", [3, 2, 3072], F32)
    wt = [kb.sbuf(f"wt{i}", [128, 32, 512], F32) for i in range(2)]
    ps = [kb.psum(f"ps{i}", [128, 512], F32) for i in range(2)]
    kb.load(s, s[:], cv, cv.ap())
    kb.load(bt, bt[:], bia, bia.ap().partition_broadcast(3))
    kb.op("act", lambda h: h.activation(s[:], s[:], AF.Silu), reads=[s], writes=[s])
    it = 0
    for l in range(2):
        wv = w.ap()[l].rearrange("(k p) n -> p k n", p=128)
        for cb in range(6):
            t = wt[it % 2]; p = ps[it % 2]
            kb.load(t, t[:], w, wv[:, :, cb * 512:(cb + 1) * 512], eng=("sp" if it % 2 == 0 else "act"))
            for k in range(32):
                kb.mm(p, p[0:3, :], s, s[:, k, :], t, t[:, k, :], k == 0, k == 31)
            kb.op("dve", lambda h: h.tensor_tensor(res[:, l, cb * 512:(cb + 1) * 512], p[0:3, :], bt[:, l, cb * 512:(cb + 1) * 512], ALU.add),
                  reads=[p, bt], writes=[res])
            it += 1
    kb.store(out, out.ap().rearrange("l r n -> r l n"), res, res[:])
    return kb.finish()


def launch_A(inp):
    nc = build_A()
    cvec = np.concatenate([inp["c"], inp["c_ctx"][None]], 0).astype(np.float32)
    cv = np.ascontiguousarray(cvec.T.reshape(32, 128, 3).transpose(1, 0, 2))
    maps = []
    for c in range(NCORES):
        maps.append({"ada_w": np.ascontiguousarray(inp["ada_w"][:, :, c * 3072:(c + 1) * 3072]),
                     "ada_b": np.ascontiguousarray(inp["ada_b"][:, c * 3072:(c + 1) * 3072]),
                     "cv": cv})
    r = run(nc, maps)
    mods = np.concatenate([r[c]["mods"] for c in range(NCORES)], axis=2)
    return mods.reshape(2, 3, 6, D)


class Arena:
    def __init__(self, kb, name, nbytes):
        self.kb = kb
        self.t = kb.es.enter_context(kb.nc.sbuf_tensor(name, [128, nbytes // 2], BF16))
        self.nbytes = nbytes

    def view(self, name, off, shape, dtype, prev=()):
        n = 1
        for s in shape[1:]: n *= s
        esz = 2 if dtype == BF16 else 4
        assert off % 4 == 0 and off + n * esz <= self.nbytes, (name, off, n * esz, self.nbytes)
        ap = self.t[0:shape[0], off // 2: off // 2 + n * esz // 2]
        if esz == 4:
            ap = ap.bitcast(dtype)
        if len(shape) == 3:
            ap = ap.rearrange("p (a b) -> p a b", a=shape[1])
        if len(shape) == 4:
            ap = ap.rearrange("p (a b c) -> p a b c", a=shape[1], b=shape[2])
        b = Buf(ap, name)
        for o in prev:
            for s, v in list(o.w.items()) + list(o.r.items()):
                b.w[s] = max(b.w.get(s, 0), v)
        self.kb.bufs.append(b)
        return b


def psum_banks(kb):
    banks = []
    for i in range(8):
        b = kb.psum(f"ps{i}", [128, 512], F32)
        b.bf = b.t[:].bitcast(BF16)
        banks.append(b)
    return banks


IN = "ExternalInput"; OUT = "ExternalOutput"


def rms_rstd(kb, ss_ap, ss_buf, eps_buf, n):
    kb.op("act", lambda h: h.activation(ss_ap, ss_ap, AF.Sqrt, bias=eps_buf[:, 0:1], scale=1.0 / n), reads=[ss_buf, eps_buf], writes=[ss_buf])
    kb.op("dve", lambda h: h.reciprocal(ss_ap, ss_ap), reads=[ss_buf], writes=[ss_buf])


def build_B(stop=None):
    kb = KB()
    xo = kb.dram("xo", [1024, 4096], F32, IN)
    xe = kb.dram("xe", [512, 4096], F32, IN)
    gs = kb.dram("gs", [128, 5, 32], F32, IN)
    rowv = kb.dram("rowv", [4, 4096], F32, IN)
    w_in = kb.dram("w_in", [4096, 7168], F32, IN)
    w_out = kb.dram("w_out", [4096, 4096], F32, IN)
    ropec = kb.dram("ropec", [128, 1536], F32, IN)
    ropes = kb.dram("ropes", [128, 1536], F32, IN)
    cbf = kb.dram("cbf", [128, 3, 128], BF16, IN)
    masks = kb.dram("masks", [128, 4, 512], BF16, IN)
    identf = kb.dram("identf", [128, 128], F32, IN)
    sink = kb.dram("sink", [1, 16], F32, IN)
    sgu_ws = kb.dram("sgu_ws", [16, 128, 128], F32, IN)
    sgu_bs = kb.dram("sgu_bs", [1, 2048], F32, IN)
    sgu_g = kb.dram("sgu_g", [1, 2048], F32, IN)
    wr = kb.dram("wr", [128, 32, 16], F32, IN)
    h1 = kb.dram("h1", [1024, 4096], F32, OUT)
    bl = kb.dram("bl", [1024, 4096], BF16, OUT)
    affT = kb.dram("affT", [16, 1024], F32, OUT)
    catT = kb.dram("catT", [4096, 1024], BF16, OUT)
    d = dict(locals())
    mixer0_body(kb, d)
    if d.get('_stopped'):
        return kb.finish()
    ffn_front(kb, d['_ar'], d['_ps'], d['_dead'], d['_epsb'], xo, catT, w_out, rowv, wr, identf, h1, bl, affT)
    return kb.finish()


def rope_evac(kb, ps, rot_ps, cosb, cos_ap, sinb, sin_ap, rt, tmpb, t1, t2, outb, out_ap):
    kb.op("act", lambda h: h.activation(tmpb[:], ps[:], AF.Copy), reads=[ps], writes=[tmpb])
    kb.mm(rot_ps, rot_ps[:], rt, rt[:], tmpb, tmpb[:], True, True)
    kb.op("dve", lambda h: h.tensor_tensor(t1[:], ps[:], cos_ap, ALU.mult), reads=[ps, cosb], writes=[t1])
    kb.op("dve", lambda h: h.tensor_tensor(t2[:], rot_ps[:], sin_ap, ALU.mult), reads=[rot_ps, sinb], writes=[t2])
    kb.op("dve", lambda h: h.tensor_tensor(out_ap, t1[:], t2[:], ALU.add), reads=[t1, t2], writes=[outb])


def mixer0_body(kb, d):
    xo, xe, gs, w_in, ropec, ropes, cbf, masks, sink = d["xo"], d["xe"], d["gs"], d["w_in"], d["ropec"], d["ropes"], d["cbf"], d["masks"], d["sink"]
    sgu_ws, sgu_bs, sgu_g, catT, identf = d["sgu_ws"], d["sgu_bs"], d["sgu_g"], d["catT"], d["identf"]
    ps = psum_banks(kb)
    ar = Arena(kb, "arenaB", 204 * 1024)
    KBY = 1024
    off = 184 * KBY
    gsb = ar.view("gsb", off, [128, 5, 32], F32); off += 640
    GS = ar.view("GS", off, [128, 4, 32], F32); off += 512
    cb = ar.view("cb", off, [128, 3, 128], BF16); off += 768
    epsb = ar.view("epsb", off, [128, 1], F32); off += 4
    ssb = ar.view("ssb", off, [128, 16], F32); off += 64
    mk = ar.view("mk", off, [128, 4, 512], BF16); off += 4096
    esk = ar.view("esk", off, [128, 16], F32); off += 64
    eskhl = ar.view("eskhl", off, [2, 16, 128], BF16); off += 4096
    eskf = ar.view("eskf", off, [1, 16, 128], F32); off += 8192
    assert off <= 204 * KBY
    ident = Buf(cb.t[:, 0, :], "ident"); rt = Buf(cb.t[:, 1, :], "rt"); ones = Buf(cb.t[:, 2, :], "ones")
    for b_ in (ident, rt, ones): b_.w = cb.w; b_.r = cb.r
    kb.load(gsb, gsb[:], gs, gs.ap())
    kb.load(cb, cb[:], cbf, cbf.ap())
    kb.load(mk, mk[:], masks, masks.ap())
    kb.op("dve", lambda h: h.memset(epsb[:], 1e-6), writes=[epsb])
    kb.op("dve", lambda h: h.scalar_tensor_tensor(GS[:, 0, :], gsb[:, 1, :], 1.0, gsb[:, 0, :], ALU.add, ALU.mult), reads=[gsb], writes=[GS])
    kb.op("dve", lambda h: h.tensor_copy(GS[:, 1, :], gsb[:, 2, :]), reads=[gsb], writes=[GS])
    kb.op("dve", lambda h: h.scalar_tensor_tensor(GS[:, 2, :], gsb[:, 3, :], 1.0, gsb[:, 0, :], ALU.add, ALU.mult), reads=[gsb], writes=[GS])
    kb.op("dve", lambda h: h.tensor_copy(GS[:, 3, :], gsb[:, 4, :]), reads=[gsb], writes=[GS])
    kb.load(esk, esk[0:1, :], sink, sink.ap())
    kb.op("act", lambda h: h.activation(esk[0:1, :], esk[0:1, :], AF.Exp), reads=[esk], writes=[esk])
    kb.op("dve", lambda h: h.tensor_copy(eskf[0:1, :, :], esk[0:1, :].unsqueeze(2).to_broadcast([1, 16, 128])), reads=[esk], writes=[eskf])
    kb.op("dve", lambda h: h.tensor_copy(eskhl[0:1, :, :], eskf[0:1, :, :]), reads=[eskf], writes=[eskhl])
    kb.op("dve", lambda h: h.tensor_tensor(eskf[0:1, :, :], eskf[0:1, :, :], eskhl[0:1, :, :], ALU.subtract), reads=[eskf, eskhl], writes=[eskf])
    lo_tmp = ar.view("lo_tmp", 180 * KBY, [1, 16, 128], BF16)
    kb.op("dve", lambda h: h.tensor_copy(lo_tmp[0:1, :, :], eskf[0:1, :, :]), reads=[eskf], writes=[lo_tmp])
    kb.dma("sp", lambda h: h.dma_start(out=eskhl[1:2, :, :], in_=lo_tmp[0:1, :, :]), reads=[lo_tmp], writes=[eskhl], sembuf=eskhl)

    if d.get('stop') == 'C':
        d['_stopped'] = True
        return
    aT = ar.view("aT", 0, [128, 32, 1024], BF16)
    aTx = ar.view("aTx", 64 * KBY, [128, 32, 512], BF16)
    wA = ar.view("wA", 96 * KBY, [128, 32, 512], BF16)
    xt = [ar.view("xt0", 128 * KBY, [128, 4096], F32), ar.view("xt1", 144 * KBY, [128, 4096], F32)]
    xn = ar.view("xn", 160 * KBY, [128, 4096], BF16)

    def norm_tile(i, src, row0, dst, col0, gi):
        x_ = xt[i % 2]
        kb.load(x_, x_[:], src, src.ap()[row0:row0 + 128, :])
        kb.op("act", lambda h: h.activation(xn[:], x_[:], AF.Square, accum_out=ssb[:, 0:1]), reads=[x_], writes=[xn, ssb])
        rms_rstd(kb, ssb[:, 0:1], ssb, epsb, 4096)
        kb.op("act", lambda h: h.activation(xn[:], x_[:], AF.Copy, scale=ssb[:, 0:1]), reads=[x_, ssb], writes=[xn])
        for bk in range(4):
            p = ps[bk]
            for j in range(8):
                k = bk * 8 + j
                kb.op("pe", lambda h: h.transpose(p.bf[:, j * 128:(j + 1) * 128], xn[:, k * 128:(k + 1) * 128], ident[:]),
                      reads=[xn, ident], writes=[p], sig=(j == 7))
            for j in range(8):
                k = bk * 8 + j
                o_ap = dst[:, k, col0:col0 + 128]
                i_ap = p.bf[:, j * 128:(j + 1) * 128]
                if bk % 2 == 0:
                    kb.op("act", lambda h: h.activation(o_ap, i_ap, AF.Identity, bias=GS[:, gi + 1, k:k + 1], scale=GS[:, gi, k:k + 1]),
                          reads=[p, GS], writes=[dst])
                else:
                    kb.op("dve", lambda h: h.tensor_scalar(o_ap, i_ap, GS[:, gi, k:k + 1], GS[:, gi + 1, k:k + 1], ALU.mult, ALU.add),
                          reads=[p, GS], writes=[dst])
    for i in range(4):
        norm_tile(i, xe, i * 128, aTx, i * 128, 0 if i < 2 else 2)
    for i in range(8):
        norm_tile(i, xo, i * 128, aT, i * 128, 0)

    if d.get('stop') == 'B1':
        d['_stopped'] = True
        return
    r6 = 128 * KBY
    cosb = ar.view("cosb", r6, [128, 1536], F32, prev=xt + [xn]); sinb = ar.view("sinb", r6 + 6 * KBY, [128, 1536], F32, prev=xt + [xn])
    kT = ar.view("kT", r6 + 12 * KBY, [128, 4, 1536], BF16, prev=xt + [xn])
    V = ar.view("V", r6 + 24 * KBY, [128, 12, 512], BF16, prev=xt + [xn])
    tmpb = ar.view("tmpb", 164 * KBY, [128, 512], BF16, prev=[xn])
    t1 = ar.view("t1", 165 * KBY, [128, 512], F32, prev=[xn]); t2 = ar.view("t2", 167 * KBY, [128, 512], F32, prev=[xn])
    kb.load(cosb, cosb[:], ropec, ropec.ap())
    kb.load(sinb, sinb[:], ropes, ropes.ap())
    wv_in = w_in.ap().rearrange("(k p) n -> p k n", p=128)

    def wload(t, c0, eng="pool"):
        kb.dma(eng, lambda h: h.dma_start(out=t[:], in_=wv_in[:, :, c0:c0 + 512]), reads=[w_in], writes=[t], sembuf=t)

    def tok_rhs(tt):
        return (aT, lambda k: aT[:, k, tt * 512:(tt + 1) * 512]) if tt < 2 else (aTx, lambda k: aTx[:, k, :])

    def tok_lhs(t128):
        return (aT, lambda k: aT[:, k, t128 * 128:(t128 + 1) * 128]) if t128 < 8 else (aTx, lambda k: aTx[:, k, (t128 - 8) * 128:(t128 - 7) * 128])

    wload(wA, 2048)
    it = 0
    for hk in range(4):
        for tt in range(3):
            p = ps[4 + it % 2]; it += 1
            ab, af = tok_rhs(tt)
            for k in range(32):
                kb.mm(p, p[:], wA, wA[:, k, hk * 128:(hk + 1) * 128], ab, af(k), k == 0, k == 31)
            if d.get('stop') == 'B15a':
                d['_stopped'] = True
                return
            rope_evac(kb, p, ps[6], cosb, cosb[:, tt * 512:(tt + 1) * 512], sinb, sinb[:, tt * 512:(tt + 1) * 512], rt, tmpb, t1, t2,
                      kT, kT[:, hk, tt * 512:(tt + 1) * 512])
            if d.get('stop') == 'B15b':
                d['_stopped'] = True
                return
    if d.get('stop') == 'B15c':
        d['_stopped'] = True
        return
    wload(wA, 2560)
    for t128 in range(12):
        p = ps[4 + t128 % 2]
        ab, af = tok_lhs(t128)
        for k in range(32):
            kb.mm(p, p[:], ab, af(k), wA, wA[:, k, :], k == 0, k == 31)
        kb.op("act", lambda h: h.activation(V[:, t128, :], p[:], AF.Copy), reads=[p], writes=[V])

    if d.get('stop') == 'B15':
        d['_stopped'] = True
        return
    wB = ar.view("wB", 64 * KBY, [128, 32, 512], BF16, prev=[aTx])
    wbufs = [wA, wB]
    qT = ar.view("qT", 169 * KBY, [128, 4, 1024], BF16)
    Pt = [ar.view(f"P{c}", 177 * KBY + c * 1024, [128, 512], BF16, prev=[lo_tmp]) for c in range(5)]
    Oh = ar.view("Oh", 172 * KBY + 10 * KBY, [128, 4, 1024], BF16) if False else None
    Oh = [ar.view(f"Oh{g}", 120 * KBY + g * 2 * KBY, [128, 1024], BF16) for g in range(4)] if False else None
    wi = 0
    nxt = wbufs[wi % 2]; wload(nxt, 0)
    ohst = ar.view("ohst", 182 * KBY, [128, 4, 128], BF16, prev=[lo_tmp])
    for hk in range(4):
        wq = wbufs[wi % 2]; wi += 1
        if hk < 3:
            wload(wbufs[wi % 2], (hk + 1) * 512)
        for g in range(4):
            for tt in range(2):
                p = ps[4 + (g * 2 + tt) % 2]
                for k in range(32):
                    kb.mm(p, p[:], wq, wq[:, k, g * 128:(g + 1) * 128], aT, aT[:, k, tt * 512:(tt + 1) * 512], k == 0, k == 31)
                rope_evac(kb, p, ps[6], cosb, cosb[:, tt * 512:(tt + 1) * 512], sinb, sinb[:, tt * 512:(tt + 1) * 512], rt, tmpb, t1, t2,
                          qT, qT[:, g, tt * 512:(tt + 1) * 512])
        for j in range(8):
            prev_c = ((j - 1) * 128, j - 1, 0) if j >= 1 else (1024, 8, 2)
            next_c = ((j + 1) * 128, j + 1, 1) if j <= 6 else (1152, 9, 3)
            chunks = [prev_c, (j * 128, j, None), next_c, (1280, 10, None), (1408, 11, None)]
            q_ap = qT[:, :, j * 128:(j + 1) * 128]
            for c, (ko, vt, mi) in enumerate(chunks):
                sp_ = ps[c % 4]
                kb.mm(sp_, sp_[:], kT, kT[:, hk, ko:ko + 128], qT, q_ap, True, True)
                kb.op("act", lambda h: h.activation(Pt[c][:], sp_[:], AF.Exp, scale=128 ** -0.5), reads=[sp_], writes=[Pt[c]])
                if mi is not None:
                    kb.op("dve", lambda h: h.tensor_tensor(Pt[c][:], Pt[c][:], mk[:, mi, :], ALU.mult), reads=[Pt[c], mk], writes=[Pt[c]])
            o_ps = ps[4 + j % 2]; d_ps = ps[6 + j % 2]
            for c, (ko, vt, mi) in enumerate(chunks):
                kb.mm(o_ps, o_ps[:], V, V[:, vt, hk * 128:(hk + 1) * 128], Pt[c], Pt[c][:], c == 0, c == 4)
            for c in range(5):
                kb.mm(d_ps, d_ps[:], ones, ones[:], Pt[c], Pt[c][:], c == 0, False)
            kb.mm(d_ps, d_ps[:], ones, ones[0:2, :], eskhl, eskhl[0:2, hk * 4:(hk + 1) * 4, :], False, True)
            kb.op("dve", lambda h: h.reciprocal(t1[:], d_ps[:]), reads=[d_ps], writes=[t1])
            kb.op("dve", lambda h: h.tensor_tensor(ohst[:], o_ps[:], t1[:], ALU.mult), reads=[o_ps, t1], writes=[ohst])
            kb.store(catT, catT.ap()[hk * 512:(hk + 1) * 512, j * 128:(j + 1) * 128].rearrange("(g d) t -> d g t", g=4), ohst, ohst[:])

    if d.get('stop') == 'B2':
        d['_stopped'] = True
        return
    r6v = [cosb, sinb, kT, V, qT, tmpb, t1, t2] + Pt
    zg = ar.view("zg", r6, [128, 512], F32, prev=r6v); sq = ar.view("sq", r6 + 2 * KBY, [128, 512], F32, prev=r6v)
    zn = ar.view("zn", r6 + 4 * KBY, [128, 8, 512], BF16, prev=r6v)
    uT = ar.view("uT", r6 + 12 * KBY, [128, 4, 1024], BF16, prev=r6v)
    wsT = ar.view("wsT", r6 + 20 * KBY, [128, 16, 128], BF16, prev=r6v)
    bsb = ar.view("bsb", r6 + 24 * KBY, [128, 2048], F32, prev=r6v)
    sgb = ar.view("sgb", r6 + 32 * KBY, [128, 2048], F32, prev=r6v)
    wsf = ar.view("wsf", r6 + 40 * KBY, [128, 128], F32, prev=r6v)
    st4 = ar.view("st4", r6 + 41 * KBY, [128, 8], F32, prev=r6v)
    Sst = ar.view("Sst", r6 + 42 * KBY, [128, 512], BF16, prev=r6v)
    idf = ar.view("idf", r6 + 43 * KBY, [128, 128], F32, prev=r6v)
    kb.load(bsb, bsb[:], sgu_bs, sgu_bs.ap()[0, :].partition_broadcast(128))
    kb.load(sgb, sgb[:], sgu_g, sgu_g.ap()[0, :].partition_broadcast(128))
    kb.load(idf, idf[:], identf, identf.ap())
    for g in range(16):
        kb.load(wsf, wsf[:], sgu_ws, sgu_ws.ap()[g])
        p = ps[g % 2]
        kb.op("pe", lambda h: h.transpose(p[:, 0:128], wsf[:], idf[:]), reads=[wsf, idf], writes=[p])
        kb.op("act", lambda h: h.activation(wsT[:, g, :], p[:, 0:128], AF.Copy), reads=[p], writes=[wsT])
    for cbk in range(4):
        wz = wbufs[wi % 2]; wi += 1
        wu = wbufs[wi % 2]; wi += 1
        wload(wz, 5120 + cbk * 512)
        wload(wu, 3072 + cbk * 512)
        for tq in range(8):
            p = ps[tq % 2]
            for k in range(32):
                kb.mm(p, p[:], aT, aT[:, k, tq * 128:(tq + 1) * 128], wz, wz[:, k, :], k == 0, k == 31)
            kb.op("act", lambda h: h.activation(zg[:], p[:], AF.Gelu), reads=[p], writes=[zg])
            zg3 = zg[:].rearrange("p (g c) -> p g c", g=4)
            kb.op("dve", lambda h: h.tensor_reduce(st4[:, 0:4], zg3, AX.X, ALU.add), reads=[zg], writes=[st4])
            kb.op("pool", lambda h: h.tensor_tensor(sq[:], zg[:], zg[:], ALU.mult), reads=[zg], writes=[sq])
            kb.op("dve", lambda h: h.tensor_reduce(st4[:, 4:8], sq[:].rearrange("p (g c) -> p g c", g=4), AX.X, ALU.add), reads=[sq], writes=[st4])
            kb.op("dve", lambda h: h.tensor_scalar(st4[:, 0:4], st4[:, 0:4], 1.0 / 128, None, ALU.mult), reads=[st4], writes=[st4])
            kb.op("dve", lambda h: h.tensor_tensor(sq[:, 0:4], st4[:, 0:4], st4[:, 0:4], ALU.mult), reads=[st4], writes=[sq])
            kb.op("dve", lambda h: h.scalar_tensor_tensor(st4[:, 4:8], st4[:, 4:8], 1.0 / 128, sq[:, 0:4], ALU.mult, ALU.subtract), reads=[st4, sq], writes=[st4])
            kb.op("act", lambda h: h.activation(st4[:, 4:8], st4[:, 4:8], AF.Sqrt, bias=epsb[:, 0:1], scale=1.0), reads=[st4, epsb], writes=[st4])
            kb.op("dve", lambda h: h.reciprocal(st4[:, 4:8], st4[:, 4:8]), reads=[st4], writes=[st4])
            for g4 in range(4):
                kb.op("dve", lambda h: h.tensor_scalar(zg[:, g4 * 128:(g4 + 1) * 128], zg[:, g4 * 128:(g4 + 1) * 128], st4[:, g4:g4 + 1], st4[:, 4 + g4:5 + g4],
                                                        ALU.subtract, ALU.mult), reads=[zg, st4], writes=[zg])
            kb.op("pool", lambda h: h.tensor_tensor(zn[:, tq, :], zg[:], sgb[:, cbk * 512:(cbk + 1) * 512], ALU.mult), reads=[zg, sgb], writes=[zn])
        for g4 in range(4):
            for tt in range(2):
                p = ps[2 + (g4 * 2 + tt) % 2]
                for k in range(32):
                    kb.mm(p, p[:], wu, wu[:, k, g4 * 128:(g4 + 1) * 128], aT, aT[:, k, tt * 512:(tt + 1) * 512], k == 0, k == 31)
                kb.op("act", lambda h: h.activation(uT[:, g4, tt * 512:(tt + 1) * 512], p[:], AF.Gelu), reads=[p], writes=[uT])
        for g4 in range(4):
            g = cbk * 4 + g4
            for half in range(2):
                p = ps[4 + half]
                for ch in range(4):
                    tq = half * 4 + ch
                    kb.op("pe", lambda h: h.matmul(p[:, ch * 128:(ch + 1) * 128], zn[:, tq, g4 * 128:(g4 + 1) * 128], wsT[:, g, :], start=True, stop=True),
                          reads=[zn, wsT], writes=[p], sig=(ch == 3))
                p3 = p[:].rearrange("p (c q) -> p c q", c=4)
                kb.op("dve", lambda h: h.tensor_tensor(sq[:].rearrange("p (c q) -> p c q", c=4), p3,
                                                        bsb[:, g * 128:(g + 1) * 128].unsqueeze(1).to_broadcast([128, 4, 128]), ALU.add),
                      reads=[p, bsb], writes=[sq])
                kb.op("dve", lambda h: h.tensor_tensor(Sst[:], sq[:], uT[:, g4, half * 512:(half + 1) * 512], ALU.mult), reads=[sq, uT], writes=[Sst])
                kb.store(catT, catT.ap()[2048 + g * 128:2048 + (g + 1) * 128, half * 512:(half + 1) * 512], Sst, Sst[:])
    if d.get('stop') == 'B3':
        d['_stopped'] = True
        return
    d["_ar"] = ar; d["_ps"] = ps; d["_dead"] = [aT, aTx, wA, wB, zg, sq, zn, uT, wsT, bsb, sgb, wsf, st4, Sst, idf, xt[0], xt[1], xn, ohst, qT, kT, V, cosb, sinb, t1, t2, tmpb] + Pt
    d["_epsb"] = epsb


def ffn_front(kb, ar, ps, dead, epsb, xres, catT, w_out, rowv, wr, identf, h1, bl, affT):
    KBY = 1024
    cT = ar.view("cT", 0, [128, 32, 1024], BF16, prev=dead)
    wo = [ar.view("wo0", 64 * KBY, [128, 32, 512], BF16, prev=dead), ar.view("wo1", 96 * KBY, [128, 32, 512], BF16, prev=dead)]
    r6 = 128 * KBY
    m2b = ar.view("m2b", r6, [128, 4096], F32, prev=dead)
    xs = [ar.view(f"xs{i}", r6 + 16 * KBY + i * 2 * KBY, [128, 512], F32, prev=dead) for i in range(2)]
    hs = [ar.view(f"hs{i}", r6 + 20 * KBY + i * 2 * KBY, [128, 512], F32, prev=dead) for i in range(2)]
    ssq = ar.view("ssq", r6 + 24 * KBY, [128, 8, 8], F32, prev=dead)
    junk = ar.view("junk", r6 + 25 * KBY, [128, 512], BF16, prev=dead)
    ssum = ar.view("ssum", r6 + 26 * KBY, [128, 8], F32, prev=dead)
    ones16 = ar.view("ones16", r6 + 26 * KBY + 64, [16, 16], F32, prev=dead)
    ex = ar.view("ex", r6 + 27 * KBY, [16, 128], F32, prev=dead)
    affs = ar.view("affs", r6 + 28 * KBY, [16, 1024], F32, prev=dead)
    wrb = ar.view("wrb", r6 + 32 * KBY, [128, 32, 16], F32, prev=dead)
    idf = ar.view("idf2", r6 + 34 * KBY, [128, 128], F32, prev=dead)
    kb.load(cT, cT[:], catT, catT.ap().rearrange("(k p) t -> p k t", p=128))
    kb.load(m2b, m2b[:], rowv, rowv.ap()[0, :].partition_broadcast(128))
    kb.load(wrb, wrb[:], wr, wr.ap())
    kb.load(idf, idf[:], identf, identf.ap())
    kb.op("dve", lambda h: h.memset(ones16[:], 1.0), writes=[ones16])
    wov = w_out.ap().rearrange("(k p) n -> p k n", p=128)

    def wload(t, c0):
        kb.dma("pool", lambda h: h.dma_start(out=t[:], in_=wov[:, :, c0:c0 + 512]), reads=[w_out], writes=[t], sembuf=t)
    wload(wo[0], 0)
    it = 0
    for ct in range(8):
        w = wo[ct % 2]
        if ct < 7:
            wload(wo[(ct + 1) % 2], (ct + 1) * 512)
        for tq in range(8):
            p = ps[it % 4]; x_ = xs[it % 2]; h_ = hs[it % 2]; it += 1
            kb.load(x_, x_[:], xres, xres.ap()[tq * 128:(tq + 1) * 128, ct * 512:(ct + 1) * 512])
            for k in range(32):
                kb.mm(p, p[:], cT, cT[:, k, tq * 128:(tq + 1) * 128], w, w[:, k, :], k == 0, k == 31)
            kb.op("dve", lambda h: h.tensor_tensor(h_[:], p[:], m2b[:, ct * 512:(ct + 1) * 512], ALU.mult), reads=[p, m2b], writes=[h_])
            kb.op("dve", lambda h: h.tensor_tensor(h_[:], h_[:], x_[:], ALU.add), reads=[h_, x_], writes=[h_])
            kb.op("act", lambda h: h.activation(junk[:], h_[:], AF.Square, accum_out=ssq[:, tq, ct:ct + 1]), reads=[h_], writes=[junk, ssq])
            kb.store(h1, h1.ap()[tq * 128:(tq + 1) * 128, ct * 512:(ct + 1) * 512], h_, h_[:])
    kb.op("dve", lambda h: h.tensor_reduce(ssum[:], ssq[:], AX.X, ALU.add), reads=[ssq], writes=[ssum])
    rms_rstd(kb, ssum[:], ssum, epsb, 4096)
    G4 = ar.view("G4", 0, [128, 4096], F32, prev=[cT]); S3 = ar.view("S3", 16 * KBY, [128, 4096], F32, prev=[cT])
    gf = ar.view("gf", 32 * KBY, [128, 4096], F32, prev=[cT]); m4 = ar.view("m4", 48 * KBY, [128, 4096], F32, prev=[cT])
    kb.load(gf, gf[:], rowv, rowv.ap()[1, :].partition_broadcast(128))
    kb.load(m4, m4[:], rowv, rowv.ap()[2, :].partition_broadcast(128))
    kb.load(S3, S3[:], rowv, rowv.ap()[3, :].partition_broadcast(128))
    kb.op("dve", lambda h: h.scalar_tensor_tensor(G4[:], m4[:], 1.0, gf[:], ALU.add, ALU.mult), reads=[m4, gf], writes=[G4])
    ht = ar.view("ht", 64 * KBY, [128, 4096], F32, prev=wo); bt = ar.view("bt", 80 * KBY, [128, 4096], F32, prev=wo)
    bb = ar.view("bb", 96 * KBY, [128, 4096], BF16, prev=wo); bT = ar.view("bT", 104 * KBY, [128, 32, 128], F32, prev=wo)
    for tq in range(8):
        kb.load(ht, ht[:], h1, h1.ap()[tq * 128:(tq + 1) * 128, :])
        kb.op("act", lambda h: h.activation(bt[:], ht[:], AF.Copy, scale=ssum[:, tq:tq + 1]), reads=[ht, ssum], writes=[bt])
        kb.op("dve", lambda h: h.tensor_tensor(bt[:], bt[:], G4[:], ALU.mult), reads=[bt, G4], writes=[bt])
        kb.op("pool", lambda h: h.tensor_tensor(bt[:], bt[:], S3[:], ALU.add), reads=[bt, S3], writes=[bt])
        kb.op("act", lambda h: h.activation(bb[:], bt[:], AF.Copy), reads=[bt], writes=[bb])
        kb.store(bl, bl.ap()[tq * 128:(tq + 1) * 128, :], bb, bb[:])
        for bk in range(8):
            p = ps[bk]
            for j in range(4):
                k = bk * 4 + j
                kb.op("pe", lambda h: h.transpose(p[:, j * 128:(j + 1) * 128], bt[:, k * 128:(k + 1) * 128], idf[:]), reads=[bt, idf], writes=[p], sig=(j == 3))
            o_ap = bT[:, bk * 4:(bk + 1) * 4, :]
            if bk % 2 == 0:
                kb.op("act", lambda h: h.activation(o_ap, p[:].rearrange("p (a b) -> p a b", a=4), AF.Copy), reads=[p], writes=[bT])
            else:
                kb.op("dve", lambda h: h.tensor_copy(o_ap, p[:].rearrange("p (a b) -> p a b", a=4)), reads=[p], writes=[bT])
        lg = ps[tq % 2]
        for k in range(32):
            kb.mm(lg, lg[0:16, 0:128], wrb, wrb[:, k, :], bT, bT[:, k, :], k == 0, k == 31)
        kb.op("act", lambda h: h.activation(ex[:], lg[0:16, 0:128], AF.Exp), reads=[lg], writes=[ex])
        sm = ps[2 + tq % 2]
        kb.mm(sm, sm[0:16, 0:128], ones16, ones16[:], ex, ex[:], True, True)
        kb.op("dve", lambda h: h.reciprocal(affs[:, tq * 128:(tq + 1) * 128], sm[0:16, 0:128]), reads=[sm], writes=[affs])
        kb.op("dve", lambda h: h.tensor_tensor(affs[:, tq * 128:(tq + 1) * 128], affs[:, tq * 128:(tq + 1) * 128], ex[:], ALU.mult), reads=[ex, affs], writes=[affs])
    kb.store(affT, affT.ap(), affs, affs[:])


def fm(v):
    return np.ascontiguousarray(np.asarray(v, np.float32).reshape(32, 128).T)


def consts_B(q):
    ident = np.eye(128, dtype=np.float32)
    R = np.zeros((128, 128), np.float32)
    for half in (0, 64):
        for i in range(32):
            R[half + i, half + i + 32] = -1.0
            R[half + i + 32, half + i] = 1.0
    cbf = np.stack([ident, R.T, np.ones((128, 128), np.float32)], 1).astype(NPBF)
    kk = np.arange(128)[:, None]; qq = np.arange(128)[None, :]
    m0 = (kk >= qq).astype(np.float32); m1 = (kk <= qq).astype(np.float32)
    ms = np.stack([m0, m1, m0 * (1.0 if q > 0 else 0.0), m1 * (1.0 if q < 3 else 0.0)], 0)
    masks = np.ascontiguousarray(np.tile(ms[:, :, None, :], (1, 1, 4, 1)).reshape(4, 128, 512).transpose(1, 0, 2)).astype(NPBF)
    t0 = 1024 * q
    tok = np.concatenate([np.arange(t0, t0 + 1024), np.arange(t0 - 128, t0), np.arange(t0 + 1024, t0 + 1152)])
    row = (tok // 64).astype(np.float32); col = (tok % 64).astype(np.float32)
    inv = (10000.0 ** (-np.arange(0, 64, 2, dtype=np.float32) / 64)).astype(np.float32)
    ang = np.zeros((128, 1280), np.float32)
    for d in range(128):
        pos = row if d < 64 else col
        ang[d] = pos * inv[d % 32]
    cosT = np.concatenate([np.cos(ang), np.ones((128, 256), np.float32)], 1).astype(np.float32)
    sinT = np.concatenate([np.sin(ang), np.zeros((128, 256), np.float32)], 1).astype(np.float32)
    return cbf, masks, cosT, sinT, ident


def launch_B(inp, mods, stop=None):
    nc = build_B(stop)
    maps = []
    x = inp["x"]; ctx = inp["ctx"]
    w_in = np.ascontiguousarray(inp["attn_sgu_w_in"][0]); w_out = np.ascontiguousarray(inp["attn_sgu_w_out"][0])
    wr = np.ascontiguousarray(np.asarray(inp["router_w"][0], np.float32).reshape(32, 128, 16).transpose(1, 0, 2))
    for c in range(NCORES):
        b, q = c // 4, c % 4
        t0 = 1024 * q
        z = np.zeros((128, D), np.float32)
        hp = x[b, t0 - 128:t0] if q > 0 else z
        hn = x[b, t0 + 1024:t0 + 1152] if q < 3 else z
        cbf, masks, cosT, sinT, ident = consts_B(q)
        gs = np.stack([fm(inp["norm_mix_g"][0]), fm(mods[0, b, 1]), fm(mods[0, b, 0]), fm(mods[0, 2, 1]), fm(mods[0, 2, 0])], 1)
        rowv = np.stack([mods[0, b, 2], inp["norm_ffn_g"][0], mods[0, b, 4], mods[0, b, 3]], 0).astype(np.float32)
        maps.append({
            "xo": np.ascontiguousarray(x[b, t0:t0 + 1024]), "xe": np.ascontiguousarray(np.concatenate([hp, hn, ctx[b]], 0)),
            "gs": np.ascontiguousarray(gs), "rowv": np.ascontiguousarray(rowv), "w_in": w_in, "w_out": w_out,
            "ropec": cosT, "ropes": sinT, "cbf": cbf, "masks": masks, "identf": ident,
            "sink": np.asarray(inp["attn_sink"][0], np.float32).reshape(1, 16),
            "sgu_ws": np.ascontiguousarray(inp["sgu_w_s"][0]), "sgu_bs": np.asarray(inp["sgu_b_s"][0], np.float32).reshape(1, 2048),
            "sgu_g": np.asarray(inp["sgu_norm_g"][0], np.float32).reshape(1, 2048), "wr": wr})
    r = run(nc, maps)
    return r


def build_C():
    kb = KB()
    affd = kb.dram("aff", [32, 4096], F32, IN)
    bld = kb.dram("bl", [8192, 4096], BF16, IN)
    wg = kb.dram("wg", [2, 4096, 1024], F32, IN)
    wu = kb.dram("wu", [2, 4096, 1024], F32, IN)
    wd = kb.dram("wd", [2, 1024, 4096], F32, IN)
    seld = kb.dram("sel", [32, 4], F32, IN)
    iotad = kb.dram("iota", [128, 512], F32, IN)
    rowidd = kb.dram("rowid", [128, 64], F32, IN)
    identd = kb.dram("ident", [128, 128], BF16, IN)
    ysd = kb.dram("ys", [4, 513, 4096], BF16, OUT)
    slotd = kb.dram("slotm", [32, 4096], F32, OUT)
    ps = psum_banks(kb)
    ar = Arena(kb, "arenaC", 204 * 1024)
    KBY = 1024
    aff = ar.view("aff", 0, [32, 4096], F32)
    msk = ar.view("msk", 16 * KBY, [32, 4096], F32)
    cum = ar.view("cum", 32 * KBY, [32, 4096], F32)
    onesr = ar.view("onesr", 48 * KBY, [32, 4096], F32)
    sm = ar.view("sm", 64 * KBY, [32, 16], F32)
    sel = ar.view("sel", 64 * KBY + 64, [32, 4], F32)
    kb.load(aff, aff[:], affd, affd.ap())
    kb.load(sel, sel[:], seld, seld.ap())
    lo, hi, mid, cnt, ge, tmp = (sm[:, i:i + 1] for i in range(6))
    kb.op("dve", lambda h: h.memset(sm[:], 0.0), writes=[sm])
    kb.op("dve", lambda h: h.memset(sm[:, 1:2], 1.0), writes=[sm])
    kb.op("dve", lambda h: h.memset(onesr[:], 1.0), writes=[onesr])
    for it in range(30):
        kb.op("dve", lambda h: h.scalar_tensor_tensor(mid, lo, 1.0, hi, ALU.mult, ALU.add), reads=[sm], writes=[sm])
        kb.op("dve", lambda h: h.tensor_scalar(mid, mid, 0.5, None, ALU.mult), reads=[sm], writes=[sm])
        kb.op("dve", lambda h: h.tensor_scalar(msk[:], aff[:], mid, None, ALU.is_gt), reads=[aff, sm], writes=[msk])
        kb.op("dve", lambda h: h.tensor_reduce(cnt, msk[:], AX.X, ALU.add), reads=[msk], writes=[sm])
        kb.op("dve", lambda h: h.tensor_scalar(ge, cnt, 511.5, None, ALU.is_gt), reads=[sm], writes=[sm])
        kb.op("dve", lambda h: h.tensor_tensor(tmp, mid, lo, ALU.subtract), reads=[sm], writes=[sm])
        kb.op("dve", lambda h: h.scalar_tensor_tensor(lo, tmp, ge, lo, ALU.mult, ALU.add), reads=[sm], writes=[sm])
        kb.op("dve", lambda h: h.tensor_tensor(tmp, hi, mid, ALU.subtract), reads=[sm], writes=[sm])
        kb.op("dve", lambda h: h.scalar_tensor_tensor(hi, tmp, ge, mid, ALU.mult, ALU.add), reads=[sm], writes=[sm])
    kb.op("dve", lambda h: h.tensor_scalar(msk[:], aff[:], lo, None, ALU.is_gt), reads=[aff, sm], writes=[msk])
    kb.op("dve", lambda h: h.tensor_tensor_scan(cum[:], onesr[:], msk[:], 0.0, ALU.mult, ALU.add), reads=[onesr, msk], writes=[cum])
    kb.op("dve", lambda h: h.tensor_scalar(cum[:], cum[:], -513.0, None, ALU.add), reads=[cum], writes=[cum])
    kb.op("dve", lambda h: h.tensor_tensor(cum[:], cum[:], msk[:], ALU.mult), reads=[cum, msk], writes=[cum])
    kb.op("dve", lambda h: h.tensor_scalar(cum[:], cum[:], 512.0, 512.0, ALU.add, ALU.min), reads=[cum], writes=[cum])
    kb.store(slotd, slotd.ap(), cum, cum[:])

    r1 = 65 * KBY
    stT = ar.view("stT", r1, [128, 32, 8], F32)
    iota = ar.view("iota", r1 + 1 * KBY, [128, 512], F32)
    rowid = ar.view("rowid", r1 + 3 * KBY, [128, 64], F32)
    ident = ar.view("identc", r1 + 4 * KBY, [128, 128], BF16)
    tv = ar.view("tv", r1 + 5 * KBY, [128, 32, 2], F32)
    oh = [ar.view(f"oh{i}", r1 + 6 * KBY + i * 2 * KBY, [128, 512], F32) for i in range(2)]
    idxf = ar.view("idxf", r1 + 10 * KBY, [128, 4, 2], F32)
    idxi = [ar.view(f"idxi{j}", r1 + 10 * KBY + 64 + j * 16, [128, 4], I32) for j in range(4)]
    gat = [ar.view(f"gat{j}", r1 + 10 * KBY + 128 + j * 16, [128, 4], F32) for j in range(4)]
    zrow = ar.view("zrow", r1 + 11 * KBY, [1, 4096], BF16)
    kb.load(iota, iota[:], iotad, iotad.ap())
    kb.load(rowid, rowid[:], rowidd, rowidd.ap())
    kb.load(ident, ident[:], identd, identd.ap())
    kb.op("dve", lambda h: h.memset(zrow[:], 0.0), writes=[zrow])
    for j in range(4):
        kb.store(ysd, ysd.ap()[j, 512:513, :], zrow, zrow[:])
    for ti in range(32):
        p = ps[ti % 2]
        kb.mm(p, p[:, 0:4], cum, cum[:, ti * 128:(ti + 1) * 128], sel, sel[:], True, True)
        kb.mm(p, p[:, 4:8], aff, aff[:, ti * 128:(ti + 1) * 128], sel, sel[:], True, True)
        kb.op("act", lambda h: h.activation(stT[:, ti, :], p[:, 0:8], AF.Copy), reads=[p], writes=[stT])
    for j in range(4):
        b = j % 2
        kb.op("dve", lambda h: h.tensor_copy(tv[:, :, 0], rowid[:, b * 32:(b + 1) * 32]), reads=[rowid], writes=[tv])
        kb.op("dve", lambda h: h.tensor_copy(tv[:, :, 1], stT[:, :, 4 + j]), reads=[stT], writes=[tv])
        ip = ps[2 + j % 2]
        for ti in range(32):
            o_ = oh[ti % 2]
            kb.op("dve", lambda h: h.tensor_scalar(o_[:], iota[:], stT[:, ti, j:j + 1], None, ALU.is_equal), reads=[iota, stT], writes=[o_])
            for sc in range(4):
                kb.op("pe", lambda h: h.matmul(ip[:, sc * 2:sc * 2 + 2], o_[:, sc * 128:(sc + 1) * 128], tv[:, ti, :], start=(ti == 0 and sc == 0), stop=(ti == 31),
                                               skip_group_check=True),
                      reads=[o_, tv], writes=[ip], sig=(sc == 3))
        kb.op("dve", lambda h: h.tensor_copy(idxf[:], ip[:, 0:8].rearrange("p (a b) -> p a b", a=4)), reads=[ip], writes=[idxf])
        kb.op("dve", lambda h: h.tensor_copy(idxi[j][:], idxf[:, :, 0]), reads=[idxf], writes=[idxi[j]])
        kb.op("dve", lambda h: h.tensor_copy(gat[j][:], idxf[:, :, 1]), reads=[idxf], writes=[gat[j]])

    route_dead = [aff, msk, cum, onesr, stT, iota, tv, oh[0], oh[1], rowid]
    xs = ar.view("xs", 84 * KBY, [128, 4096], BF16)
    xsT = [ar.view(f"xsT{b}", 92 * KBY + b * 32 * KBY, [128, 32, 512], BF16) for b in range(2)]
    actT = [ar.view(f"actT{b}", 156 * KBY + b * 8 * KBY, [128, 8, 512], BF16) for b in range(2)]
    wb = [ar.view("wb0", 0, [128, 32, 512], BF16, prev=route_dead), ar.view("wb1", 32 * KBY, [128, 32, 512], BF16, prev=route_dead),
          ar.view("wb2", 172 * KBY, [128, 32, 512], BF16)]
    wbd = [ar.t[:, 0:16384].rearrange("p (k n) -> p k n", k=8), ar.t[:, 16384:32768].rearrange("p (k n) -> p k n", k=8),
           ar.t[:, 86 * 1024:86 * 1024 + 16384].rearrange("p (k n) -> p k n", k=8)]
    sgt = ar.view("sgt", r1, [128, 512], F32, prev=route_dead)
    yst = [ar.view(f"yst{i}", r1 + 2 * KBY + i * KBY, [128, 512], BF16, prev=route_dead) for i in range(2)]
    wi = 0
    for el in range(2):
        for b in range(2):
            j = el * 2 + b
            for sc in range(4):
                kb.dma("pool", lambda h: h.indirect_dma_start(out=xs[:], out_offset=None, in_=bld.ap(),
                                                              in_offset=bass.IndirectOffsetOnAxis(ap=idxi[j][:, sc:sc + 1], axis=0)),
                       reads=[bld, idxi[j]], writes=[xs], sembuf=xs)
                for bk in range(4):
                    p = ps[4 + bk]
                    for q in range(8):
                        k = bk * 8 + q
                        kb.op("pe", lambda h: h.transpose(p.bf[:, q * 128:(q + 1) * 128], xs[:, k * 128:(k + 1) * 128], ident[:]), reads=[xs, ident], writes=[p], sig=(q == 7))
                    o_ap = xsT[b][:, bk * 8:(bk + 1) * 8, sc * 128:(sc + 1) * 128]
                    i_ap = p.bf[:].rearrange("p (a b) -> p a b", a=8)
                    if bk % 2 == 0:
                        kb.op("act", lambda h: h.activation(o_ap, i_ap, AF.Copy), reads=[p], writes=[xsT[b]])
                    else:
                        kb.op("dve", lambda h: h.tensor_copy(o_ap, i_ap), reads=[p], writes=[xsT[b]])
        wgv = wg.ap()[el].rearrange("(k p) n -> p k n", p=128)
        wuv = wu.ap()[el].rearrange("(k p) n -> p k n", p=128)
        wdv = wd.ap()[el].rearrange("(k p) n -> p k n", p=128)
        it = 0
        for hf in range(2):
            gi = wi % 3; wi += 1
            ui = wi % 3; wi += 1
            kb.dma("pool", lambda h: h.dma_start(out=wb[gi][:], in_=wgv[:, :, hf * 512:(hf + 1) * 512]), reads=[wg], writes=[wb[gi]], sembuf=wb[gi])
            kb.dma("pool", lambda h: h.dma_start(out=wb[ui][:], in_=wuv[:, :, hf * 512:(hf + 1) * 512]), reads=[wu], writes=[wb[ui]], sembuf=wb[ui])
            for b in range(2):
                for f4 in range(4):
                    gp = ps[(it * 2) % 4]; up = ps[(it * 2 + 1) % 4]; it += 1
                    for k in range(32):
                        kb.mm(gp, gp[:], wb[gi], wb[gi][:, k, f4 * 128:(f4 + 1) * 128], xsT[b], xsT[b][:, k, :], k == 0, k == 31)
                    for k in range(32):
                        kb.mm(up, up[:], wb[ui], wb[ui][:, k, f4 * 128:(f4 + 1) * 128], xsT[b], xsT[b][:, k, :], k == 0, k == 31)
                    kb.op("act", lambda h: h.activation(sgt[:], gp[:], AF.Silu), reads=[gp], writes=[sgt])
                    kb.op("dve", lambda h: h.tensor_tensor(actT[b][:, hf * 4 + f4, :], sgt[:], up[:], ALU.mult), reads=[sgt, up], writes=[actT[b]])
        it = 0
        for dq in range(2):
            di = wi % 3; wi += 1
            kb.dma("pool", lambda h: h.dma_start(out=wbd[di], in_=wdv[:, :, dq * 2048:(dq + 1) * 2048]), reads=[wd], writes=[wb[di]], sembuf=wb[di])
            for b in range(2):
                j = el * 2 + b
                for sc in range(4):
                    for c4 in range(4):
                        yp = ps[4 + it % 4]; ys_ = yst[it % 2]; it += 1
                        for k in range(8):
                            kb.mm(yp, yp[:], actT[b], actT[b][:, k, sc * 128:(sc + 1) * 128], wb[di], wbd[di][:, k, c4 * 512:(c4 + 1) * 512], k == 0, k == 7)
                        kb.op("act", lambda h: h.activation(ys_[:], yp[:], AF.Copy, scale=gat[j][:, sc:sc + 1]), reads=[yp, gat[j]], writes=[ys_])
                        c0 = dq * 2048 + c4 * 512
                        kb.store(ysd, ysd.ap()[j, sc * 128:(sc + 1) * 128, c0:c0 + 512], ys_, ys_[:])
    return kb.finish()


def launch_C(inp, layer, affT_all, bl_all):
    nc = build_C()
    iota = np.tile(np.arange(512, dtype=np.float32)[None], (128, 1))
    rowid = (np.arange(64, dtype=np.float32)[None, :] * 128 + np.arange(128, dtype=np.float32)[:, None]).astype(np.float32)
    ident = np.eye(128, dtype=np.float32).astype(NPBF)
    maps = []
    for c in range(NCORES):
        sel = np.zeros((32, 4), np.float32)
        for el in range(2):
            for b in range(2):
                sel[b * 16 + 2 * c + el, el * 2 + b] = 1.0
        maps.append({"aff": affT_all, "bl": bl_all,
                     "wg": np.ascontiguousarray(inp["expert_w_gate"][layer, 2 * c:2 * c + 2]),
                     "wu": np.ascontiguousarray(inp["expert_w_up"][layer, 2 * c:2 * c + 2]),
                     "wd": np.ascontiguousarray(inp["expert_w_down"][layer, 2 * c:2 * c + 2]),
                     "sel": sel, "iota": iota, "rowid": rowid, "ident": ident})
    r = run(nc, maps)
    ys_all = np.zeros((2, 16, 513, D), NPBF)
    for c in range(NCORES):
        for el in range(2):
            for b in range(2):
                ys_all[b, 2 * c + el] = r[c]["ys"][el * 2 + b]
    return ys_all, r[0]["slotm"]


def build_D(final):
    kb = KB()
    hin = kb.dram("hin", [1024, 4096], F32, IN)
    ysf = kb.dram("ysf", [16 * 513, 4096], BF16, IN)
    slotd = kb.dram("slot", [16, 1024], F32, IN)
    rows = kb.dram("rows", [2, 4096], F32, IN)
    cf = kb.dram("cf", [128, 32], F32, IN)
    identd = kb.dram("ident", [128, 128], BF16, IN)
    if final:
        outd = kb.dram("out", [1024, 4096], F32, OUT)
    else:
        gsd = kb.dram("gs", [128, 3, 32], F32, IN)
        h2d = kb.dram("h2", [1024, 4096], F32, OUT)
        aTd = kb.dram("aT1", [4096, 1024], BF16, OUT)
    ps = psum_banks(kb)
    ar = Arena(kb, "arenaD", 204 * 1024)
    KBY = 1024
    G = [ar.view(f"G{i}", i * 8 * KBY, [128, 4096], BF16) for i in range(4)]
    hi_ = ar.view("hi", 32 * KBY, [128, 4096], F32)
    h2t = ar.view("h2t", 48 * KBY, [128, 4096], F32)
    m5b = ar.view("m5b", 64 * KBY, [128, 4096], F32)
    xn = ar.view("xn", 80 * KBY, [128, 4096], BF16)
    big = ar.view("big", 88 * KBY, [128, 32, 1024], BF16) if not final else ar.view("gfin", 88 * KBY, [128, 4096], F32)
    slot = ar.view("slot", 152 * KBY, [16, 1024], F32)
    cfb = ar.view("cfb", 156 * KBY, [128, 32], F32)
    ident = ar.view("identd", 156 * KBY + 128, [128, 128], BF16)
    posf = ar.view("posf", 157 * KBY, [128, 16], F32)
    posi = ar.view("posi", 157 * KBY + 64, [128, 16], I32)
    ssb = ar.view("ssb", 157 * KBY + 128, [128, 8], F32)
    epsb = ar.view("epsb", 157 * KBY + 160, [128, 1], F32)
    GS = ar.view("GS", 158 * KBY, [128, 2, 32], F32)
    gsb = ar.view("gsb", 158 * KBY + 256, [128, 3, 32], F32)
    kb.load(slot, slot[:], slotd, slotd.ap())
    kb.load(cfb, cfb[:], cf, cf.ap())
    kb.load(ident, ident[:], identd, identd.ap())
    kb.load(m5b, m5b[:], rows, rows.ap()[0, :].partition_broadcast(128))
    kb.op("dve", lambda h: h.memset(epsb[:], 1e-6), writes=[epsb])
    if final:
        kb.load(big, big[:], rows, rows.ap()[1, :].partition_broadcast(128))
    else:
        kb.load(gsb, gsb[:], gsd, gsd.ap())
        kb.op("dve", lambda h: h.scalar_tensor_tensor(GS[:, 0, :], gsb[:, 1, :], 1.0, gsb[:, 0, :], ALU.add, ALU.mult), reads=[gsb], writes=[GS])
        kb.op("dve", lambda h: h.tensor_copy(GS[:, 1, :], gsb[:, 2, :]), reads=[gsb], writes=[GS])
    gi = 0
    for tq in range(8):
        pp = ps[0]
        kb.mm(pp, pp[:, 0:16], slot, slot[0:16, tq * 128:(tq + 1) * 128], cfb, cfb[0:16, 0:16], True, True)
        kb.op("dve", lambda h: h.tensor_tensor(posf[:], pp[:, 0:16], cfb[:, 16:32], ALU.add), reads=[pp, cfb], writes=[posf])
        kb.op("dve", lambda h: h.tensor_copy(posi[:], posf[:]), reads=[posf], writes=[posi])
        kb.load(hi_, hi_[:], hin, hin.ap()[tq * 128:(tq + 1) * 128, :])
        for e in range(16):
            g_ = G[gi % 4]; gi += 1
            kb.dma("pool", lambda h: h.indirect_dma_start(out=g_[:], out_offset=None, in_=ysf.ap(),
                                                          in_offset=bass.IndirectOffsetOnAxis(ap=posi[:, e:e + 1], axis=0)),
                   reads=[ysf, posi], writes=[g_], sembuf=g_)
            for ct in range(8):
                kb.mm(ps[ct], ps[ct][:], ident, ident[:], g_, g_[:, ct * 512:(ct + 1) * 512], e == 0, e == 15, sig=(ct == 7 or e == 15))
        for ct in range(8):
            sl = slice(ct * 512, (ct + 1) * 512)
            kb.op("dve", lambda h: h.tensor_tensor(h2t[:, sl], ps[ct][:], m5b[:, sl], ALU.mult), reads=[ps[ct], m5b], writes=[h2t])
        kb.op("dve", lambda h: h.tensor_tensor(h2t[:], h2t[:], hi_[:], ALU.add), reads=[h2t, hi_], writes=[h2t])
        kb.op("act", lambda h: h.activation(xn[:], h2t[:], AF.Square, accum_out=ssb[:, 0:1]), reads=[h2t], writes=[xn, ssb])
        rms_rstd(kb, ssb[:, 0:1], ssb, epsb, 4096)
        if final:
            kb.op("act", lambda h: h.activation(hi_[:], h2t[:], AF.Copy, scale=ssb[:, 0:1]), reads=[h2t, ssb], writes=[hi_])
            kb.op("dve", lambda h: h.tensor_tensor(hi_[:], hi_[:], big[:], ALU.mult), reads=[hi_, big], writes=[hi_])
            kb.store(outd, outd.ap()[tq * 128:(tq + 1) * 128, :], hi_, hi_[:])
        else:
            kb.store(h2d, h2d.ap()[tq * 128:(tq + 1) * 128, :], h2t, h2t[:])
            kb.op("act", lambda h: h.activation(xn[:], h2t[:], AF.Copy, scale=ssb[:, 0:1]), reads=[h2t, ssb], writes=[xn])
            for bk in range(4):
                p = ps[bk]
                for j in range(8):
                    k = bk * 8 + j
                    kb.op("pe", lambda h: h.transpose(p.bf[:, j * 128:(j + 1) * 128], xn[:, k * 128:(k + 1) * 128], ident[:]), reads=[xn, ident], writes=[p], sig=(j == 7))
                for j in range(8):
                    k = bk * 8 + j
                    o_ap = big[:, k, tq * 128:(tq + 1) * 128]; i_ap = p.bf[:, j * 128:(j + 1) * 128]
                    if bk % 2 == 0:
                        kb.op("act", lambda h: h.activation(o_ap, i_ap, AF.Identity, bias=GS[:, 1, k:k + 1], scale=GS[:, 0, k:k + 1]), reads=[p, GS], writes=[big])
                    else:
                        kb.op("dve", lambda h: h.tensor_scalar(o_ap, i_ap, GS[:, 0, k:k + 1], GS[:, 1, k:k + 1], ALU.mult, ALU.add), reads=[p, GS], writes=[big])
    if not final:
        kb.store(aTd, aTd.ap().rearrange("(k p) t -> p k t", p=128), big, big[:])
    return kb.finish()


def launch_D(inp, final, h_in, ys_all, slotm, mods, layer):
    nc = build_D(final)
    cf = np.zeros((128, 32), np.float32)
    cf[0:16, 0:16] = np.eye(16, dtype=np.float32)
    cf[:, 16:32] = (np.arange(16, dtype=np.float32) * 513)[None, :]
    ident = np.eye(128, dtype=np.float32).astype(NPBF)
    maps = []
    for c in range(NCORES):
        b, q = c // 4, c % 4
        m = {"hin": np.ascontiguousarray(h_in[b, q * 1024:(q + 1) * 1024]), "ysf": ys_all[b].reshape(16 * 513, D),
             "slot": np.ascontiguousarray(slotm[b * 16:(b + 1) * 16, q * 1024:(q + 1) * 1024]),
             "rows": np.stack([mods[layer, b, 5], np.asarray(inp["final_norm_g"], np.float32)], 0).astype(np.float32),
             "cf": cf, "ident": ident}
        if not final:
            m["gs"] = np.ascontiguousarray(np.stack([fm(inp["norm_mix_g"][1]), fm(mods[1, b, 1]), fm(mods[1, b, 0])], 1))
        maps.append(m)
    return run(nc, maps)


def build_F():
    kb = KB()
    xres = kb.dram("xres", [1024, 4096], F32, IN)
    catT = kb.dram("catT", [4096, 1024], BF16, IN)
    w_out = kb.dram("w_out", [4096, 4096], F32, IN)
    rowv = kb.dram("rowv", [4, 4096], F32, IN)
    wr = kb.dram("wr", [128, 32, 16], F32, IN)
    identf = kb.dram("identf", [128, 128], F32, IN)
    h1 = kb.dram("h1", [1024, 4096], F32, OUT)
    bl = kb.dram("bl", [1024, 4096], BF16, OUT)
    affT = kb.dram("affT", [16, 1024], F32, OUT)
    ps = psum_banks(kb)
    ar = Arena(kb, "arenaF", 204 * 1024)
    epsb = ar.view("epsb", 200 * 1024, [128, 1], F32)
    kb.op("dve", lambda h: h.memset(epsb[:], 1e-6), writes=[epsb])
    ffn_front(kb, ar, ps, [], epsb, xres, catT, w_out, rowv, wr, identf, h1, bl, affT)
    return kb.finish()


def launch_F(inp, mods, h2, finT_all):
    nc = build_F()
    w_out = np.ascontiguousarray(inp["hyena_w_out"][0])
    wr = np.ascontiguousarray(np.asarray(inp["router_w"][1], np.float32).reshape(32, 128, 16).transpose(1, 0, 2))
    ident = np.eye(128, dtype=np.float32)
    maps = []
    for c in range(NCORES):
        b, q = c // 4, c % 4
        rowv = np.stack([mods[1, b, 2], inp["norm_ffn_g"][1], mods[1, b, 4], mods[1, b, 3]], 0).astype(np.float32)
        maps.append({"xres": np.ascontiguousarray(h2[b, q * 1024:(q + 1) * 1024]),
                     "catT": np.ascontiguousarray(finT_all[b][:, q * 1024:(q + 1) * 1024]),
                     "w_out": w_out, "rowv": np.ascontiguousarray(rowv), "wr": wr, "identf": ident})
    return run(nc, maps)


def gather_rows(r, key):
    return np.stack([np.concatenate([r[b * 4 + q][key] for q in range(4)], 0) for b in range(2)], 0)


def hy_consts():
    N = 8192
    P = np.arange(256)[:, None]; FP = np.arange(256)[None, :]
    w = 2 * np.pi * P * FP / 256
    W256cat = np.stack([np.concatenate([np.cos(w[h * 128:(h + 1) * 128]), -np.sin(w[h * 128:(h + 1) * 128])], 1) for h in range(2)], 1)
    m = np.arange(128); a_of = m // 4; c_of = m % 4
    tw = 2 * np.pi * a_of[:, None] * np.arange(256)[None, :] / N
    TW = np.stack([np.cos(tw), -np.sin(tw)], 1)
    ang32 = 2 * np.pi * a_of[:, None] * a_of[None, :] / 32
    same = (c_of[:, None] == c_of[None, :]).astype(np.float64)
    M32 = np.stack([np.cos(ang32) * same, -np.sin(ang32) * same], 1)
    Vre = np.cos(ang32) * same; Vim = np.sin(ang32) * same
    Vcat = np.stack([np.concatenate([Vre, Vim], 1), np.concatenate([-Vim, Vre], 1)], 1)
    T2 = np.zeros((128, 2, 2, 128)); U = np.zeros((128, 2, 2, 128))
    for jc in range(2):
        fp = jc * 128 + np.arange(128)
        t2 = 2 * np.pi * fp[:, None] * a_of[None, :] / N
        T2[:, jc, 0] = np.cos(t2); T2[:, jc, 1] = np.sin(t2)
        u = 2 * np.pi * fp[:, None] * np.arange(128)[None, :] / 256
        U[:, jc, 0] = np.cos(u) / N; U[:, jc, 1] = -np.sin(u) / N
    return (W256cat.astype(NPBF), TW.astype(np.float32), M32.astype(NPBF), Vcat.astype(NPBF), T2.astype(np.float32), U.astype(NPBF))


def build_E():
    kb = KB()
    aTf = kb.dram("aTf", [2, 4096, 4098], BF16, IN)
    wind = kb.dram("win", [4096, 1536], F32, IN)
    cwd = kb.dram("cw", [128, 12, 4], F32, IN)
    hbd = kb.dram("hb", [128, 4], F32, IN)
    featd = kb.dram("feat", [2, 33, 4096], F32, IN)
    mlpd = kb.dram("mlp", [64, 200], F32, IN)
    w3d = kb.dram("w3", [64, 2, 512], F32, IN)
    t01d = kb.dram("t01", [128, 2, 32], F32, IN)
    deld = kb.dram("delt", [128, 512], F32, IN)
    m0d = kb.dram("m0", [128, 1], F32, IN)
    c_w256 = kb.dram("c_w256", [128, 2, 512], BF16, IN)
    c_tw = kb.dram("c_tw", [128, 2, 256], F32, IN)
    c_m32 = kb.dram("c_m32", [128, 2, 128], BF16, IN)
    c_vcat = kb.dram("c_vcat", [128, 2, 256], BF16, IN)
    c_t2 = kb.dram("c_t2", [128, 2, 2, 128], F32, IN)
    c_u = kb.dram("c_u", [128, 2, 2, 128], BF16, IN)
    c_id = kb.dram("c_id", [128, 128], BF16, IN)
    Khd = kb.dram("Kh", [128, 128, 512], F32, OUT)
    find = kb.dram("finT", [512, 2, 4096], BF16, OUT)
    ps = psum_banks(kb)
    ar = Arena(kb, "arenaE", 204 * 1024)
    KBY = 1024
    o = 160 * KBY
    def cv(name, shape, dt):
        nonlocal o
        n = 1
        for s_ in shape[1:]: n *= s_
        b_ = ar.view(name, o, shape, dt); o += ((n * (2 if dt == BF16 else 4) + 3) // 4) * 4
        return b_
    w256 = cv("w256", [128, 2, 512], BF16); tw = cv("tw", [128, 2, 256], F32); m32 = cv("m32", [128, 2, 128], BF16)
    vcat = cv("vcat", [128, 2, 256], BF16); t2c = cv("t2c", [128, 4, 128], F32); uc = cv("uc", [128, 4, 128], BF16)
    ident = cv("identE", [128, 128], BF16); cwb = cv("cwb", [128, 12, 4], F32); hbb = cv("hbb", [128, 4], F32)
    rn = cv("rn", [128, 4], F32); m0 = cv("m0", [128, 1], F32); t01 = cv("t01", [128, 2, 32], F32)
    hpi = cv("hpi", [128, 1], F32); onesb = cv("onesb", [128, 1], BF16)
    ta = [cv(f"ta{i}", [128, 256], F32) for i in range(4)]
    Bp = cv("Bp", [128, 768], BF16); Yh = cv("Yh", [128, 512], BF16)
    Cp = cv("Cp", [128, 2, 2, 512], BF16)
    Kb = [cv(f"Kb{i}", [128, 512], F32) for i in range(2)]
    assert o <= 204 * KBY, o
    for (dst, src) in ((w256, c_w256), (tw, c_tw), (m32, c_m32), (vcat, c_vcat), (ident, c_id), (cwb, cwd), (hbb, hbd), (m0, m0d), (t01, t01d)):
        kb.load(dst, dst[:], src, src.ap())
    kb.load(t2c, t2c[:], c_t2, c_t2.ap().rearrange("p a b c -> p (a b) c"))
    kb.load(uc, uc[:], c_u, c_u.ap().rearrange("p a b c -> p (a b) c"))
    kb.op("dve", lambda h: h.memset(hpi[:], math.pi / 2), writes=[hpi])
    kb.op("dve", lambda h: h.memset(onesb[:], 1.0), writes=[onesb])

    ktok = ar.view("ktok", 32 * KBY, [128, 2, 128, 128], BF16)
    r0 = 96 * KBY
    feat = ar.view("feat", r0, [33, 4096], F32)
    hid1 = ar.view("hid1", r0 + 16 * KBY, [64, 4096], F32)
    hid2 = [ar.view(f"hid2{i}", r0 + 32 * KBY + i * 16 * KBY, [64, 4096], F32) for i in range(2)]
    zreg = 0
    mlp = ar.view("mlp", zreg, [64, 200], F32)
    w3 = ar.view("w3", zreg + 1 * KBY, [64, 2, 512], F32)
    delt = ar.view("delt", zreg + 5 * KBY, [128, 512], F32)
    dec = ar.view("dec", zreg + 7 * KBY, [128, 512], F32)
    kf = ar.view("kf", zreg + 9 * KBY, [128, 512], F32)
    kab = ar.view("kab", zreg + 11 * KBY, [128, 512], BF16)
    s1 = ar.view("s1", zreg + 12 * KBY, [64, 512], F32); s2 = ar.view("s2", zreg + 14 * KBY, [64, 512], F32); sa = ar.view("sa", zreg + 16 * KBY, [64, 512], F32)
    sx = ar.view("sx", zreg + 18 * KBY, [64, 512], F32)
    kb.load(mlp, mlp[:], mlpd, mlpd.ap()); kb.load(w3, w3[:], w3d, w3d.ap()); kb.load(delt, delt[:], deld, deld.ap())

    def sin_layer(src_ps, bcol, fcol, dst_ap, dstb):
        kb.op("dve", lambda h: h.tensor_scalar(sx[:], src_ps[0:64, :], mlp[:, bcol:bcol + 1], mlp[:, fcol:fcol + 1], ALU.add, ALU.mult), reads=[src_ps, mlp], writes=[sx])
        kb.op("act", lambda h: h.activation(s1[:], sx[:], AF.Sin, scale=0.5), reads=[sx], writes=[s1])
        kb.op("act", lambda h: h.activation(sa[:], sx[:], AF.Abs, scale=0.5), reads=[sx], writes=[sa])
        kb.op("act", lambda h: h.activation(s2[:], sa[:], AF.Sin, bias=hpi[0:64, 0:1], scale=-1.0), reads=[sa, hpi], writes=[s2])
        kb.op("dve", lambda h: h.scalar_tensor_tensor(dst_ap, s1[:], 2.0, s2[:], ALU.mult, ALU.mult), reads=[s1, s2], writes=[dstb])

    for half in range(2):
        kb.load(feat, feat[:], featd, featd.ap()[half])
        for tt in range(8):
            p = ps[tt % 2]
            kb.mm(p, p[0:64, :], mlp, mlp[0:33, 0:64], feat, feat[:, tt * 512:(tt + 1) * 512], True, True)
            sin_layer(p, 128, 130, hid1[:, tt * 512:(tt + 1) * 512], hid1)
        for tt in range(8):
            p = ps[2 + tt % 2]
            kb.mm(p, p[0:64, :], mlp, mlp[:, 64:128], hid1, hid1[:, tt * 512:(tt + 1) * 512], True, True)
            sin_layer(p, 129, 131, hid2[half][:, tt * 512:(tt + 1) * 512], hid2[half])
    nps = ps[7]
    first = True
    for half in range(2):
        h3 = hid2[half][:].rearrange("k (p a) -> k a p", a=32)
        for a in range(32):
            p = ps[4 + a % 2]
            kb.mm(p, p[:], hid2[half], h3[:, a, :], w3, w3[:, half, :], True, True)
            kb.op("act", lambda h: h.activation(dec[:], delt[:], AF.Exp, scale=t01[:, half, a:a + 1]), reads=[delt, t01], writes=[dec])
            kb.op("dve", lambda h: h.tensor_tensor(kf[:], p[:], dec[:], ALU.mult), reads=[p, dec], writes=[kf])
            if half == 1 and a == 0:
                kb.op("dve", lambda h: h.tensor_scalar(kf[:], kf[:], m0[:, 0:1], None, ALU.mult), reads=[kf, m0], writes=[kf])
            kb.op("act", lambda h: h.activation(ktok[:, half, :, a * 4:(a + 1) * 4], kf[:].rearrange("p (g c) -> p g c", c=4), AF.Copy), reads=[kf], writes=[ktok])
            kb.op("act", lambda h: h.activation(kab[:], kf[:], AF.Abs), reads=[kf], writes=[kab])
            for cc in range(4):
                last = (half == 1 and a == 31)
                kb.op("pe", lambda h: h.matmul(nps[:, cc:cc + 1], kab[:, cc * 128:(cc + 1) * 128], onesb[:], start=first, stop=last, skip_group_check=True),
                      reads=[kab, onesb], writes=[nps], sig=(cc == 3))
                first = False
    kb.op("dve", lambda h: h.reciprocal(rn[:], nps[:, 0:4]), reads=[nps], writes=[rn])

    cnt = {"g": 0}

    def fft_fwd(srcb, src_aps):
        i = cnt["g"]; cnt["g"] += 1
        p1 = ps[i % 2]; p2 = ps[2 + i % 2]
        for hh, ap_ in enumerate(src_aps):
            kb.mm(p1, p1[:], srcb, ap_, w256, w256[:, hh, :], hh == 0, hh == len(src_aps) - 1)
        bre = p1[:, 0:256]; bim = p1[:, 256:512]
        kb.op("dve", lambda h: h.tensor_tensor(ta[0][:], bre, tw[:, 0, :], ALU.mult), reads=[p1, tw], writes=[ta[0]])
        kb.op("dve", lambda h: h.tensor_tensor(ta[1][:], bim, tw[:, 1, :], ALU.mult), reads=[p1, tw], writes=[ta[1]])
        kb.op("dve", lambda h: h.tensor_tensor(ta[2][:], bre, tw[:, 1, :], ALU.mult), reads=[p1, tw], writes=[ta[2]])
        kb.op("dve", lambda h: h.tensor_tensor(ta[3][:], bim, tw[:, 0, :], ALU.mult), reads=[p1, tw], writes=[ta[3]])
        kb.op("dve", lambda h: h.tensor_tensor(Bp[:, 256:512], ta[0][:], ta[1][:], ALU.subtract), reads=[ta[0], ta[1]], writes=[Bp])
        kb.op("dve", lambda h: h.tensor_tensor(ta[2][:], ta[2][:], ta[3][:], ALU.add), reads=[ta[2], ta[3]], writes=[ta[2]])
        kb.op("act", lambda h: h.activation(Bp[:, 512:768], ta[2][:], AF.Copy), reads=[ta[2]], writes=[Bp])
        kb.op("act", lambda h: h.activation(Bp[:, 0:256], ta[2][:], AF.Copy, scale=-1.0), reads=[ta[2]], writes=[Bp])
        kb.mm(p2, p2[:], m32, m32[:, 0, :], Bp, Bp[:, 256:768], True, False)
        kb.mm(p2, p2[:], m32, m32[:, 1, :], Bp, Bp[:, 0:512], False, True)
        return p2

    for g in range(128):
        p2 = fft_fwd(ktok, [ktok[:, hh, g, :] for hh in range(2)])
        kbuf = Kb[g % 2]
        kb.op("act", lambda h: h.activation(kbuf[:], p2[:], AF.Copy), reads=[p2], writes=[kbuf])
        kb.store(Khd, Khd.ap()[g], kbuf, kbuf[:])

    filt_dead = [ktok, feat, hid1, hid2[0], hid2[1], mlp, w3, delt, dec, kf, kab, s1, s2, sa, sx]
    zT = ar.view("zT", 0, [128, 4, 4096], BF16, prev=filt_dead)
    atile = [ar.view(f"at{i}", 32 * KBY + i * 32 * KBY, [128, 32, 512], BF16, prev=filt_dead) for i in range(2)]
    ztok = ar.view("ztok", 32 * KBY, [128, 128, 128], BF16, prev=filt_dead)
    ytok = ar.view("ytok", 64 * KBY, [128, 32, 512], BF16, prev=filt_dead)
    ztok.w = atile[0].w; ztok.r = atile[0].r; ytok.w = atile[1].w; ytok.r = atile[1].r
    yT = ar.view("yT", 96 * KBY, [128, 4, 4096], BF16, prev=filt_dead)
    wp = ar.view("wp", 128 * KBY, [128, 32, 512], BF16, prev=filt_dead)
    ct_ = [ar.view(f"ct{i}", 96 * KBY + i * 2 * KBY, [128, 512], F32, prev=filt_dead) for i in range(2)]
    cx = [ar.view(f"cx{i}", 156 * KBY + i * 2 * KBY, [128, 512], F32) for i in range(2)] if False else None
    winv = wind.ap().rearrange("(k p) n -> p k n", p=128)
    tiles = [(510 * i, 510) for i in range(8)] + [(4080, 16)]
    for b in range(2):
        xdead = [ztok, ytok] if b > 0 else []
        for part in (1, 2, 0):
            kb.dma("pool", lambda h: h.dma_start(out=wp[:], in_=winv[:, :, part * 512:(part + 1) * 512]), reads=[wind], writes=[wp], sembuf=wp)
            if part == 0:
                for cbk in range(4):
                    for a0 in range(0, 32, 8):
                        p = ps[4 + (a0 // 8) % 2]
                        z3 = zT[:, cbk, :].rearrange("c (p a) -> c a p", a=32)
                        for q in range(8):
                            kb.op("pe", lambda h: h.transpose(p.bf[:, q * 128:(q + 1) * 128], z3[:, a0 + q, :], ident[:]), reads=[zT, ident], writes=[p], sig=(q == 7))
                        kb.op("act", lambda h: h.activation(ztok[:, cbk * 32:(cbk + 1) * 32, a0 * 4:(a0 + 8) * 4].rearrange("p g (a c) -> p g a c", c=4), p.bf[:].rearrange("p (a g c) -> p g a c", a=8, c=4), AF.Copy),
                              reads=[p], writes=[ztok])
                kb.load(Kb[0], Kb[0][:], Khd, Khd.ap()[0])
                for g in range(128):
                    if g < 127:
                        kb.load(Kb[(g + 1) % 2], Kb[(g + 1) % 2][:], Khd, Khd.ap()[g + 1])
                    kbuf = Kb[g % 2]
                    p2 = fft_fwd(ztok, [ztok[:, g, :]])
                    zre = p2[:, 0:256]; zim = p2[:, 256:512]
                    kb.op("dve", lambda h: h.tensor_tensor(ta[0][:], zre, kbuf[:, 0:256], ALU.mult), reads=[p2, kbuf], writes=[ta[0]])
                    kb.op("dve", lambda h: h.tensor_tensor(ta[1][:], zim, kbuf[:, 256:512], ALU.mult), reads=[p2, kbuf], writes=[ta[1]])
                    kb.op("dve", lambda h: h.tensor_tensor(ta[2][:], zre, kbuf[:, 256:512], ALU.mult), reads=[p2, kbuf], writes=[ta[2]])
                    kb.op("dve", lambda h: h.tensor_tensor(ta[3][:], zim, kbuf[:, 0:256], ALU.mult), reads=[p2, kbuf], writes=[ta[3]])
                    kb.op("dve", lambda h: h.tensor_tensor(Yh[:, 0:256], ta[0][:], ta[1][:], ALU.subtract), reads=[ta[0], ta[1]], writes=[Yh])
                    kb.op("dve", lambda h: h.tensor_tensor(Yh[:, 256:512], ta[2][:], ta[3][:], ALU.add), reads=[ta[2], ta[3]], writes=[Yh])
                    g4 = g % 4
                    for jc in range(2):
                        p3 = ps[4 + jc]
                        kb.mm(p3, p3[:, 0:256], Yh, Yh[:, jc * 128:(jc + 1) * 128], vcat, vcat[:, 0, :], True, False)
                        kb.mm(p3, p3[:, 0:256], Yh, Yh[:, 256 + jc * 128:256 + (jc + 1) * 128], vcat, vcat[:, 1, :], False, True)
                        cre = p3[:, 0:128]; cim = p3[:, 128:256]
                        tre = t2c[:, jc * 2, :]; tim = t2c[:, jc * 2 + 1, :]
                        kb.op("dve", lambda h: h.tensor_tensor(ta[0][:, 0:128], cre, tre, ALU.mult), reads=[p3, t2c], writes=[ta[0]])
                        kb.op("dve", lambda h: h.tensor_tensor(ta[1][:, 0:128], cim, tim, ALU.mult), reads=[p3, t2c], writes=[ta[1]])
                        kb.op("dve", lambda h: h.tensor_tensor(ta[2][:, 0:128], cre, tim, ALU.mult), reads=[p3, t2c], writes=[ta[2]])
                        kb.op("dve", lambda h: h.tensor_tensor(ta[3][:, 0:128], cim, tre, ALU.mult), reads=[p3, t2c], writes=[ta[3]])
                        kb.op("dve", lambda h: h.tensor_tensor(Cp[:, jc, 0, g4 * 128:(g4 + 1) * 128], ta[0][:, 0:128], ta[1][:, 0:128], ALU.subtract), reads=[ta[0], ta[1]], writes=[Cp])
                        kb.op("dve", lambda h: h.tensor_tensor(Cp[:, jc, 1, g4 * 128:(g4 + 1) * 128], ta[2][:, 0:128], ta[3][:, 0:128], ALU.add), reads=[ta[2], ta[3]], writes=[Cp])
                    if g4 == 3:
                        G4 = g // 4
                        p4 = ps[6 + G4 % 2]
                        kb.mm(p4, p4[:], uc, uc[:, 0, :], Cp, Cp[:, 0, 0, :], True, False)
                        kb.mm(p4, p4[:], uc, uc[:, 1, :], Cp, Cp[:, 0, 1, :], False, False)
                        kb.mm(p4, p4[:], uc, uc[:, 2, :], Cp, Cp[:, 1, 0, :], False, False)
                        kb.mm(p4, p4[:], uc, uc[:, 3, :], Cp, Cp[:, 1, 1, :], False, True)
                        kb.op("act", lambda h: h.activation(ytok[:, :, 16 * G4:16 * G4 + 16].rearrange("p a (g c) -> p a g c", g=4),
                                                             p4[:].rearrange("p (g a c) -> p a g c", g=4, a=32), AF.Copy), reads=[p4], writes=[ytok])
                for cbk in range(4):
                    y3 = yT[:, cbk, :].rearrange("c (p a) -> c a p", a=32)
                    for a0 in range(0, 32, 8):
                        p = ps[(a0 // 8) % 2]
                        for q in range(8):
                            kb.op("pe", lambda h: h.transpose(p.bf[:, q * 128:(q + 1) * 128], ytok[:, a0 + q, cbk * 128:(cbk + 1) * 128], ident[:]), reads=[ytok, ident], writes=[p], sig=(q == 7))
                        kb.op("act", lambda h: h.activation(y3[:, a0:a0 + 8, :], p.bf[:].rearrange("c (a p) -> c a p", a=8), AF.Copy), reads=[p], writes=[yT])
            for ti, (t0, n) in enumerate(tiles):
                at = atile[ti % 2]
                kb.dma("sp", lambda h: h.dma_start(out=at[:, :, 0:n + 2], in_=aTf.ap()[b].rearrange("(k p) t -> p k t", p=128)[:, :, t0:t0 + n + 2]),
                       reads=[aTf], writes=[at], sembuf=at)
                for cbk in range(4):
                    p = ps[4 + cbk % 2] if part != 0 else ps[2 + cbk % 2]
                    for k in range(32):
                        kb.mm(p, p[:, 0:n + 2], wp, wp[:, k, cbk * 128:(cbk + 1) * 128], at, at[:, k, 0:n + 2], k == 0, k == 31)
                    blk = part * 4 + cbk
                    c1 = Kb[0]; c2 = Kb[1]
                    kb.op("act", lambda h: h.activation(c1[:, 0:n], p[:, 1:n + 1], AF.Identity, bias=cwb[:, blk, 3:4], scale=cwb[:, blk, 1:2]), reads=[p, cwb], writes=[c1])
                    kb.op("dve", lambda h: h.scalar_tensor_tensor(c1[:, 0:n], p[:, 0:n], cwb[:, blk, 0:1], c1[:, 0:n], ALU.mult, ALU.add), reads=[p, cwb, c1], writes=[c1])
                    zsl = zT[:, cbk, t0:t0 + n]
                    if part == 1:
                        kb.op("dve", lambda h: h.scalar_tensor_tensor(zsl, p[:, 2:n + 2], cwb[:, blk, 2:3], c1[:, 0:n], ALU.mult, ALU.add), reads=[p, cwb, c1], writes=[zT])
                    elif part == 2:
                        kb.op("dve", lambda h: h.scalar_tensor_tensor(c1[:, 0:n], p[:, 2:n + 2], cwb[:, blk, 2:3], c1[:, 0:n], ALU.mult, ALU.add), reads=[p, cwb, c1], writes=[c1])
                        kb.op("dve", lambda h: h.tensor_tensor(zsl, zsl, c1[:, 0:n], ALU.mult), reads=[zT, c1], writes=[zT])
                    else:
                        kb.op("dve", lambda h: h.scalar_tensor_tensor(c1[:, 0:n], p[:, 2:n + 2], cwb[:, blk, 2:3], c1[:, 0:n], ALU.mult, ALU.add), reads=[p, cwb, c1], writes=[c1])
                        kb.op("dve", lambda h: h.tensor_scalar(c2[:, 0:n], zsl, hbb[:, cbk:cbk + 1], None, ALU.mult), reads=[zT, hbb], writes=[c2])
                        kb.op("dve", lambda h: h.scalar_tensor_tensor(c2[:, 0:n], yT[:, cbk, t0:t0 + n], rn[:, cbk:cbk + 1], c2[:, 0:n], ALU.mult, ALU.add), reads=[yT, rn, c2], writes=[c2])
                        fo = Yh if False else None
                        kb.op("dve", lambda h: h.tensor_tensor(Bp[:, 0:n], c2[:, 0:n], c1[:, 0:n], ALU.mult), reads=[c1, c2], writes=[Bp])
                        kb.store(find, find.ap()[cbk * 128:(cbk + 1) * 128, b, t0:t0 + n], Bp, Bp[:, 0:n])
    return kb.finish()


def launch_E(inp, aT1_all):
    nc = build_E()
    W256cat, TW, M32, Vcat, T2, U = hy_consts()
    aTf = np.zeros((2, 4096, 4098), NPBF)
    aTf[:, :, 1:4097] = aT1_all
    Lm = 4096
    pos = np.arange(Lm, dtype=np.float32)
    def feats(posv):
        t01 = posv / (Lm - 1)
        bands = np.linspace(1e-4, 15, 16, dtype=np.float32)
        ang = (2.0 * math.pi / Lm) * posv[:, None] * bands[None, :]
        return np.concatenate([t01[:, None], np.cos(ang), -np.sin(ang)], -1).astype(np.float32)
    jrev = (4096 - np.arange(Lm)).astype(np.float32); jrev[0] = 0.0
    feat = np.ascontiguousarray(np.stack([feats(pos).T, feats(jrev).T], 0))
    t01 = np.zeros((128, 2, 32), np.float32)
    ii = (32 * np.arange(128)[:, None] + np.arange(32)[None, :]).astype(np.float32)
    t01[:, 0, :] = -ii / (Lm - 1)
    t01[:, 1, :] = -(4096 - ii) / (Lm - 1)
    deltas = np.abs(np.linspace(math.log(1e-2) / 1.5, math.log(1e-2) / 0.3, 4096, dtype=np.float32))
    m0 = np.ones((128, 1), np.float32); m0[0, 0] = 0.0
    mlp = np.zeros((64, 200), np.float32)
    mlp[:33, 0:64] = inp["hyena_filt_w1"][0]; mlp[:, 64:128] = inp["hyena_filt_w2"][0]
    mlp[:, 128] = inp["hyena_filt_b1"][0]; mlp[:, 129] = inp["hyena_filt_b2"][0]
    mlp[:, 130] = inp["hyena_filt_freq"][0][0]; mlp[:, 131] = inp["hyena_filt_freq"][0][1]
    ident = np.eye(128, dtype=np.float32).astype(NPBF)
    w_in = inp["hyena_w_in"][0]; cwt = np.asarray(inp["hyena_conv_w"][0], np.float32); cbt = np.asarray(inp["hyena_conv_b"][0], np.float32)
    w3full = np.asarray(inp["hyena_filt_w3"][0], np.float32); hbias = np.asarray(inp["hyena_bias"][0], np.float32)
    maps = []
    for c in range(NCORES):
        C0 = 512 * c
        cols = np.concatenate([np.arange(part * 4096 + C0, part * 4096 + C0 + 512) for part in range(3)])
        cw = np.zeros((128, 12, 4), np.float32)
        for blk in range(12):
            cc = cols[blk * 128:(blk + 1) * 128]
            cw[:, blk, 0:3] = cwt[:, cc].T
            cw[:, blk, 3] = cbt[cc]
        maps.append({"aTf": aTf, "win": np.ascontiguousarray(w_in[:, cols]), "cw": cw,
                     "hb": np.ascontiguousarray(hbias[C0:C0 + 512].reshape(4, 128).T), "feat": feat, "mlp": mlp,
                     "w3": np.ascontiguousarray(np.stack([w3full[:, C0:C0 + 512], w3full[:, 4096 + C0:4096 + C0 + 512]], 1)),
                     "t01": t01, "delt": np.ascontiguousarray(np.tile(deltas[None, C0:C0 + 512], (128, 1))), "m0": m0,
                     "c_w256": W256cat, "c_tw": TW, "c_m32": M32, "c_vcat": Vcat, "c_t2": T2, "c_u": U, "c_id": ident})
    r = run(nc, maps)
    fin = np.concatenate([r[c]["finT"] for c in range(NCORES)], 0)
    return np.ascontiguousarray(fin.transpose(1, 0, 2))


def kernel(**inp):
    inp = {k: np.asarray(v) for k, v in inp.items()}
    mods = launch_A(inp)
    rB = launch_B(inp, mods)
    h1 = gather_rows(rB, "h1"); bl = gather_rows(rB, "bl")
    affT = np.concatenate([np.concatenate([rB[b * 4 + q]["affT"] for q in range(4)], 1) for b in range(2)], 0)
    ys_all, slotm = launch_C(inp, 0, np.ascontiguousarray(affT), np.ascontiguousarray(bl.reshape(8192, D)))
    rD = launch_D(inp, False, h1, ys_all, slotm, mods, 0)
    h2 = gather_rows(rD, "h2")
    aT1 = np.stack([np.concatenate([rD[b * 4 + q]["aT1"] for q in range(4)], 1) for b in range(2)], 0)
    finT = launch_E(inp, aT1)
    rF = launch_F(inp, mods, h2, finT)
    h3 = gather_rows(rF, "h1"); bl2 = gather_rows(rF, "bl")
    affT2 = np.concatenate([np.concatenate([rF[b * 4 + q]["affT"] for q in range(4)], 1) for b in range(2)], 0)
    ys2, slotm2 = launch_C(inp, 1, np.ascontiguousarray(affT2), np.ascontiguousarray(bl2.reshape(8192, D)))
    rH = launch_D(inp, True, h3, ys2, slotm2, mods, 1)
    return gather_rows(rH, "out").astype(np.float32)
```

```python
import math
import numpy as np
from contextlib import ExitStack
import ml_dtypes
import concourse.bass as bass
import concourse.mybir as mybir
from concourse.bass_utils import run_bass_kernel_spmd

F32 = mybir.dt.float32; BF16 = mybir.dt.bfloat16; I32 = mybir.dt.int32; U32 = mybir.dt.uint32
ALU = mybir.AluOpType; AF = mybir.ActivationFunctionType; AX = mybir.AxisListType
NPBF = ml_dtypes.bfloat16
NCORES = 8
D = 4096; L = 4096; B = 2


class Buf:
    def __init__(self, t, name):
        self.t = t; self.name = name; self.w = {}; self.r = {}; self.dsem = None; self.dcnt = 0

    def __getitem__(self, k):
        return self.t[k]

    def ap(self):
        return self.t.ap()


class Eng:
    def __init__(self, h, sem, name):
        self.h = h; self.sem = sem; self.cnt = 0; self.waited = {}; self.name = name


class KB:
    def __init__(self):
        self.nc = bass.Bass("TRN2", target_bir_lowering=False)
        self.es = ExitStack()
        nc = self.nc
        self.E = {}
        for nm, h in (("pe", nc.tensor), ("act", nc.scalar), ("dve", nc.vector), ("pool", nc.gpsimd), ("sp", nc.sync)):
            self.E[nm] = Eng(h, self.es.enter_context(nc.semaphore("s_" + nm)), nm)
        self.bufs = []
        self.same_engine_sync = True

    def _reg(self, b):
        self.bufs.append(b); return b

    def dram(self, name, shape, dtype, kind="Internal"):
        return self._reg(Buf(self.nc.dram_tensor(name, list(shape), dtype, kind=kind), name))

    def sbuf(self, name, shape, dtype):
        return self._reg(Buf(self.es.enter_context(self.nc.sbuf_tensor(name, list(shape), dtype)), name))

    def psum(self, name, shape, dtype=F32):
        b = self._reg(Buf(self.es.enter_context(self.nc.psum_tensor(name, list(shape), dtype)), name))
        b.psum = True
        return b

    def _waits(self, e, reads, writes):
        deps = {}
        for b in reads:
            for s, v in b.w.items(): deps[s] = max(deps.get(s, 0), v)
        for b in writes:
            for s, v in b.w.items(): deps[s] = max(deps.get(s, 0), v)
            for s, v in b.r.items(): deps[s] = max(deps.get(s, 0), v)
        for s, v in deps.items():
            if s is e.sem and (e.name == "pe" or not self.same_engine_sync):
                continue
            if e.waited.get(s, 0) < v:
                for o in self.E.values():
                    if o.sem is s:
                        assert v <= o.cnt, f"wait on not-yet-emitted signal: {e.name} waits {o.name}>={v} (cnt {o.cnt})"
                e.h.wait_ge(s, v); e.waited[s] = v

    def op(self, eng, fn, reads=(), writes=(), sig=True):
        e = self.E[eng]
        writes = list(writes) + [b for b in reads if getattr(b, "psum", False)]
        reads = [b for b in reads if not getattr(b, "psum", False)]
        self._waits(e, reads, writes)
        ins = fn(e.h)
        if sig:
            e.cnt += 1
            ins.then_inc(e.sem, 1)
        tok = e.cnt if sig else e.cnt + 1
        for b in writes: b.w[e.sem] = max(b.w.get(e.sem, 0), tok)
        for b in reads: b.r[e.sem] = max(b.r.get(e.sem, 0), tok)
        return ins

    def dma(self, eng, fn, reads=(), writes=(), sembuf=None):
        e = self.E[eng]
        self._waits(e, reads, writes)
        sb = sembuf if sembuf is not None else (writes[0] if writes else reads[0])
        if sb.dsem is None:
            sb.dsem = self.es.enter_context(self.nc.semaphore("d_" + sb.name))
        ins = fn(e.h)
        sb.dcnt += 16
        ins.then_inc(sb.dsem, 16)
        for b in writes: b.w[sb.dsem] = sb.dcnt
        for b in reads: b.r[sb.dsem] = sb.dcnt
        return ins

    def load(self, dst, dst_ap, src, src_ap, eng="sp"):
        return self.dma(eng, lambda h: h.dma_start(out=dst_ap, in_=src_ap), reads=[src], writes=[dst], sembuf=dst)

    def store(self, dst, dst_ap, src, src_ap, eng="sp"):
        return self.dma(eng, lambda h: h.dma_start(out=dst_ap, in_=src_ap), reads=[src], writes=[dst], sembuf=src)

    def mm(self, out, out_ap, lhs, lhs_ap, rhs, rhs_ap, start, stop, extra_reads=(), sig=None):
        return self.op("pe", lambda h: h.matmul(out_ap, lhs_ap, rhs_ap, start=start, stop=stop),
                       reads=[lhs, rhs] + list(extra_reads), writes=[out], sig=(stop if sig is None else sig))

    def finish(self):
        e = self.E["sp"]
        for b in self.bufs:
            if b.dsem is not None and e.waited.get(b.dsem, 0) < b.dcnt:
                e.h.wait_ge(b.dsem, b.dcnt); e.waited[b.dsem] = b.dcnt
        for nm, o in self.E.items():
            if o.cnt > 0 and e.waited.get(o.sem, 0) < o.cnt:
                e.h.wait_ge(o.sem, o.cnt)
        self.es.close()
        return self.nc


def run(nc, in_maps):
    res = run_bass_kernel_spmd(nc, in_maps, core_ids=list(range(NCORES)))
    return res.results


def build_A():
    kb = KB()
    w = kb.dram("ada_w", [2, 4096, 3072], F32, "ExternalInput")
    bia = kb.dram("ada_b", [2, 3072], F32, "ExternalInput")
    cv = kb.dram("cv", [128, 32, 3], F32, "ExternalInput")
    out = kb.dram("mods", [2, 3, 3072], F32, "ExternalOutput")
    s = kb.sbuf("s", [128, 32, 3], F32)
    bt = kb.sbuf("bt", [3, 2, 3072], F32)
    res = kb.sbuf("res", [3, 2, 3072], F32)
    wt = [kb.sbuf(f"wt{i}", [128, 32, 512], F32) for i in range(2)]
    ps = [kb.psum(f"ps{i}", [128, 512], F32) for i in range(2)]
    kb.load(s, s[:], cv, cv.ap())
    kb.load(bt, bt[:], bia, bia.ap().partition_broadcast(3))
    kb.op("act", lambda h: h.activation(s[:], s[:], AF.Silu), reads=[s], writes=[s])
    it = 0
    for l in range(2):
        wv = w.ap()[l].rearrange("(k p) n -> p k n", p=128)
        for cb in range(6):
            t = wt[it % 2]; p = ps[it % 2]
            kb.load(t, t[:], w, wv[:, :, cb * 512:(cb + 1) * 512], eng=("sp" if it % 2 == 0 else "act"))
            for k in range(32):
                kb.mm(p, p[0:3, :], s, s[:, k, :], t, t[:, k, :], k == 0, k == 31)
            kb.op("dve", lambda h: h.tensor_tensor(res[:, l, cb * 512:(cb + 1) * 512], p[0:3, :], bt[:, l, cb * 512:(cb + 1) * 512], ALU.add),
                  reads=[p, bt], writes=[res])
            it += 1
    kb.store(out, out.ap().rearrange("l r n -> r l n"), res, res[:])
    return kb.finish()


def launch_A(inp):
    nc = build_A()
    cvec = np.concatenate([inp["c"], inp["c_ctx"][None]], 0).astype(np.float32)
    cv = np.ascontiguousarray(cvec.T.reshape(32, 128, 3).transpose(1, 0, 2))
    maps = []
    for c in range(NCORES):
        maps.append({"ada_w": np.ascontiguousarray(inp["ada_w"][:, :, c * 3072:(c + 1) * 3072]),
                     "ada_b": np.ascontiguousarray(inp["ada_b"][:, c * 3072:(c + 1) * 3072]),
                     "cv": cv})
    r = run(nc, maps)
    mods = np.concatenate([r[c]["mods"] for c in range(NCORES)], axis=2)
    return mods.reshape(2, 3, 6, D)


class Arena:
    def __init__(self, kb, name, nbytes):
        self.kb = kb
        self.t = kb.es.enter_context(kb.nc.sbuf_tensor(name, [128, nbytes // 2], BF16))
        self.nbytes = nbytes

    def view(self, name, off, shape, dtype, prev=()):
        n = 1
        for s in shape[1:]: n *= s
        esz = 2 if dtype == BF16 else 4
        assert off % 4 == 0 and off + n * esz <= self.nbytes, (name, off, n * esz, self.nbytes)
        ap = self.t[0:shape[0], off // 2: off // 2 + n * esz // 2]
        if esz == 4:
            ap = ap.bitcast(dtype)
        if len(shape) == 3:
            ap = ap.rearrange("p (a b) -> p a b", a=shape[1])
        if len(shape) == 4:
            ap = ap.rearrange("p (a b c) -> p a b c", a=shape[1], b=shape[2])
        b = Buf(ap, name)
        for o in prev:
            for s, v in list(o.w.items()) + list(o.r.items()):
                b.w[s] = max(b.w.get(s, 0), v)
        self.kb.bufs.append(b)
        return b


def psum_banks(kb):
    banks = []
    for i in range(8):
        b = kb.psum(f"ps{i}", [128, 512], F32)
        b.bf = b.t[:].bitcast(BF16)
        banks.append(b)
    return banks


IN = "ExternalInput"; OUT = "ExternalOutput"


def rms_rstd(kb, ss_ap, ss_buf, eps_buf, n):
    kb.op("act", lambda h: h.activation(ss_ap, ss_ap, AF.Sqrt, bias=eps_buf[:, 0:1], scale=1.0 / n), reads=[ss_buf, eps_buf], writes=[ss_buf])
    kb.op("dve", lambda h: h.reciprocal(ss_ap, ss_ap), reads=[ss_buf], writes=[ss_buf])


def build_B(stop=None):
    kb = KB()
    xo = kb.dram("xo", [1024, 4096], F32, IN)
    xe = kb.dram("xe", [512, 4096], F32, IN)
    gs = kb.dram("gs", [128, 5, 32], F32, IN)
    rowv = kb.dram("rowv", [4, 4096], F32, IN)
    w_in = kb.dram("w_in", [4096, 7168], F32, IN)
    w_out = kb.dram("w_out", [4096, 4096], F32, IN)
    ropec = kb.dram("ropec", [128, 1536], F32, IN)
    ropes = kb.dram("ropes", [128, 1536], F32, IN)
    cbf = kb.dram("cbf", [128, 3, 128], BF16, IN)
    masks = kb.dram("masks", [128, 4, 512], BF16, IN)
    identf = kb.dram("identf", [128, 128], F32, IN)
    sink = kb.dram("sink", [1, 16], F32, IN)
    sgu_ws = kb.dram("sgu_ws", [16, 128, 128], F32, IN)
    sgu_bs = kb.dram("sgu_bs", [1, 2048], F32, IN)
    sgu_g = kb.dram("sgu_g", [1, 2048], F32, IN)
    wr = kb.dram("wr", [128, 32, 16], F32, IN)
    h1 = kb.dram("h1", [1024, 4096], F32, OUT)
    bl = kb.dram("bl", [1024, 4096], BF16, OUT)
    affT = kb.dram("affT", [16, 1024], F32, OUT)
    catT = kb.dram("catT", [4096, 1024], BF16, OUT)
    d = dict(locals())
    mixer0_body(kb, d)
    if d.get('_stopped'):
        return kb.finish()
    ffn_front(kb, d['_ar'], d['_ps'], d['_dead'], d['_epsb'], xo, catT, w_out, rowv, wr, identf, h1, bl, affT)
    return kb.finish()


def rope_evac(kb, ps, rot_ps, cosb, cos_ap, sinb, sin_ap, rt, tmpb, t1, t2, outb, out_ap):
    kb.op("act", lambda h: h.activation(tmpb[:], ps[:], AF.Copy), reads=[ps], writes=[tmpb])
    kb.mm(rot_ps, rot_ps[:], rt, rt[:], tmpb, tmpb[:], True, True)
    kb.op("dve", lambda h: h.tensor_tensor(t1[:], ps[:], cos_ap, ALU.mult), reads=[ps, cosb], writes=[t1])
    kb.op("dve", lambda h: h.tensor_tensor(t2[:], rot_ps[:], sin_ap, ALU.mult), reads=[rot_ps, sinb], writes=[t2])
    kb.op("dve", lambda h: h.tensor_tensor(out_ap, t1[:], t2[:], ALU.add), reads=[t1, t2], writes=[outb])


def mixer0_body(kb, d):
    xo, xe, gs, w_in, ropec, ropes, cbf, masks, sink = d["xo"], d["xe"], d["gs"], d["w_in"], d["ropec"], d["ropes"], d["cbf"], d["masks"], d["sink"]
    sgu_ws, sgu_bs, sgu_g, catT, identf = d["sgu_ws"], d["sgu_bs"], d["sgu_g"], d["catT"], d["identf"]
    ps = psum_banks(kb)
    ar = Arena(kb, "arenaB", 204 * 1024)
    KBY = 1024
    off = 184 * KBY
    gsb = ar.view("gsb", off, [128, 5, 32], F32); off += 640
    GS = ar.view("GS", off, [128, 4, 32], F32); off += 512
    cb = ar.view("cb", off, [128, 3, 128], BF16); off += 768
    epsb = ar.view("epsb", off, [128, 1], F32); off += 4
    ssb = ar.view("ssb", off, [128, 16], F32); off += 64
    mk = ar.view("mk", off, [128, 4, 512], BF16); off += 4096
    esk = ar.view("esk", off, [128, 16], F32); off += 64
    eskhl = ar.view("eskhl", off, [2, 16, 128], BF16); off += 4096
    eskf = ar.view("eskf", off, [1, 16, 128], F32); off += 8192
    assert off <= 204 * KBY
    ident = Buf(cb.t[:, 0, :], "ident"); rt = Buf(cb.t[:, 1, :], "rt"); ones = Buf(cb.t[:, 2, :], "ones")
    for b_ in (ident, rt, ones): b_.w = cb.w; b_.r = cb.r
    kb.load(gsb, gsb[:], gs, gs.ap())
    kb.load(cb, cb[:], cbf, cbf.ap())
    kb.load(mk, mk[:], masks, masks.ap())
    kb.op("dve", lambda h: h.memset(epsb[:], 1e-6), writes=[epsb])
    kb.op("dve", lambda h: h.scalar_tensor_tensor(GS[:, 0, :], gsb[:, 1, :], 1.0, gsb[:, 0, :], ALU.add, ALU.mult), reads=[gsb], writes=[GS])
    kb.op("dve", lambda h: h.tensor_copy(GS[:, 1, :], gsb[:, 2, :]), reads=[gsb], writes=[GS])
    kb.op("dve", lambda h: h.scalar_tensor_tensor(GS[:, 2, :], gsb[:, 3, :], 1.0, gsb[:, 0, :], ALU.add, ALU.mult), reads=[gsb], writes=[GS])
    kb.op("dve", lambda h: h.tensor_copy(GS[:, 3, :], gsb[:, 4, :]), reads=[gsb], writes=[GS])
    kb.load(esk, esk[0:1, :], sink, sink.ap())
    kb.op("act", lambda h: h.activation(esk[0:1, :], esk[0:1, :], AF.Exp), reads=[esk], writes=[esk])
    kb.op("dve", lambda h: h.tensor_copy(eskf[0:1, :, :], esk[0:1, :].unsqueeze(2).to_broadcast([1, 16, 128])), reads=[esk], writes=[eskf])
    kb.op("dve", lambda h: h.tensor_copy(eskhl[0:1, :, :], eskf[0:1, :, :]), reads=[eskf], writes=[eskhl])
    kb.op("dve", lambda h: h.tensor_tensor(eskf[0:1, :, :], eskf[0:1, :, :], eskhl[0:1, :, :], ALU.subtract), reads=[eskf, eskhl], writes=[eskf])
    lo_tmp = ar.view("lo_tmp", 180 * KBY, [1, 16, 128], BF16)
    kb.op("dve", lambda h: h.tensor_copy(lo_tmp[0:1, :, :], eskf[0:1, :, :]), reads=[eskf], writes=[lo_tmp])
    kb.dma("sp", lambda h: h.dma_start(out=eskhl[1:2, :, :], in_=lo_tmp[0:1, :, :]), reads=[lo_tmp], writes=[eskhl], sembuf=eskhl)

    if d.get('stop') == 'C':
        d['_stopped'] = True
        return
    aT = ar.view("aT", 0, [128, 32, 1024], BF16)
    aTx = ar.view("aTx", 64 * KBY, [128, 32, 512], BF16)
    wA = ar.view("wA", 96 * KBY, [128, 32, 512], BF16)
    xt = [ar.view("xt0", 128 * KBY, [128, 4096], F32), ar.view("xt1", 144 * KBY, [128, 4096], F32)]
    xn = ar.view("xn", 160 * KBY, [128, 4096], BF16)

    def norm_tile(i, src, row0, dst, col0, gi):
        x_ = xt[i % 2]
        kb.load(x_, x_[:], src, src.ap()[row0:row0 + 128, :])
        kb.op("act", lambda h: h.activation(xn[:], x_[:], AF.Square, accum_out=ssb[:, 0:1]), reads=[x_], writes=[xn, ssb])
        rms_rstd(kb, ssb[:, 0:1], ssb, epsb, 4096)
        kb.op("act", lambda h: h.activation(xn[:], x_[:], AF.Copy, scale=ssb[:, 0:1]), reads=[x_, ssb], writes=[xn])
        for bk in range(4):
            p = ps[bk]
            for j in range(8):
                k = bk * 8 + j
                kb.op("pe", lambda h: h.transpose(p.bf[:, j * 128:(j + 1) * 128], xn[:, k * 128:(k + 1) * 128], ident[:]),
                      reads=[xn, ident], writes=[p], sig=(j == 7))
            for j in range(8):
                k = bk * 8 + j
                o_ap = dst[:, k, col0:col0 + 128]
                i_ap = p.bf[:, j * 128:(j + 1) * 128]
                if bk % 2 == 0:
                    kb.op("act", lambda h: h.activation(o_ap, i_ap, AF.Identity, bias=GS[:, gi + 1, k:k + 1], scale=GS[:, gi, k:k + 1]),
                          reads=[p, GS], writes=[dst])
                else:
                    kb.op("dve", lambda h: h.tensor_scalar(o_ap, i_ap, GS[:, gi, k:k + 1], GS[:, gi + 1, k:k + 1], ALU.mult, ALU.add),
                          reads=[p, GS], writes=[dst])
    for i in range(4):
        norm_tile(i, xe, i * 128, aTx, i * 128, 0 if i < 2 else 2)
    for i in range(8):
        norm_tile(i, xo, i * 128, aT, i * 128, 0)

    if d.get('stop') == 'B1':
        d['_stopped'] = True
        return
    r6 = 128 * KBY
    cosb = ar.view("cosb", r6, [128, 1536], F32, prev=xt + [xn]); sinb = ar.view("sinb", r6 + 6 * KBY, [128, 1536], F32, prev=xt + [xn])
    kT = ar.view("kT", r6 + 12 * KBY, [128, 4, 1536], BF16, prev=xt + [xn])
    V = ar.view("V", r6 + 24 * KBY, [128, 12, 512], BF16, prev=xt + [xn])
    tmpb = ar.view("tmpb", 164 * KBY, [128, 512], BF16, prev=[xn])
    t1 = ar.view("t1", 165 * KBY, [128, 512], F32, prev=[xn]); t2 = ar.view("t2", 167 * KBY, [128, 512], F32, prev=[xn])
    kb.load(cosb, cosb[:], ropec, ropec.ap())
    kb.load(sinb, sinb[:], ropes, ropes.ap())
    wv_in = w_in.ap().rearrange("(k p) n -> p k n", p=128)

    def wload(t, c0, eng="pool"):
        kb.dma(eng, lambda h: h.dma_start(out=t[:], in_=wv_in[:, :, c0:c0 + 512]), reads=[w_in], writes=[t], sembuf=t)

    def tok_rhs(tt):
        return (aT, lambda k: aT[:, k, tt * 512:(tt + 1) * 512]) if tt < 2 else (aTx, lambda k: aTx[:, k, :])

    def tok_lhs(t128):
        return (aT, lambda k: aT[:, k, t128 * 128:(t128 + 1) * 128]) if t128 < 8 else (aTx, lambda k: aTx[:, k, (t128 - 8) * 128:(t128 - 7) * 128])

    wload(wA, 2048)
    it = 0
    for hk in range(4):
        for tt in range(3):
            p = ps[4 + it % 2]; it += 1
            ab, af = tok_rhs(tt)
            for k in range(32):
                kb.mm(p, p[:], wA, wA[:, k, hk * 128:(hk + 1) * 128], ab, af(k), k == 0, k == 31)
            if d.get('stop') == 'B15a':
                d['_stopped'] = True
                return
            rope_evac(kb, p, ps[6], cosb, cosb[:, tt * 512:(tt + 1) * 512], sinb, sinb[:, tt * 512:(tt + 1) * 512], rt, tmpb, t1, t2,
                      kT, kT[:, hk, tt * 512:(tt + 1) * 512])
            if d.get('stop') == 'B15b':
                d['_stopped'] = True
                return
    if d.get('stop') == 'B15c':
        d['_stopped'] = True
        return
    wload(wA, 2560)
    for t128 in range(12):
        p = ps[4 + t128 % 2]
        ab, af = tok_lhs(t128)
        for k in range(32):
            kb.mm(p, p[:], ab, af(k), wA, wA[:, k, :], k == 0, k == 31)
        kb.op("act", lambda h: h.activation(V[:, t128, :], p[:], AF.Copy), reads=[p], writes=[V])

    if d.get('stop') == 'B15':
        d['_stopped'] = True
        return
    wB = ar.view("wB", 64 * KBY, [128, 32, 512], BF16, prev=[aTx])
    wbufs = [wA, wB]
    qT = ar.view("qT", 169 * KBY, [128, 4, 1024], BF16)
    Pt = [ar.view(f"P{c}", 177 * KBY + c * 1024, [128, 512], BF16, prev=[lo_tmp]) for c in range(5)]
    Oh = ar.view("Oh", 172 * KBY + 10 * KBY, [128, 4, 1024], BF16) if False else None
    Oh = [ar.view(f"Oh{g}", 120 * KBY + g * 2 * KBY, [128, 1024], BF16) for g in range(4)] if False else None
    wi = 0
    nxt = wbufs[wi % 2]; wload(nxt, 0)
    ohst = ar.view("ohst", 182 * KBY, [128, 4, 128], BF16, prev=[lo_tmp])
    for hk in range(4):
        wq = wbufs[wi % 2]; wi += 1
        if hk < 3:
            wload(wbufs[wi % 2], (hk + 1) * 512)
        for g in range(4):
            for tt in range(2):
                p = ps[4 + (g * 2 + tt) % 2]
                for k in range(32):
                    kb.mm(p, p[:], wq, wq[:, k, g * 128:(g + 1) * 128], aT, aT[:, k, tt * 512:(tt + 1) * 512], k == 0, k == 31)
                rope_evac(kb, p, ps[6], cosb, cosb[:, tt * 512:(tt + 1) * 512], sinb, sinb[:, tt * 512:(tt + 1) * 512], rt, tmpb, t1, t2,
                          qT, qT[:, g, tt * 512:(tt + 1) * 512])
        for j in range(8):
            prev_c = ((j - 1) * 128, j - 1, 0) if j >= 1 else (1024, 8, 2)
            next_c = ((j + 1) * 128, j + 1, 1) if j <= 6 else (1152, 9, 3)
            chunks = [prev_c, (j * 128, j, None), next_c, (1280, 10, None), (1408, 11, None)]
            q_ap = qT[:, :, j * 128:(j + 1) * 128]
            for c, (ko, vt, mi) in enumerate(chunks):
                sp_ = ps[c % 4]
                kb.mm(sp_, sp_[:], kT, kT[:, hk, ko:ko + 128], qT, q_ap, True, True)
                kb.op("act", lambda h: h.activation(Pt[c][:], sp_[:], AF.Exp, scale=128 ** -0.5), reads=[sp_], writes=[Pt[c]])
                if mi is not None:
                    kb.op("dve", lambda h: h.tensor_tensor(Pt[c][:], Pt[c][:], mk[:, mi, :], ALU.mult), reads=[Pt[c], mk], writes=[Pt[c]])
            o_ps = ps[4 + j % 2]; d_ps = ps[6 + j % 2]
            for c, (ko, vt, mi) in enumerate(chunks):
                kb.mm(o_ps, o_ps[:], V, V[:, vt, hk * 128:(hk + 1) * 128], Pt[c], Pt[c][:], c == 0, c == 4)
            for c in range(5):
                kb.mm(d_ps, d_ps[:], ones, ones[:], Pt[c], Pt[c][:], c == 0, False)
            kb.mm(d_ps, d_ps[:], ones, ones[0:2, :], eskhl, eskhl[0:2, hk * 4:(hk + 1) * 4, :], False, True)
            kb.op("dve", lambda h: h.reciprocal(t1[:], d_ps[:]), reads=[d_ps], writes=[t1])
            kb.op("dve", lambda h: h.tensor_tensor(ohst[:], o_ps[:], t1[:], ALU.mult), reads=[o_ps, t1], writes=[ohst])
            kb.store(catT, catT.ap()[hk * 512:(hk + 1) * 512, j * 128:(j + 1) * 128].rearrange("(g d) t -> d g t", g=4), ohst, ohst[:])

    if d.get('stop') == 'B2':
        d['_stopped'] = True
        return
    r6v = [cosb, sinb, kT, V, qT, tmpb, t1, t2] + Pt
    zg = ar.view("zg", r6, [128, 512], F32, prev=r6v); sq = ar.view("sq", r6 + 2 * KBY, [128, 512], F32, prev=r6v)
    zn = ar.view("zn", r6 + 4 * KBY, [128, 8, 512], BF16, prev=r6v)
    uT = ar.view("uT", r6 + 12 * KBY, [128, 4, 1024], BF16, prev=r6v)
    wsT = ar.view("wsT", r6 + 20 * KBY, [128, 16, 128], BF16, prev=r6v)
    bsb = ar.view("bsb", r6 + 24 * KBY, [128, 2048], F32, prev=r6v)
    sgb = ar.view("sgb", r6 + 32 * KBY, [128, 2048], F32, prev=r6v)
    wsf = ar.view("wsf", r6 + 40 * KBY, [128, 128], F32, prev=r6v)
    st4 = ar.view("st4", r6 + 41 * KBY, [128, 8], F32, prev=r6v)
    Sst = ar.view("Sst", r6 + 42 * KBY, [128, 512], BF16, prev=r6v)
    idf = ar.view("idf", r6 + 43 * KBY, [128, 128], F32, prev=r6v)
    kb.load(bsb, bsb[:], sgu_bs, sgu_bs.ap()[0, :].partition_broadcast(128))
    kb.load(sgb, sgb[:], sgu_g, sgu_g.ap()[0, :].partition_broadcast(128))
    kb.load(idf, idf[:], identf, identf.ap())
    for g in range(16):
        kb.load(wsf, wsf[:], sgu_ws, sgu_ws.ap()[g])
        p = ps[g % 2]
        kb.op("pe", lambda h: h.transpose(p[:, 0:128], wsf[:], idf[:]), reads=[wsf, idf], writes=[p])
        kb.op("act", lambda h: h.activation(wsT[:, g, :], p[:, 0:128], AF.Copy), reads=[p], writes=[wsT])
    for cbk in range(4):
        wz = wbufs[wi % 2]; wi += 1
        wu = wbufs[wi % 2]; wi += 1
        wload(wz, 5120 + cbk * 512)
        wload(wu, 3072 + cbk * 512)
        for tq in range(8):
            p = ps[tq % 2]
            for k in range(32):
                kb.mm(p, p[:], aT, aT[:, k, tq * 128:(tq + 1) * 128], wz, wz[:, k, :], k == 0, k == 31)
            kb.op("act", lambda h: h.activation(zg[:], p[:], AF.Gelu), reads=[p], writes=[zg])
            zg3 = zg[:].rearrange("p (g c) -> p g c", g=4)
            kb.op("dve", lambda h: h.tensor_reduce(st4[:, 0:4], zg3, AX.X, ALU.add), reads=[zg], writes=[st4])
            kb.op("pool", lambda h: h.tensor_tensor(sq[:], zg[:], zg[:], ALU.mult), reads=[zg], writes=[sq])
            kb.op("dve", lambda h: h.tensor_reduce(st4[:, 4:8], sq[:].rearrange("p (g c) -> p g c", g=4), AX.X, ALU.add), reads=[sq], writes=[st4])
            kb.op("dve", lambda h: h.tensor_scalar(st4[:, 0:4], st4[:, 0:4], 1.0 / 128, None, ALU.mult), reads=[st4], writes=[st4])
            kb.op("dve", lambda h: h.tensor_tensor(sq[:, 0:4], st4[:, 0:4], st4[:, 0:4], ALU.mult), reads=[st4], writes=[sq])
            kb.op("dve", lambda h: h.scalar_tensor_tensor(st4[:, 4:8], st4[:, 4:8], 1.0 / 128, sq[:, 0:4], ALU.mult, ALU.subtract), reads=[st4, sq], writes=[st4])
            kb.op("act", lambda h: h.activation(st4[:, 4:8], st4[:, 4:8], AF.Sqrt, bias=epsb[:, 0:1], scale=1.0), reads=[st4, epsb], writes=[st4])
            kb.op("dve", lambda h: h.reciprocal(st4[:, 4:8], st4[:, 4:8]), reads=[st4], writes=[st4])
            for g4 in range(4):
                kb.op("dve", lambda h: h.tensor_scalar(zg[:, g4 * 128:(g4 + 1) * 128], zg[:, g4 * 128:(g4 + 1) * 128], st4[:, g4:g4 + 1], st4[:, 4 + g4:5 + g4],
                                                        ALU.subtract, ALU.mult), reads=[zg, st4], writes=[zg])
            kb.op("pool", lambda h: h.tensor_tensor(zn[:, tq, :], zg[:], sgb[:, cbk * 512:(cbk + 1) * 512], ALU.mult), reads=[zg, sgb], writes=[zn])
        for g4 in range(4):
            for tt in range(2):
                p = ps[2 + (g4 * 2 + tt) % 2]
                for k in range(32):
                    kb.mm(p, p[:], wu, wu[:, k, g4 * 128:(g4 + 1) * 128], aT, aT[:, k, tt * 512:(tt + 1) * 512], k == 0, k == 31)
                kb.op("act", lambda h: h.activation(uT[:, g4, tt * 512:(tt + 1) * 512], p[:], AF.Gelu), reads=[p], writes=[uT])
        for g4 in range(4):
            g = cbk * 4 + g4
            for half in range(2):
                p = ps[4 + half]
                for ch in range(4):
                    tq = half * 4 + ch
                    kb.op("pe", lambda h: h.matmul(p[:, ch * 128:(ch + 1) * 128], zn[:, tq, g4 * 128:(g4 + 1) * 128], wsT[:, g, :], start=True, stop=True),
                          reads=[zn, wsT], writes=[p], sig=(ch == 3))
                p3 = p[:].rearrange("p (c q) -> p c q", c=4)
                kb.op("dve", lambda h: h.tensor_tensor(sq[:].rearrange("p (c q) -> p c q", c=4), p3,
                                                        bsb[:, g * 128:(g + 1) * 128].unsqueeze(1).to_broadcast([128, 4, 128]), ALU.add),
                      reads=[p, bsb], writes=[sq])
                kb.op("dve", lambda h: h.tensor_tensor(Sst[:], sq[:], uT[:, g4, half * 512:(half + 1) * 512], ALU.mult), reads=[sq, uT], writes=[Sst])
                kb.store(catT, catT.ap()[2048 + g * 128:2048 + (g + 1) * 128, half * 512:(half + 1) * 512], Sst, Sst[:])
    if d.get('stop') == 'B3':
        d['_stopped'] = True
        return
    d["_ar"] = ar; d["_ps"] = ps; d["_dead"] = [aT, aTx, wA, wB, zg, sq, zn, uT, wsT, bsb, sgb, wsf, st4, Sst, idf, xt[0], xt[1], xn, ohst, qT, kT, V, cosb, sinb, t1, t2, tmpb] + Pt
    d["_epsb"] = epsb


def ffn_front(kb, ar, ps, dead, epsb, xres, catT, w_out, rowv, wr, identf, h1, bl, affT):
    KBY = 1024
    cT = ar.view("cT", 0, [128, 32, 1024], BF16, prev=dead)
    wo = [ar.view("wo0", 64 * KBY, [128, 32, 512], BF16, prev=dead), ar.view("wo1", 96 * KBY, [128, 32, 512], BF16, prev=dead)]
    r6 = 128 * KBY
    m2b = ar.view("m2b", r6, [128, 4096], F32, prev=dead)
    xs = [ar.view(f"xs{i}", r6 + 16 * KBY + i * 2 * KBY, [128, 512], F32, prev=dead) for i in range(2)]
    hs = [ar.view(f"hs{i}", r6 + 20 * KBY + i * 2 * KBY, [128, 512], F32, prev=dead) for i in range(2)]
    ssq = ar.view("ssq", r6 + 24 * KBY, [128, 8, 8], F32, prev=dead)
    junk = ar.view("junk", r6 + 25 * KBY, [128, 512], BF16, prev=dead)
    ssum = ar.view("ssum", r6 + 26 * KBY, [128, 8], F32, prev=dead)
    ones16 = ar.view("ones16", r6 + 26 * KBY + 64, [16, 16], F32, prev=dead)
    ex = ar.view("ex", r6 + 27 * KBY, [16, 128], F32, prev=dead)
    affs = ar.view("affs", r6 + 28 * KBY, [16, 1024], F32, prev=dead)
    wrb = ar.view("wrb", r6 + 32 * KBY, [128, 32, 16], F32, prev=dead)
    idf = ar.view("idf2", r6 + 34 * KBY, [128, 128], F32, prev=dead)
    kb.load(cT, cT[:], catT, catT.ap().rearrange("(k p) t -> p k t", p=128))
    kb.load(m2b, m2b[:], rowv, rowv.ap()[0, :].partition_broadcast(128))
    kb.load(wrb, wrb[:], wr, wr.ap())
    kb.load(idf, idf[:], identf, identf.ap())
    kb.op("dve", lambda h: h.memset(ones16[:], 1.0), writes=[ones16])
    wov = w_out.ap().rearrange("(k p) n -> p k n", p=128)

    def wload(t, c0):
        kb.dma("pool", lambda h: h.dma_start(out=t[:], in_=wov[:, :, c0:c0 + 512]), reads=[w_out], writes=[t], sembuf=t)
    wload(wo[0], 0)
    it = 0
    for ct in range(8):
        w = wo[ct % 2]
        if ct < 7:
            wload(wo[(ct + 1) % 2], (ct + 1) * 512)
        for tq in range(8):
            p = ps[it % 4]; x_ = xs[it % 2]; h_ = hs[it % 2]; it += 1
            kb.load(x_, x_[:], xres, xres.ap()[tq * 128:(tq + 1) * 128, ct * 512:(ct + 1) * 512])
            for k in range(32):
                kb.mm(p, p[:], cT, cT[:, k, tq * 128:(tq + 1) * 128], w, w[:, k, :], k == 0, k == 31)
            kb.op("dve", lambda h: h.tensor_tensor(h_[:], p[:], m2b[:, ct * 512:(ct + 1) * 512], ALU.mult), reads=[p, m2b], writes=[h_])
            kb.op("dve", lambda h: h.tensor_tensor(h_[:], h_[:], x_[:], ALU.add), reads=[h_, x_], writes=[h_])
            kb.op("act", lambda h: h.activation(junk[:], h_[:], AF.Square, accum_out=ssq[:, tq, ct:ct + 1]), reads=[h_], writes=[junk, ssq])
            kb.store(h1, h1.ap()[tq * 128:(tq + 1) * 128, ct * 512:(ct + 1) * 512], h_, h_[:])
    kb.op("dve", lambda h: h.tensor_reduce(ssum[:], ssq[:], AX.X, ALU.add), reads=[ssq], writes=[ssum])
    rms_rstd(kb, ssum[:], ssum, epsb, 4096)
    G4 = ar.view("G4", 0, [128, 4096], F32, prev=[cT]); S3 = ar.view("S3", 16 * KBY, [128, 4096], F32, prev=[cT])
    gf = ar.view("gf", 32 * KBY, [128, 4096], F32, prev=[cT]); m4 = ar.view("m4", 48 * KBY, [128, 4096], F32, prev=[cT])
    kb.load(gf, gf[:], rowv, rowv.ap()[1, :].partition_broadcast(128))
    kb.load(m4, m4[:], rowv, rowv.ap()[2, :].partition_broadcast(128))
    kb.load(S3, S3[:], rowv, rowv.ap()[3, :].partition_broadcast(128))
    kb.op("dve", lambda h: h.scalar_tensor_tensor(G4[:], m4[:], 1.0, gf[:], ALU.add, ALU.mult), reads=[m4, gf], writes=[G4])
    ht = ar.view("ht", 64 * KBY, [128, 4096], F32, prev=wo); bt = ar.view("bt", 80 * KBY, [128, 4096], F32, prev=wo)
    bb = ar.view("bb", 96 * KBY, [128, 4096], BF16, prev=wo); bT = ar.view("bT", 104 * KBY, [128, 32, 128], F32, prev=wo)
    for tq in range(8):
        kb.load(ht, ht[:], h1, h1.ap()[tq * 128:(tq + 1) * 128, :])
        kb.op("act", lambda h: h.activation(bt[:], ht[:], AF.Copy, scale=ssum[:, tq:tq + 1]), reads=[ht, ssum], writes=[bt])
        kb.op("dve", lambda h: h.tensor_tensor(bt[:], bt[:], G4[:], ALU.mult), reads=[bt, G4], writes=[bt])
        kb.op("pool", lambda h: h.tensor_tensor(bt[:], bt[:], S3[:], ALU.add), reads=[bt, S3], writes=[bt])
        kb.op("act", lambda h: h.activation(bb[:], bt[:], AF.Copy), reads=[bt], writes=[bb])
        kb.store(bl, bl.ap()[tq * 128:(tq + 1) * 128, :], bb, bb[:])
        for bk in range(8):
            p = ps[bk]
            for j in range(4):
                k = bk * 4 + j
                kb.op("pe", lambda h: h.transpose(p[:, j * 128:(j + 1) * 128], bt[:, k * 128:(k + 1) * 128], idf[:]), reads=[bt, idf], writes=[p], sig=(j == 3))
            o_ap = bT[:, bk * 4:(bk + 1) * 4, :]
            if bk % 2 == 0:
                kb.op("act", lambda h: h.activation(o_ap, p[:].rearrange("p (a b) -> p a b", a=4), AF.Copy), reads=[p], writes=[bT])
            else:
                kb.op("dve", lambda h: h.tensor_copy(o_ap, p[:].rearrange("p (a b) -> p a b", a=4)), reads=[p], writes=[bT])
        lg = ps[tq % 2]
        for k in range(32):
            kb.mm(lg, lg[0:16, 0:128], wrb, wrb[:, k, :], bT, bT[:, k, :], k == 0, k == 31)
        kb.op("act", lambda h: h.activation(ex[:], lg[0:16, 0:128], AF.Exp), reads=[lg], writes=[ex])
        sm = ps[2 + tq % 2]
        kb.mm(sm, sm[0:16, 0:128], ones16, ones16[:], ex, ex[:], True, True)
        kb.op("dve", lambda h: h.reciprocal(affs[:, tq * 128:(tq + 1) * 128], sm[0:16, 0:128]), reads=[sm], writes=[affs])
        kb.op("dve", lambda h: h.tensor_tensor(affs[:, tq * 128:(tq + 1) * 128], affs[:, tq * 128:(tq + 1) * 128], ex[:], ALU.mult), reads=[ex, affs], writes=[affs])
    kb.store(affT, affT.ap(), affs, affs[:])


def fm(v):
    return np.ascontiguousarray(np.asarray(v, np.float32).reshape(32, 128).T)


def consts_B(q):
    ident = np.eye(128, dtype=np.float32)
    R = np.zeros((128, 128), np.float32)
    for half in (0, 64):
        for i in range(32):
            R[half + i, half + i + 32] = -1.0
            R[half + i + 32, half + i] = 1.0
    cbf = np.stack([ident, R.T, np.ones((128, 128), np.float32)], 1).astype(NPBF)
    kk = np.arange(128)[:, None]; qq = np.arange(128)[None, :]
    m0 = (kk >= qq).astype(np.float32); m1 = (kk <= qq).astype(np.float32)
    ms = np.stack([m0, m1, m0 * (1.0 if q > 0 else 0.0), m1 * (1.0 if q < 3 else 0.0)], 0)
    masks = np.ascontiguousarray(np.tile(ms[:, :, None, :], (1, 1, 4, 1)).reshape(4, 128, 512).transpose(1, 0, 2)).astype(NPBF)
    t0 = 1024 * q
    tok = np.concatenate([np.arange(t0, t0 + 1024), np.arange(t0 - 128, t0), np.arange(t0 + 1024, t0 + 1152)])
    row = (tok // 64).astype(np.float32); col = (tok % 64).astype(np.float32)
    inv = (10000.0 ** (-np.arange(0, 64, 2, dtype=np.float32) / 64)).astype(np.float32)
    ang = np.zeros((128, 1280), np.float32)
    for d in range(128):
        pos = row if d < 64 else col
        ang[d] = pos * inv[d % 32]
    cosT = np.concatenate([np.cos(ang), np.ones((128, 256), np.float32)], 1).astype(np.float32)
    sinT = np.concatenate([np.sin(ang), np.zeros((128, 256), np.float32)], 1).astype(np.float32)
    return cbf, masks, cosT, sinT, ident


def launch_B(inp, mods, stop=None):
    nc = build_B(stop)
    maps = []
    x = inp["x"]; ctx = inp["ctx"]
    w_in = np.ascontiguousarray(inp["attn_sgu_w_in"][0]); w_out = np.ascontiguousarray(inp["attn_sgu_w_out"][0])
    wr = np.ascontiguousarray(np.asarray(inp["router_w"][0], np.float32).reshape(32, 128, 16).transpose(1, 0, 2))
    for c in range(NCORES):
        b, q = c // 4, c % 4
        t0 = 1024 * q
        z = np.zeros((128, D), np.float32)
        hp = x[b, t0 - 128:t0] if q > 0 else z
        hn = x[b, t0 + 1024:t0 + 1152] if q < 3 else z
        cbf, masks, cosT, sinT, ident = consts_B(q)
        gs = np.stack([fm(inp["norm_mix_g"][0]), fm(mods[0, b, 1]), fm(mods[0, b, 0]), fm(mods[0, 2, 1]), fm(mods[0, 2, 0])], 1)
        rowv = np.stack([mods[0, b, 2], inp["norm_ffn_g"][0], mods[0, b, 4], mods[0, b, 3]], 0).astype(np.float32)
        maps.append({
            "xo": np.ascontiguousarray(x[b, t0:t0 + 1024]), "xe": np.ascontiguousarray(np.concatenate([hp, hn, ctx[b]], 0)),
            "gs": np.ascontiguousarray(gs), "rowv": np.ascontiguousarray(rowv), "w_in": w_in, "w_out": w_out,
            "ropec": cosT, "ropes": sinT, "cbf": cbf, "masks": masks, "identf": ident,
            "sink": np.asarray(inp["attn_sink"][0], np.float32).reshape(1, 16),
            "sgu_ws": np.ascontiguousarray(inp["sgu_w_s"][0]), "sgu_bs": np.asarray(inp["sgu_b_s"][0], np.float32).reshape(1, 2048),
            "sgu_g": np.asarray(inp["sgu_norm_g"][0], np.float32).reshape(1, 2048), "wr": wr})
    r = run(nc, maps)
    return r


def build_C():
    kb = KB()
    affd = kb.dram("aff", [32, 4096], F32, IN)
    bld = kb.dram("bl", [8192, 4096], BF16, IN)
    wg = kb.dram("wg", [2, 4096, 1024], F32, IN)
    wu = kb.dram("wu", [2, 4096, 1024], F32, IN)
    wd = kb.dram("wd", [2, 1024, 4096], F32, IN)
    seld = kb.dram("sel", [32, 4], F32, IN)
    iotad = kb.dram("iota", [128, 512], F32, IN)
    rowidd = kb.dram("rowid", [128, 64], F32, IN)
    identd = kb.dram("ident", [128, 128], BF16, IN)
    ysd = kb.dram("ys", [4, 513, 4096], BF16, OUT)
    slotd = kb.dram("slotm", [32, 4096], F32, OUT)
    ps = psum_banks(kb)
    ar = Arena(kb, "arenaC", 204 * 1024)
    KBY = 1024
    aff = ar.view("aff", 0, [32, 4096], F32)
    msk = ar.view("msk", 16 * KBY, [32, 4096], F32)
    cum = ar.view("cum", 32 * KBY, [32, 4096], F32)
    onesr = ar.view("onesr", 48 * KBY, [32, 4096], F32)
    sm = ar.view("sm", 64 * KBY, [32, 16], F32)
    sel = ar.view("sel", 64 * KBY + 64, [32, 4], F32)
    kb.load(aff, aff[:], affd, affd.ap())
    kb.load(sel, sel[:], seld, seld.ap())
    lo, hi, mid, cnt, ge, tmp = (sm[:, i:i + 1] for i in range(6))
    kb.op("dve", lambda h: h.memset(sm[:], 0.0), writes=[sm])
    kb.op("dve", lambda h: h.memset(sm[:, 1:2], 1.0), writes=[sm])
    kb.op("dve", lambda h: h.memset(onesr[:], 1.0), writes=[onesr])
    for it in range(30):
        kb.op("dve", lambda h: h.scalar_tensor_tensor(mid, lo, 1.0, hi, ALU.mult, ALU.add), reads=[sm], writes=[sm])
        kb.op("dve", lambda h: h.tensor_scalar(mid, mid, 0.5, None, ALU.mult), reads=[sm], writes=[sm])
        kb.op("dve", lambda h: h.tensor_scalar(msk[:], aff[:], mid, None, ALU.is_gt), reads=[aff, sm], writes=[msk])
        kb.op("dve", lambda h: h.tensor_reduce(cnt, msk[:], AX.X, ALU.add), reads=[msk], writes=[sm])
        kb.op("dve", lambda h: h.tensor_scalar(ge, cnt, 511.5, None, ALU.is_gt), reads=[sm], writes=[sm])
        kb.op("dve", lambda h: h.tensor_tensor(tmp, mid, lo, ALU.subtract), reads=[sm], writes=[sm])
        kb.op("dve", lambda h: h.scalar_tensor_tensor(lo, tmp, ge, lo, ALU.mult, ALU.add), reads=[sm], writes=[sm])
        kb.op("dve", lambda h: h.tensor_tensor(tmp, hi, mid, ALU.subtract), reads=[sm], writes=[sm])
        kb.op("dve", lambda h: h.scalar_tensor_tensor(hi, tmp, ge, mid, ALU.mult, ALU.add), reads=[sm], writes=[sm])
    kb.op("dve", lambda h: h.tensor_scalar(msk[:], aff[:], lo, None, ALU.is_gt), reads=[aff, sm], writes=[msk])
    kb.op("dve", lambda h: h.tensor_tensor_scan(cum[:], onesr[:], msk[:], 0.0, ALU.mult, ALU.add), reads=[onesr, msk], writes=[cum])
    kb.op("dve", lambda h: h.tensor_scalar(cum[:], cum[:], -513.0, None, ALU.add), reads=[cum], writes=[cum])
    kb.op("dve", lambda h: h.tensor_tensor(cum[:], cum[:], msk[:], ALU.mult), reads=[cum, msk], writes=[cum])
    kb.op("dve", lambda h: h.tensor_scalar(cum[:], cum[:], 512.0, 512.0, ALU.add, ALU.min), reads=[cum], writes=[cum])
    kb.store(slotd, slotd.ap(), cum, cum[:])

    r1 = 65 * KBY
    stT = ar.view("stT", r1, [128, 32, 8], F32)
    iota = ar.view("iota", r1 + 1 * KBY, [128, 512], F32)
    rowid = ar.view("rowid", r1 + 3 * KBY, [128, 64], F32)
    ident = ar.view("identc", r1 + 4 * KBY, [128, 128], BF16)
    tv = ar.view("tv", r1 + 5 * KBY, [128, 32, 2], F32)
    oh = [ar.view(f"oh{i}", r1 + 6 * KBY + i * 2 * KBY, [128, 512], F32) for i in range(2)]
    idxf = ar.view("idxf", r1 + 10 * KBY, [128, 4, 2], F32)
    idxi = [ar.view(f"idxi{j}", r1 + 10 * KBY + 64 + j * 16, [128, 4], I32) for j in range(4)]
    gat = [ar.view(f"gat{j}", r1 + 10 * KBY + 128 + j * 16, [128, 4], F32) for j in range(4)]
    zrow = ar.view("zrow", r1 + 11 * KBY, [1, 4096], BF16)
    kb.load(iota, iota[:], iotad, iotad.ap())
    kb.load(rowid, rowid[:], rowidd, rowidd.ap())
    kb.load(ident, ident[:], identd, identd.ap())
    kb.op("dve", lambda h: h.memset(zrow[:], 0.0), writes=[zrow])
    for j in range(4):
        kb.store(ysd, ysd.ap()[j, 512:513, :], zrow, zrow[:])
    for ti in range(32):
        p = ps[ti % 2]
        kb.mm(p, p[:, 0:4], cum, cum[:, ti * 128:(ti + 1) * 128], sel, sel[:], True, True)
        kb.mm(p, p[:, 4:8], aff, aff[:, ti * 128:(ti + 1) * 128], sel, sel[:], True, True)
        kb.op("act", lambda h: h.activation(stT[:, ti, :], p[:, 0:8], AF.Copy), reads=[p], writes=[stT])
    for j in range(4):
        b = j % 2
        kb.op("dve", lambda h: h.tensor_copy(tv[:, :, 0], rowid[:, b * 32:(b + 1) * 32]), reads=[rowid], writes=[tv])
        kb.op("dve", lambda h: h.tensor_copy(tv[:, :, 1], stT[:, :, 4 + j]), reads=[stT], writes=[tv])
        ip = ps[2 + j % 2]
        for ti in range(32):
            o_ = oh[ti % 2]
            kb.op("dve", lambda h: h.tensor_scalar(o_[:], iota[:], stT[:, ti, j:j + 1], None, ALU.is_equal), reads=[iota, stT], writes=[o_])
            for sc in range(4):
                kb.op("pe", lambda h: h.matmul(ip[:, sc * 2:sc * 2 + 2], o_[:, sc * 128:(sc + 1) * 128], tv[:, ti, :], start=(ti == 0 and sc == 0), stop=(ti == 31),
                                               skip_group_check=True),
                      reads=[o_, tv], writes=[ip], sig=(sc == 3))
        kb.op("dve", lambda h: h.tensor_copy(idxf[:], ip[:, 0:8].rearrange("p (a b) -> p a b", a=4)), reads=[ip], writes=[idxf])
        kb.op("dve", lambda h: h.tensor_copy(idxi[j][:], idxf[:, :, 0]), reads=[idxf], writes=[idxi[j]])
        kb.op("dve", lambda h: h.tensor_copy(gat[j][:], idxf[:, :, 1]), reads=[idxf], writes=[gat[j]])

    route_dead = [aff, msk, cum, onesr, stT, iota, tv, oh[0], oh[1], rowid]
    xs = ar.view("xs", 84 * KBY, [128, 4096], BF16)
    xsT = [ar.view(f"xsT{b}", 92 * KBY + b * 32 * KBY, [128, 32, 512], BF16) for b in range(2)]
    actT = [ar.view(f"actT{b}", 156 * KBY + b * 8 * KBY, [128, 8, 512], BF16) for b in range(2)]
    wb = [ar.view("wb0", 0, [128, 32, 512], BF16, prev=route_dead), ar.view("wb1", 32 * KBY, [128, 32, 512], BF16, prev=route_dead),
          ar.view("wb2", 172 * KBY, [128, 32, 512], BF16)]
    wbd = [ar.t[:, 0:16384].rearrange("p (k n) -> p k n", k=8), ar.t[:, 16384:32768].rearrange("p (k n) -> p k n", k=8),
           ar.t[:, 86 * 1024:86 * 1024 + 16384].rearrange("p (k n) -> p k n", k=8)]
    sgt = ar.view("sgt", r1, [128, 512], F32, prev=route_dead)
    yst = [ar.view(f"yst{i}", r1 + 2 * KBY + i * KBY, [128, 512], BF16, prev=route_dead) for i in range(2)]
    wi = 0
    for el in range(2):
        for b in range(2):
            j = el * 2 + b
            for sc in range(4):
                kb.dma("pool", lambda h: h.indirect_dma_start(out=xs[:], out_offset=None, in_=bld.ap(),
                                                              in_offset=bass.IndirectOffsetOnAxis(ap=idxi[j][:, sc:sc + 1], axis=0)),
                       reads=[bld, idxi[j]], writes=[xs], sembuf=xs)
                for bk in range(4):
                    p = ps[4 + bk]
                    for q in range(8):
                        k = bk * 8 + q
                        kb.op("pe", lambda h: h.transpose(p.bf[:, q * 128:(q + 1) * 128], xs[:, k * 128:(k + 1) * 128], ident[:]), reads=[xs, ident], writes=[p], sig=(q == 7))
                    o_ap = xsT[b][:, bk * 8:(bk + 1) * 8, sc * 128:(sc + 1) * 128]
                    i_ap = p.bf[:].rearrange("p (a b) -> p a b", a=8)
                    if bk % 2 == 0:
                        kb.op("act", lambda h: h.activation(o_ap, i_ap, AF.Copy), reads=[p], writes=[xsT[b]])
                    else:
                        kb.op("dve", lambda h: h.tensor_copy(o_ap, i_ap), reads=[p], writes=[xsT[b]])
        wgv = wg.ap()[el].rearrange("(k p) n -> p k n", p=128)
        wuv = wu.ap()[el].rearrange("(k p) n -> p k n", p=128)
        wdv = wd.ap()[el].rearrange("(k p) n -> p k n", p=128)
        it = 0
        for hf in range(2):
            gi = wi % 3; wi += 1
            ui = wi % 3; wi += 1
            kb.dma("pool", lambda h: h.dma_start(out=wb[gi][:], in_=wgv[:, :, hf * 512:(hf + 1) * 512]), reads=[wg], writes=[wb[gi]], sembuf=wb[gi])
            kb.dma("pool", lambda h: h.dma_start(out=wb[ui][:], in_=wuv[:, :, hf * 512:(hf + 1) * 512]), reads=[wu], writes=[wb[ui]], sembuf=wb[ui])
            for b in range(2):
                for f4 in range(4):
                    gp = ps[(it * 2) % 4]; up = ps[(it * 2 + 1) % 4]; it += 1
                    for k in range(32):
                        kb.mm(gp, gp[:], wb[gi], wb[gi][:, k, f4 * 128:(f4 + 1) * 128], xsT[b], xsT[b][:, k, :], k == 0, k == 31)
                    for k in range(32):
                        kb.mm(up, up[:], wb[ui], wb[ui][:, k, f4 * 128:(f4 + 1) * 128], xsT[b], xsT[b][:, k, :], k == 0, k == 31)
                    kb.op("act", lambda h: h.activation(sgt[:], gp[:], AF.Silu), reads=[gp], writes=[sgt])
                    kb.op("dve", lambda h: h.tensor_tensor(actT[b][:, hf * 4 + f4, :], sgt[:], up[:], ALU.mult), reads=[sgt, up], writes=[actT[b]])
        it = 0
        for dq in range(2):
            di = wi % 3; wi += 1
            kb.dma("pool", lambda h: h.dma_start(out=wbd[di], in_=wdv[:, :, dq * 2048:(dq + 1) * 2048]), reads=[wd], writes=[wb[di]], sembuf=wb[di])
            for b in range(2):
                j = el * 2 + b
                for sc in range(4):
                    for c4 in range(4):
                        yp = ps[4 + it % 4]; ys_ = yst[it % 2]; it += 1
                        for k in range(8):
                            kb.mm(yp, yp[:], actT[b], actT[b][:, k, sc * 128:(sc + 1) * 128], wb[di], wbd[di][:, k, c4 * 512:(c4 + 1) * 512], k == 0, k == 7)
                        kb.op("act", lambda h: h.activation(ys_[:], yp[:], AF.Copy, scale=gat[j][:, sc:sc + 1]), reads=[yp, gat[j]], writes=[ys_])
                        c0 = dq * 2048 + c4 * 512
                        kb.store(ysd, ysd.ap()[j, sc * 128:(sc + 1) * 128, c0:c0 + 512], ys_, ys_[:])
    return kb.finish()


def launch_C(inp, layer, affT_all, bl_all):
    nc = build_C()
    iota = np.tile(np.arange(512, dtype=np.float32)[None], (128, 1))
    rowid = (np.arange(64, dtype=np.float32)[None, :] * 128 + np.arange(128, dtype=np.float32)[:, None]).astype(np.float32)
    ident = np.eye(128, dtype=np.float32).astype(NPBF)
    maps = []
    for c in range(NCORES):
        sel = np.zeros((32, 4), np.float32)
        for el in range(2):
            for b in range(2):
                sel[b * 16 + 2 * c + el, el * 2 + b] = 1.0
        maps.append({"aff": affT_all, "bl": bl_all,
                     "wg": np.ascontiguousarray(inp["expert_w_gate"][layer, 2 * c:2 * c + 2]),
                     "wu": np.ascontiguousarray(inp["expert_w_up"][layer, 2 * c:2 * c + 2]),
                     "wd": np.ascontiguousarray(inp["expert_w_down"][layer, 2 * c:2 * c + 2]),
                     "sel": sel, "iota": iota, "rowid": rowid, "ident": ident})
    r = run(nc, maps)
    ys_all = np.zeros((2, 16, 513, D), NPBF)
    for c in range(NCORES):
        for el in range(2):
            for b in range(2):
                ys_all[b, 2 * c + el] = r[c]["ys"][el * 2 + b]
    return ys_all, r[0]["slotm"]


def build_D(final):
    kb = KB()
    hin = kb.dram("hin", [1024, 4096], F32, IN)
    ysf = kb.dram("ysf", [16 * 513, 4096], BF16, IN)
    slotd = kb.dram("slot", [16, 1024], F32, IN)
    rows = kb.dram("rows", [2, 4096], F32, IN)
    cf = kb.dram("cf", [128, 32], F32, IN)
    identd = kb.dram("ident", [128, 128], BF16, IN)
    if final:
        outd = kb.dram("out", [1024, 4096], F32, OUT)
    else:
        gsd = kb.dram("gs", [128, 3, 32], F32, IN)
        h2d = kb.dram("h2", [1024, 4096], F32, OUT)
        aTd = kb.dram("aT1", [4096, 1024], BF16, OUT)
    ps = psum_banks(kb)
    ar = Arena(kb, "arenaD", 204 * 1024)
    KBY = 1024
    G = [ar.view(f"G{i}", i * 8 * KBY, [128, 4096], BF16) for i in range(4)]
    hi_ = ar.view("hi", 32 * KBY, [128, 4096], F32)
    h2t = ar.view("h2t", 48 * KBY, [128, 4096], F32)
    m5b = ar.view("m5b", 64 * KBY, [128, 4096], F32)
    xn = ar.view("xn", 80 * KBY, [128, 4096], BF16)
    big = ar.view("big", 88 * KBY, [128, 32, 1024], BF16) if not final else ar.view("gfin", 88 * KBY, [128, 4096], F32)
    slot = ar.view("slot", 152 * KBY, [16, 1024], F32)
    cfb = ar.view("cfb", 156 * KBY, [128, 32], F32)
    ident = ar.view("identd", 156 * KBY + 128, [128, 128], BF16)
    posf = ar.view("posf", 157 * KBY, [128, 16], F32)
    posi = ar.view("posi", 157 * KBY + 64, [128, 16], I32)
    ssb = ar.view("ssb", 157 * KBY + 128, [128, 8], F32)
    epsb = ar.view("epsb", 157 * KBY + 160, [128, 1], F32)
    GS = ar.view("GS", 158 * KBY, [128, 2, 32], F32)
    gsb = ar.view("gsb", 158 * KBY + 256, [128, 3, 32], F32)
    kb.load(slot, slot[:], slotd, slotd.ap())
    kb.load(cfb, cfb[:], cf, cf.ap())
    kb.load(ident, ident[:], identd, identd.ap())
    kb.load(m5b, m5b[:], rows, rows.ap()[0, :].partition_broadcast(128))
    kb.op("dve", lambda h: h.memset(epsb[:], 1e-6), writes=[epsb])
    if final:
        kb.load(big, big[:], rows, rows.ap()[1, :].partition_broadcast(128))
    else:
        kb.load(gsb, gsb[:], gsd, gsd.ap())
        kb.op("dve", lambda h: h.scalar_tensor_tensor(GS[:, 0, :], gsb[:, 1, :], 1.0, gsb[:, 0, :], ALU.add, ALU.mult), reads=[gsb], writes=[GS])
        kb.op("dve", lambda h: h.tensor_copy(GS[:, 1, :], gsb[:, 2, :]), reads=[gsb], writes=[GS])
    gi = 0
    for tq in range(8):
        pp = ps[0]
        kb.mm(pp, pp[:, 0:16], slot, slot[0:16, tq * 128:(tq + 1) * 128], cfb, cfb[0:16, 0:16], True, True)
        kb.op("dve", lambda h: h.tensor_tensor(posf[:], pp[:, 0:16], cfb[:, 16:32], ALU.add), reads=[pp, cfb], writes=[posf])
        kb.op("dve", lambda h: h.tensor_copy(posi[:], posf[:]), reads=[posf], writes=[posi])
        kb.load(hi_, hi_[:], hin, hin.ap()[tq * 128:(tq + 1) * 128, :])
        for e in range(16):
            g_ = G[gi % 4]; gi += 1
            kb.dma("pool", lambda h: h.indirect_dma_start(out=g_[:], out_offset=None, in_=ysf.ap(),
                                                          in_offset=bass.IndirectOffsetOnAxis(ap=posi[:, e:e + 1], axis=0)),
                   reads=[ysf, posi], writes=[g_], sembuf=g_)
            for ct in range(8):
                kb.mm(ps[ct], ps[ct][:], ident, ident[:], g_, g_[:, ct * 512:(ct + 1) * 512], e == 0, e == 15, sig=(ct == 7 or e == 15))
        for ct in range(8):
            sl = slice(ct * 512, (ct + 1) * 512)
            kb.op("dve", lambda h: h.tensor_tensor(h2t[:, sl], ps[ct][:], m5b[:, sl], ALU.mult), reads=[ps[ct], m5b], writes=[h2t])
        kb.op("dve", lambda h: h.tensor_tensor(h2t[:], h2t[:], hi_[:], ALU.add), reads=[h2t, hi_], writes=[h2t])
        kb.op("act", lambda h: h.activation(xn[:], h2t[:], AF.Square, accum_out=ssb[:, 0:1]), reads=[h2t], writes=[xn, ssb])
        rms_rstd(kb, ssb[:, 0:1], ssb, epsb, 4096)
        if final:
            kb.op("act", lambda h: h.activation(hi_[:], h2t[:], AF.Copy, scale=ssb[:, 0:1]), reads=[h2t, ssb], writes=[hi_])
            kb.op("dve", lambda h: h.tensor_tensor(hi_[:], hi_[:], big[:], ALU.mult), reads=[hi_, big], writes=[hi_])
            kb.store(outd, outd.ap()[tq * 128:(tq + 1) * 128, :], hi_, hi_[:])
        else:
            kb.store(h2d, h2d.ap()[tq * 128:(tq + 1) * 128, :], h2t, h2t[:])
            kb.op("act", lambda h: h.activation(xn[:], h2t[:], AF.Copy, scale=ssb[:, 0:1]), reads=[h2t, ssb], writes=[xn])
            for bk in range(4):
                p = ps[bk]
                for j in range(8):
                    k = bk * 8 + j
                    kb.op("pe", lambda h: h.transpose(p.bf[:, j * 128:(j + 1) * 128], xn[:, k * 128:(k + 1) * 128], ident[:]), reads=[xn, ident], writes=[p], sig=(j == 7))
                for j in range(8):
                    k = bk * 8 + j
                    o_ap = big[:, k, tq * 128:(tq + 1) * 128]; i_ap = p.bf[:, j * 128:(j + 1) * 128]
                    if bk % 2 == 0:
                        kb.op("act", lambda h: h.activation(o_ap, i_ap, AF.Identity, bias=GS[:, 1, k:k + 1], scale=GS[:, 0, k:k + 1]), reads=[p, GS], writes=[big])
                    else:
                        kb.op("dve", lambda h: h.tensor_scalar(o_ap, i_ap, GS[:, 0, k:k + 1], GS[:, 1, k:k + 1], ALU.mult, ALU.add), reads=[p, GS], writes=[big])
    if not final:
        kb.store(aTd, aTd.ap().rearrange("(k p) t -> p k t", p=128), big, big[:])
    return kb.finish()


def launch_D(inp, final, h_in, ys_all, slotm, mods, layer):
    nc = build_D(final)
    cf = np.zeros((128, 32), np.float32)
    cf[0:16, 0:16] = np.eye(16, dtype=np.float32)
    cf[:, 16:32] = (np.arange(16, dtype=np.float32) * 513)[None, :]
    ident = np.eye(128, dtype=np.float32).astype(NPBF)
    maps = []
    for c in range(NCORES):
        b, q = c // 4, c % 4
        m = {"hin": np.ascontiguousarray(h_in[b, q * 1024:(q + 1) * 1024]), "ysf": ys_all[b].reshape(16 * 513, D),
             "slot": np.ascontiguousarray(slotm[b * 16:(b + 1) * 16, q * 1024:(q + 1) * 1024]),
             "rows": np.stack([mods[layer, b, 5], np.asarray(inp["final_norm_g"], np.float32)], 0).astype(np.float32),
             "cf": cf, "ident": ident}
        if not final:
            m["gs"] = np.ascontiguousarray(np.stack([fm(inp["norm_mix_g"][1]), fm(mods[1, b, 1]), fm(mods[1, b, 0])], 1))
        maps.append(m)
    return run(nc, maps)


def build_F():
    kb = KB()
    xres = kb.dram("xres", [1024, 4096], F32, IN)
    catT = kb.dram("catT", [4096, 1024], BF16, IN)
    w_out = kb.dram("w_out", [4096, 4096], F32, IN)
    rowv = kb.dram("rowv", [4, 4096], F32, IN)
    wr = kb.dram("wr", [128, 32, 16], F32, IN)
    identf = kb.dram("identf", [128, 128], F32, IN)
    h1 = kb.dram("h1", [1024, 4096], F32, OUT)
    bl = kb.dram("bl", [1024, 4096], BF16, OUT)
    affT = kb.dram("affT", [16, 1024], F32, OUT)
    ps = psum_banks(kb)
    ar = Arena(kb, "arenaF", 204 * 1024)
    epsb = ar.view("epsb", 200 * 1024, [128, 1], F32)
    kb.op("dve", lambda h: h.memset(epsb[:], 1e-6), writes=[epsb])
    ffn_front(kb, ar, ps, [], epsb, xres, catT, w_out, rowv, wr, identf, h1, bl, affT)
    return kb.finish()


def launch_F(inp, mods, h2, finT_all):
    nc = build_F()
    w_out = np.ascontiguousarray(inp["hyena_w_out"][0])
    wr = np.ascontiguousarray(np.asarray(inp["router_w"][1], np.float32).reshape(32, 128, 16).transpose(1, 0, 2))
    ident = np.eye(128, dtype=np.float32)
    maps = []
    for c in range(NCORES):
        b, q = c // 4, c % 4
        rowv = np.stack([mods[1, b, 2], inp["norm_ffn_g"][1], mods[1, b, 4], mods[1, b, 3]], 0).astype(np.float32)
        maps.append({"xres": np.ascontiguousarray(h2[b, q * 1024:(q + 1) * 1024]),
                     "catT": np.ascontiguousarray(finT_all[b][:, q * 1024:(q + 1) * 1024]),
                     "w_out": w_out, "rowv": np.ascontiguousarray(rowv), "wr": wr, "identf": ident})
    return run(nc, maps)


def gather_rows(r, key):
    return np.stack([np.concatenate([r[b * 4 + q][key] for q in range(4)], 0) for b in range(2)], 0)


def hy_consts():
    N = 8192
    P = np.arange(256)[:, None]; FP = np.arange(256)[None, :]
    w = 2 * np.pi * P * FP / 256
    W256cat = np.stack([np.concatenate([np.cos(w[h * 128:(h + 1) * 128]), -np.sin(w[h * 128:(h + 1) * 128])], 1) for h in range(2)], 1)
    m = np.arange(128); a_of = m // 4; c_of = m % 4
    tw = 2 * np.pi * a_of[:, None] * np.arange(256)[None, :] / N
    TW = np.concatenate([np.cos(tw), -np.sin(tw), np.cos(tw)], 1)
    ang32 = 2 * np.pi * a_of[:, None] * a_of[None, :] / 32
    same = (c_of[:, None] == c_of[None, :]).astype(np.float64)
    M32 = np.stack([np.cos(ang32) * same, -np.sin(ang32) * same], 1)
    Vre = np.cos(ang32) * same; Vim = np.sin(ang32) * same
    Vcat = np.stack([np.concatenate([Vre, Vim], 1), np.concatenate([-Vim, Vre], 1)], 1)
    T2 = np.zeros((128, 2, 3, 128)); U = np.zeros((128, 2, 2, 128))
    for jc in range(2):
        fp = jc * 128 + np.arange(128)
        t2 = 2 * np.pi * fp[:, None] * a_of[None, :] / N
        T2[:, jc, 0] = np.cos(t2); T2[:, jc, 1] = np.sin(t2); T2[:, jc, 2] = np.cos(t2)
        u = 2 * np.pi * fp[:, None] * np.arange(128)[None, :] / 256
        U[:, jc, 0] = np.cos(u) / N; U[:, jc, 1] = -np.sin(u) / N
    return (W256cat.astype(NPBF), TW.astype(np.float32), M32.astype(NPBF), Vcat.astype(NPBF), T2.astype(np.float32), U.astype(NPBF))


def build_E():
    kb = KB()
    aTf = kb.dram("aTf", [2, 4096, 4098], BF16, IN)
    wind = kb.dram("win", [4096, 1536], F32, IN)
    cwd = kb.dram("cw", [128, 12, 4], F32, IN)
    hbd = kb.dram("hb", [128, 4], F32, IN)
    featd = kb.dram("feat", [2, 33, 4096], F32, IN)
    mlpd = kb.dram("mlp", [64, 200], F32, IN)
    w3d = kb.dram("w3", [64, 2, 512], F32, IN)
    t01d = kb.dram("t01", [128, 2, 32], F32, IN)
    deld = kb.dram("delt", [128, 512], F32, IN)
    m0d = kb.dram("m0", [128, 1], F32, IN)
    c_w256 = kb.dram("c_w256", [128, 2, 512], BF16, IN)
    c_tw = kb.dram("c_tw", [128, 768], F32, IN)
    c_m32 = kb.dram("c_m32", [128, 2, 128], BF16, IN)
    c_vcat = kb.dram("c_vcat", [128, 2, 256], BF16, IN)
    c_t2 = kb.dram("c_t2", [128, 2, 3, 128], F32, IN)
    c_u = kb.dram("c_u", [128, 2, 2, 128], BF16, IN)
    c_id = kb.dram("c_id", [128, 128], BF16, IN)
    Khd = kb.dram("Kh", [128, 128, 768], F32, OUT)
    find = kb.dram("finT", [512, 2, 4096], BF16, OUT)
    ps = psum_banks(kb)
    ar = Arena(kb, "arenaE", 204 * 1024)
    KBY = 1024
    o = 160 * KBY
    def cv(name, shape, dt):
        nonlocal o
        n = 1
        for s_ in shape[1:]: n *= s_
        b_ = ar.view(name, o, shape, dt); o += ((n * (2 if dt == BF16 else 4) + 3) // 4) * 4
        return b_
    w256 = cv("w256", [128, 2, 512], BF16); tw = cv("tw", [128, 768], F32); m32 = cv("m32", [128, 2, 128], BF16)
    vcat = cv("vcat", [128, 2, 256], BF16); t2c = cv("t2c", [128, 2, 384], F32); uc = cv("uc", [128, 4, 128], BF16)
    ident = cv("identE", [128, 128], BF16); cwb = cv("cwb", [128, 12, 4], F32); hbb = cv("hbb", [128, 4], F32)
    rn = cv("rn", [128, 4], F32); m0 = cv("m0", [128, 1], F32); t01 = cv("t01", [128, 2, 32], F32)
    hpi = cv("hpi", [128, 1], F32); onesb = cv("onesb", [128, 1], BF16)
    ta01 = cv("ta01", [128, 512], F32); ta23 = cv("ta23", [128, 512], F32)
    Bp = cv("Bp", [128, 768], BF16); Yh = cv("Yh", [128, 512], BF16)
    Cp = cv("Cp", [128, 2, 2, 512], BF16)
    Kb = [cv(f"Kb{i}", [128, 768], F32) for i in range(2)]
    assert o <= 204 * KBY, o
    for (dst, src) in ((w256, c_w256), (tw, c_tw), (m32, c_m32), (vcat, c_vcat), (ident, c_id), (cwb, cwd), (hbb, hbd), (m0, m0d), (t01, t01d)):
        kb.load(dst, dst[:], src, src.ap())
    kb.load(t2c, t2c[:], c_t2, c_t2.ap().rearrange("p a b c -> p a (b c)"))
    kb.load(uc, uc[:], c_u, c_u.ap().rearrange("p a b c -> p (a b) c"))
    kb.op("dve", lambda h: h.memset(hpi[:], math.pi / 2), writes=[hpi])
    kb.op("dve", lambda h: h.memset(onesb[:], 1.0), writes=[onesb])

    ktok = ar.view("ktok", 32 * KBY, [128, 2, 128, 128], BF16)
    r0 = 96 * KBY
    feat = ar.view("feat", r0, [33, 4096], F32)
    hid1 = ar.view("hid1", r0 + 16 * KBY, [64, 4096], F32)
    hid2 = [ar.view(f"hid2{i}", r0 + 32 * KBY + i * 16 * KBY, [64, 4096], F32) for i in range(2)]
    zreg = 0
    mlp = ar.view("mlp", zreg, [64, 200], F32)
    w3 = ar.view("w3", zreg + 1 * KBY, [64, 2, 512], F32)
    delt = ar.view("delt", zreg + 5 * KBY, [128, 512], F32)
    dec = ar.view("dec", zreg + 7 * KBY, [128, 512], F32)
    kf = ar.view("kf", zreg + 9 * KBY, [128, 512], F32)
    kab = ar.view("kab", zreg + 11 * KBY, [128, 512], BF16)
    s1 = ar.view("s1", zreg + 12 * KBY, [64, 512], F32); s2 = ar.view("s2", zreg + 14 * KBY, [64, 512], F32); sa = ar.view("sa", zreg + 16 * KBY, [64, 512], F32)
    sx = ar.view("sx", zreg + 18 * KBY, [64, 512], F32)
    kb.load(mlp, mlp[:], mlpd, mlpd.ap()); kb.load(w3, w3[:], w3d, w3d.ap()); kb.load(delt, delt[:], deld, deld.ap())

    def sin_layer(src_ps, bcol, fcol, dst_ap, dstb):
        kb.op("dve", lambda h: h.tensor_scalar(sx[:], src_ps[0:64, :], mlp[:, bcol:bcol + 1], mlp[:, fcol:fcol + 1], ALU.add, ALU.mult), reads=[src_ps, mlp], writes=[sx])
        kb.op("act", lambda h: h.activation(s1[:], sx[:], AF.Sin, scale=0.5), reads=[sx], writes=[s1])
        kb.op("act", lambda h: h.activation(sa[:], sx[:], AF.Abs, scale=0.5), reads=[sx], writes=[sa])
        kb.op("act", lambda h: h.activation(s2[:], sa[:], AF.Sin, bias=hpi[0:64, 0:1], scale=-1.0), reads=[sa, hpi], writes=[s2])
        kb.op("dve", lambda h: h.scalar_tensor_tensor(dst_ap, s1[:], 2.0, s2[:], ALU.mult, ALU.mult), reads=[s1, s2], writes=[dstb])

    for half in range(2):
        kb.load(feat, feat[:], featd, featd.ap()[half])
        for tt in range(8):
            p = ps[tt % 2]
            kb.mm(p, p[0:64, :], mlp, mlp[0:33, 0:64], feat, feat[:, tt * 512:(tt + 1) * 512], True, True)
            sin_layer(p, 128, 130, hid1[:, tt * 512:(tt + 1) * 512], hid1)
        for tt in range(8):
            p = ps[2 + tt % 2]
            kb.mm(p, p[0:64, :], mlp, mlp[:, 64:128], hid1, hid1[:, tt * 512:(tt + 1) * 512], True, True)
            sin_layer(p, 129, 131, hid2[half][:, tt * 512:(tt + 1) * 512], hid2[half])
    nps = ps[7]
    first = True
    for half in range(2):
        h3 = hid2[half][:].rearrange("k (p a) -> k a p", a=32)
        for a in range(32):
            p = ps[4 + a % 2]
            kb.mm(p, p[:], hid2[half], h3[:, a, :], w3, w3[:, half, :], True, True)
            kb.op("act", lambda h: h.activation(dec[:], delt[:], AF.Exp, scale=t01[:, half, a:a + 1]), reads=[delt, t01], writes=[dec])
            kb.op("dve", lambda h: h.tensor_tensor(kf[:], p[:], dec[:], ALU.mult), reads=[p, dec], writes=[kf])
            if half == 1 and a == 0:
                kb.op("dve", lambda h: h.tensor_scalar(kf[:], kf[:], m0[:, 0:1], None, ALU.mult), reads=[kf, m0], writes=[kf])
            kb.op("act", lambda h: h.activation(ktok[:, half, :, a * 4:(a + 1) * 4], kf[:].rearrange("p (g c) -> p g c", c=4), AF.Copy), reads=[kf], writes=[ktok])
            kb.op("act", lambda h: h.activation(kab[:], kf[:], AF.Abs), reads=[kf], writes=[kab])
            for cc in range(4):
                last = (half == 1 and a == 31)
                kb.op("pe", lambda h: h.matmul(nps[:, cc:cc + 1], kab[:, cc * 128:(cc + 1) * 128], onesb[:], start=first, stop=last, skip_group_check=True),
                      reads=[kab, onesb], writes=[nps], sig=(cc == 3))
                first = False
    kb.op("dve", lambda h: h.reciprocal(rn[:], nps[:, 0:4]), reads=[nps], writes=[rn])

    cnt = {"g": 0}

    def fft_fwd(srcb, src_aps):
        i = cnt["g"]; cnt["g"] += 1
        p1 = ps[i % 2]; p2 = ps[2 + i % 2]
        for hh, ap_ in enumerate(src_aps):
            kb.mm(p1, p1[:], srcb, ap_, w256, w256[:, hh, :], hh == 0, hh == len(src_aps) - 1)
        kb.op("dve", lambda h: h.tensor_tensor(ta01[:], p1[:], tw[:, 0:512], ALU.mult), reads=[p1, tw], writes=[ta01])
        kb.op("dve", lambda h: h.tensor_tensor(ta23[:], p1[:], tw[:, 256:768], ALU.mult), reads=[p1, tw], writes=[ta23])
        kb.op("dve", lambda h: h.tensor_tensor(Bp[:, 256:512], ta01[:, 0:256], ta01[:, 256:512], ALU.subtract), reads=[ta01], writes=[Bp])
        kb.op("dve", lambda h: h.tensor_tensor(ta23[:, 0:256], ta23[:, 0:256], ta23[:, 256:512], ALU.add), reads=[ta23], writes=[ta23])
        kb.op("act", lambda h: h.activation(Bp[:, 512:768], ta23[:, 0:256], AF.Copy), reads=[ta23], writes=[Bp])
        kb.op("act", lambda h: h.activation(Bp[:, 0:256], ta23[:, 0:256], AF.Copy, scale=-1.0), reads=[ta23], writes=[Bp])
        kb.mm(p2, p2[:], m32, m32[:, 0, :], Bp, Bp[:, 256:768], True, False)
        kb.mm(p2, p2[:], m32, m32[:, 1, :], Bp, Bp[:, 0:512], False, True)
        return p2

    for g in range(128):
        p2 = fft_fwd(ktok, [ktok[:, hh, g, :] for hh in range(2)])
        kbuf = Kb[g % 2]
        kb.op("act", lambda h: h.activation(kbuf[:, 0:512], p2[:], AF.Copy), reads=[p2], writes=[kbuf])
        kb.op("act", lambda h: h.activation(kbuf[:, 512:768], p2[:, 0:256], AF.Copy), reads=[p2], writes=[kbuf])
        kb.store(Khd, Khd.ap()[g], kbuf, kbuf[:])

    filt_dead = [ktok, feat, hid1, hid2[0], hid2[1], mlp, w3, delt, dec, kf, kab, s1, s2, sa, sx]
    zT = ar.view("zT", 0, [128, 4, 4096], BF16, prev=filt_dead)
    atile = [ar.view(f"at{i}", 32 * KBY + i * 32 * KBY, [128, 32, 512], BF16, prev=filt_dead) for i in range(2)]
    ztok = ar.view("ztok", 32 * KBY, [128, 128, 128], BF16, prev=filt_dead)
    ytok = ar.view("ytok", 64 * KBY, [128, 32, 512], BF16, prev=filt_dead)
    ztok.w = atile[0].w; ztok.r = atile[0].r; ytok.w = atile[1].w; ytok.r = atile[1].r
    yT = ar.view("yT", 96 * KBY, [128, 4, 4096], BF16, prev=filt_dead)
    wp = ar.view("wp", 128 * KBY, [128, 32, 512], BF16, prev=filt_dead)
    ct_ = [ar.view(f"ct{i}", 96 * KBY + i * 2 * KBY, [128, 512], F32, prev=filt_dead) for i in range(2)]
    cx = [ar.view(f"cx{i}", 156 * KBY + i * 2 * KBY, [128, 512], F32) for i in range(2)] if False else None
    winv = wind.ap().rearrange("(k p) n -> p k n", p=128)
    tiles = [(510 * i, 510) for i in range(8)] + [(4080, 16)]
    for b in range(2):
        xdead = [ztok, ytok] if b > 0 else []
        for part in (1, 2, 0):
            kb.dma("pool", lambda h: h.dma_start(out=wp[:], in_=winv[:, :, part * 512:(part + 1) * 512]), reads=[wind], writes=[wp], sembuf=wp)
            if part == 0:
                for cbk in range(4):
                    for a0 in range(0, 32, 8):
                        p = ps[4 + (a0 // 8) % 2]
                        z3 = zT[:, cbk, :].rearrange("c (p a) -> c a p", a=32)
                        for q in range(8):
                            kb.op("pe", lambda h: h.transpose(p.bf[:, q * 128:(q + 1) * 128], z3[:, a0 + q, :], ident[:]), reads=[zT, ident], writes=[p], sig=(q == 7))
                        kb.op("act", lambda h: h.activation(ztok[:, cbk * 32:(cbk + 1) * 32, a0 * 4:(a0 + 8) * 4].rearrange("p g (a c) -> p g a c", c=4), p.bf[:].rearrange("p (a g c) -> p g a c", a=8, c=4), AF.Copy),
                              reads=[p], writes=[ztok])
                kb.load(Kb[0], Kb[0][:], Khd, Khd.ap()[0])
                for g in range(128):
                    if g < 127:
                        kb.load(Kb[(g + 1) % 2], Kb[(g + 1) % 2][:], Khd, Khd.ap()[g + 1])
                    kbuf = Kb[g % 2]
                    p2 = fft_fwd(ztok, [ztok[:, g, :]])
                    kb.op("dve", lambda h: h.tensor_tensor(ta01[:], p2[:], kbuf[:, 0:512], ALU.mult), reads=[p2, kbuf], writes=[ta01])
                    kb.op("dve", lambda h: h.tensor_tensor(ta23[:], p2[:], kbuf[:, 256:768], ALU.mult), reads=[p2, kbuf], writes=[ta23])
                    kb.op("dve", lambda h: h.tensor_tensor(Yh[:, 0:256], ta01[:, 0:256], ta01[:, 256:512], ALU.subtract), reads=[ta01], writes=[Yh])
                    kb.op("dve", lambda h: h.tensor_tensor(Yh[:, 256:512], ta23[:, 0:256], ta23[:, 256:512], ALU.add), reads=[ta23], writes=[Yh])
                    g4 = g % 4
                    for jc in range(2):
                        p3 = ps[4 + jc]
                        kb.mm(p3, p3[:, 0:256], Yh, Yh[:, jc * 128:(jc + 1) * 128], vcat, vcat[:, 0, :], True, False)
                        kb.mm(p3, p3[:, 0:256], Yh, Yh[:, 256 + jc * 128:256 + (jc + 1) * 128], vcat, vcat[:, 1, :], False, True)
                        kb.op("dve", lambda h: h.tensor_tensor(ta01[:, 0:256], p3[:, 0:256], t2c[:, jc, 0:256], ALU.mult), reads=[p3, t2c], writes=[ta01])
                        kb.op("dve", lambda h: h.tensor_tensor(ta23[:, 0:256], p3[:, 0:256], t2c[:, jc, 128:384], ALU.mult), reads=[p3, t2c], writes=[ta23])
                        kb.op("dve", lambda h: h.tensor_tensor(Cp[:, jc, 0, g4 * 128:(g4 + 1) * 128], ta01[:, 0:128], ta01[:, 128:256], ALU.subtract), reads=[ta01], writes=[Cp])
                        kb.op("dve", lambda h: h.tensor_tensor(Cp[:, jc, 1, g4 * 128:(g4 + 1) * 128], ta23[:, 0:128], ta23[:, 128:256], ALU.add), reads=[ta23], writes=[Cp])
                    if g4 == 3:
                        G4 = g // 4
                        p4 = ps[6 + G4 % 2]
                        kb.mm(p4, p4[:], uc, uc[:, 0, :], Cp, Cp[:, 0, 0, :], True, False)
                        kb.mm(p4, p4[:], uc, uc[:, 1, :], Cp, Cp[:, 0, 1, :], False, False)
                        kb.mm(p4, p4[:], uc, uc[:, 2, :], Cp, Cp[:, 1, 0, :], False, False)
                        kb.mm(p4, p4[:], uc, uc[:, 3, :], Cp, Cp[:, 1, 1, :], False, True)
                        kb.op("act", lambda h: h.activation(ytok[:, :, 16 * G4:16 * G4 + 16].rearrange("p a (g c) -> p a g c", g=4),
                                                             p4[:].rearrange("p (g a c) -> p a g c", g=4, a=32), AF.Copy), reads=[p4], writes=[ytok])
                for cbk in range(4):
                    y3 = yT[:, cbk, :].rearrange("c (p a) -> c a p", a=32)
                    for a0 in range(0, 32, 8):
                        p = ps[(a0 // 8) % 2]
                        for q in range(8):
                            kb.op("pe", lambda h: h.transpose(p.bf[:, q * 128:(q + 1) * 128], ytok[:, a0 + q, cbk * 128:(cbk + 1) * 128], ident[:]), reads=[ytok, ident], writes=[p], sig=(q == 7))
                        kb.op("act", lambda h: h.activation(y3[:, a0:a0 + 8, :], p.bf[:].rearrange("c (a p) -> c a p", a=8), AF.Copy), reads=[p], writes=[yT])
            for ti, (t0, n) in enumerate(tiles):
                at = atile[ti % 2]
                kb.dma("sp", lambda h: h.dma_start(out=at[:, :, 0:n + 2], in_=aTf.ap()[b].rearrange("(k p) t -> p k t", p=128)[:, :, t0:t0 + n + 2]),
                       reads=[aTf], writes=[at], sembuf=at)
                for cbk in range(4):
                    p = ps[4 + cbk % 2] if part != 0 else ps[2 + cbk % 2]
                    for k in range(32):
                        kb.mm(p, p[:, 0:n + 2], wp, wp[:, k, cbk * 128:(cbk + 1) * 128], at, at[:, k, 0:n + 2], k == 0, k == 31)
                    blk = part * 4 + cbk
                    c1 = Kb[0]; c2 = Kb[1]
                    kb.op("act", lambda h: h.activation(c1[:, 0:n], p[:, 1:n + 1], AF.Identity, bias=cwb[:, blk, 3:4], scale=cwb[:, blk, 1:2]), reads=[p, cwb], writes=[c1])
                    kb.op("dve", lambda h: h.scalar_tensor_tensor(c1[:, 0:n], p[:, 0:n], cwb[:, blk, 0:1], c1[:, 0:n], ALU.mult, ALU.add), reads=[p, cwb, c1], writes=[c1])
                    zsl = zT[:, cbk, t0:t0 + n]
                    if part == 1:
                        kb.op("dve", lambda h: h.scalar_tensor_tensor(zsl, p[:, 2:n + 2], cwb[:, blk, 2:3], c1[:, 0:n], ALU.mult, ALU.add), reads=[p, cwb, c1], writes=[zT])
                    elif part == 2:
                        kb.op("dve", lambda h: h.scalar_tensor_tensor(c1[:, 0:n], p[:, 2:n + 2], cwb[:, blk, 2:3], c1[:, 0:n], ALU.mult, ALU.add), reads=[p, cwb, c1], writes=[c1])
                        kb.op("dve", lambda h: h.tensor_tensor(zsl, zsl, c1[:, 0:n], ALU.mult), reads=[zT, c1], writes=[zT])
                    else:
                        kb.op("dve", lambda h: h.scalar_tensor_tensor(c1[:, 0:n], p[:, 2:n + 2], cwb[:, blk, 2:3], c1[:, 0:n], ALU.mult, ALU.add), reads=[p, cwb, c1], writes=[c1])
                        kb.op("dve", lambda h: h.tensor_scalar(c2[:, 0:n], zsl, hbb[:, cbk:cbk + 1], None, ALU.mult), reads=[zT, hbb], writes=[c2])
                        kb.op("dve", lambda h: h.scalar_tensor_tensor(c2[:, 0:n], yT[:, cbk, t0:t0 + n], rn[:, cbk:cbk + 1], c2[:, 0:n], ALU.mult, ALU.add), reads=[yT, rn, c2], writes=[c2])
                        fo = Yh if False else None
                        kb.op("dve", lambda h: h.tensor_tensor(Bp[:, 0:n], c2[:, 0:n], c1[:, 0:n], ALU.mult), reads=[c1, c2], writes=[Bp])
                        kb.store(find, find.ap()[cbk * 128:(cbk + 1) * 128, b, t0:t0 + n], Bp, Bp[:, 0:n])
    return kb.finish()


def launch_E(inp, aT1_all):
    nc = build_E()
    W256cat, TW, M32, Vcat, T2, U = hy_consts()
    aTf = np.zeros((2, 4096, 4098), NPBF)
    aTf[:, :, 1:4097] = aT1_all
    Lm = 4096
    pos = np.arange(Lm, dtype=np.float32)
    def feats(posv):
        t01 = posv / (Lm - 1)
        bands = np.linspace(1e-4, 15, 16, dtype=np.float32)
        ang = (2.0 * math.pi / Lm) * posv[:, None] * bands[None, :]
        return np.concatenate([t01[:, None], np.cos(ang), -np.sin(ang)], -1).astype(np.float32)
    jrev = (4096 - np.arange(Lm)).astype(np.float32); jrev[0] = 0.0
    feat = np.ascontiguousarray(np.stack([feats(pos).T, feats(jrev).T], 0))
    t01 = np.zeros((128, 2, 32), np.float32)
    ii = (32 * np.arange(128)[:, None] + np.arange(32)[None, :]).astype(np.float32)
    t01[:, 0, :] = -ii / (Lm - 1)
    t01[:, 1, :] = -(4096 - ii) / (Lm - 1)
    deltas = np.abs(np.linspace(math.log(1e-2) / 1.5, math.log(1e-2) / 0.3, 4096, dtype=np.float32))
    m0 = np.ones((128, 1), np.float32); m0[0, 0] = 0.0
    mlp = np.zeros((64, 200), np.float32)
    mlp[:33, 0:64] = inp["hyena_filt_w1"][0]; mlp[:, 64:128] = inp["hyena_filt_w2"][0]
    mlp[:, 128] = inp["hyena_filt_b1"][0]; mlp[:, 129] = inp["hyena_filt_b2"][0]
    mlp[:, 130] = inp["hyena_filt_freq"][0][0]; mlp[:, 131] = inp["hyena_filt_freq"][0][1]
    ident = np.eye(128, dtype=np.float32).astype(NPBF)
    w_in = inp["hyena_w_in"][0]; cwt = np.asarray(inp["hyena_conv_w"][0], np.float32); cbt = np.asarray(inp["hyena_conv_b"][0], np.float32)
    w3full = np.asarray(inp["hyena_filt_w3"][0], np.float32); hbias = np.asarray(inp["hyena_bias"][0], np.float32)
    maps = []
    for c in range(NCORES):
        C0 = 512 * c
        cols = np.concatenate([np.arange(part * 4096 + C0, part * 4096 + C0 + 512) for part in range(3)])
        cw = np.zeros((128, 12, 4), np.float32)
        for blk in range(12):
            cc = cols[blk * 128:(blk + 1) * 128]
            cw[:, blk, 0:3] = cwt[:, cc].T
            cw[:, blk, 3] = cbt[cc]
        maps.append({"aTf": aTf, "win": np.ascontiguousarray(w_in[:, cols]), "cw": cw,
                     "hb": np.ascontiguousarray(hbias[C0:C0 + 512].reshape(4, 128).T), "feat": feat, "mlp": mlp,
                     "w3": np.ascontiguousarray(np.stack([w3full[:, C0:C0 + 512], w3full[:, 4096 + C0:4096 + C0 + 512]], 1)),
                     "t01": t01, "delt": np.ascontiguousarray(np.tile(deltas[None, C0:C0 + 512], (128, 1))), "m0": m0,
                     "c_w256": W256cat, "c_tw": TW, "c_m32": M32, "c_vcat": Vcat, "c_t2": T2, "c_u": U, "c_id": ident})
    r = run(nc, maps)
    fin = np.concatenate([r[c]["finT"] for c in range(NCORES)], 0)
    return np.ascontiguousarray(fin.transpose(1, 0, 2))


def kernel(**inp):
    inp = {k: np.asarray(v) for k, v in inp.items()}
    mods = launch_A(inp)
    rB = launch_B(inp, mods)
    h1 = gather_rows(rB, "h1"); bl = gather_rows(rB, "bl")
    affT = np.concatenate([np.concatenate([rB[b * 4 + q]["affT"] for q in range(4)], 1) for b in range(2)], 0)
    ys_all, slotm = launch_C(inp, 0, np.ascontiguousarray(affT), np.ascontiguousarray(bl.reshape(8192, D)))
    rD = launch_D(inp, False, h1, ys_all, slotm, mods, 0)
    h2 = gather_rows(rD, "h2")
    aT1 = np.stack([np.concatenate([rD[b * 4 + q]["aT1"] for q in range(4)], 1) for b in range(2)], 0)
    finT = launch_E(inp, aT1)
    rF = launch_F(inp, mods, h2, finT)
    h3 = gather_rows(rF, "h1"); bl2 = gather_rows(rF, "bl")
    affT2 = np.concatenate([np.concatenate([rF[b * 4 + q]["affT"] for q in range(4)], 1) for b in range(2)], 0)
    ys2, slotm2 = launch_C(inp, 1, np.ascontiguousarray(affT2), np.ascontiguousarray(bl2.reshape(8192, D)))
    rH = launch_D(inp, True, h3, ys2, slotm2, mods, 1)
    return gather_rows(rH, "out").astype(np.float32)
```

```python
import math
import numpy as np
from contextlib import ExitStack
import ml_dtypes
import concourse.bass as bass
import concourse.mybir as mybir
from concourse.bass_utils import run_bass_kernel_spmd

F32 = mybir.dt.float32; BF16 = mybir.dt.bfloat16; I32 = mybir.dt.int32; U32 = mybir.dt.uint32
ALU = mybir.AluOpType; AF = mybir.ActivationFunctionType; AX = mybir.AxisListType
NPBF = ml_dtypes.bfloat16
NCORES = 8
D = 4096; L = 4096; B = 2


class Buf:
    def __init__(self, t, name):
        self.t = t; self.name = name; self.w = {}; self.r = {}; self.dsem = None; self.dcnt = 0

    def __getitem__(self, k):
        return self.t[k]

    def ap(self):
        return self.t.ap()


class Eng:
    def __init__(self, h, sem, name):
        self.h = h; self.sem = sem; self.cnt = 0; self.waited = {}; self.name = name


class KB:
    def __init__(self):
        self.nc = bass.Bass("TRN2", target_bir_lowering=False)
        self.es = ExitStack()
        nc = self.nc
        self.E = {}
        for nm, h in (("pe", nc.tensor), ("act", nc.scalar), ("dve", nc.vector), ("pool", nc.gpsimd), ("sp", nc.sync)):
            self.E[nm] = Eng(h, self.es.enter_context(nc.semaphore("s_" + nm)), nm)
        self.bufs = []
        self.same_engine_sync = True

    def _reg(self, b):
        self.bufs.append(b); return b

    def dram(self, name, shape, dtype, kind="Internal"):
        return self._reg(Buf(self.nc.dram_tensor(name, list(shape), dtype, kind=kind), name))

    def sbuf(self, name, shape, dtype):
        return self._reg(Buf(self.es.enter_context(self.nc.sbuf_tensor(name, list(shape), dtype)), name))

    def psum(self, name, shape, dtype=F32):
        b = self._reg(Buf(self.es.enter_context(self.nc.psum_tensor(name, list(shape), dtype)), name))
        b.psum = True
        return b

    def _waits(self, e, reads, writes):
        deps = {}
        for b in reads:
            for s, v in b.w.items(): deps[s] = max(deps.get(s, 0), v)
        for b in writes:
            for s, v in b.w.items(): deps[s] = max(deps.get(s, 0), v)
            for s, v in b.r.items(): deps[s] = max(deps.get(s, 0), v)
        for s, v in deps.items():
            if s is e.sem and (e.name == "pe" or not self.same_engine_sync):
                continue
            if e.waited.get(s, 0) < v:
                for o in self.E.values():
                    if o.sem is s:
                        assert v <= o.cnt, f"wait on not-yet-emitted signal: {e.name} waits {o.name}>={v} (cnt {o.cnt})"
                e.h.wait_ge(s, v); e.waited[s] = v

    def op(self, eng, fn, reads=(), writes=(), sig=True):
        e = self.E[eng]
        writes = list(writes) + [b for b in reads if getattr(b, "psum", False)]
        reads = [b for b in reads if not getattr(b, "psum", False)]
        self._waits(e, reads, writes)
        ins = fn(e.h)
        if sig:
            e.cnt += 1
            ins.then_inc(e.sem, 1)
        tok = e.cnt if sig else e.cnt + 1
        for b in writes: b.w[e.sem] = max(b.w.get(e.sem, 0), tok)
        for b in reads: b.r[e.sem] = max(b.r.get(e.sem, 0), tok)
        return ins

    def dma(self, eng, fn, reads=(), writes=(), sembuf=None):
        e = self.E[eng]
        self._waits(e, reads, writes)
        sb = sembuf if sembuf is not None else (writes[0] if writes else reads[0])
        if sb.dsem is None:
            sb.dsem = self.es.enter_context(self.nc.semaphore("d_" + sb.name))
        ins = fn(e.h)
        sb.dcnt += 16
        ins.then_inc(sb.dsem, 16)
        for b in writes: b.w[sb.dsem] = sb.dcnt
        for b in reads: b.r[sb.dsem] = sb.dcnt
        return ins

    def load(self, dst, dst_ap, src, src_ap, eng="sp"):
        return self.dma(eng, lambda h: h.dma_start(out=dst_ap, in_=src_ap), reads=[src], writes=[dst], sembuf=dst)

    def store(self, dst, dst_ap, src, src_ap, eng="sp"):
        return self.dma(eng, lambda h: h.dma_start(out=dst_ap, in_=src_ap), reads=[src], writes=[dst], sembuf=src)

    def mm(self, out, out_ap, lhs, lhs_ap, rhs, rhs_ap, start, stop, extra_reads=(), sig=None):
        return self.op("pe", lambda h: h.matmul(out_ap, lhs_ap, rhs_ap, start=start, stop=stop),
                       reads=[lhs, rhs] + list(extra_reads), writes=[out], sig=(stop if sig is None else sig))

    def finish(self):
        e = self.E["sp"]
        for b in self.bufs:
            if b.dsem is not None and e.waited.get(b.dsem, 0) < b.dcnt:
                e.h.wait_ge(b.dsem, b.dcnt); e.waited[b.dsem] = b.dcnt
        for nm, o in self.E.items():
            if o.cnt > 0 and e.waited.get(o.sem, 0) < o.cnt:
                e.h.wait_ge(o.sem, o.cnt)
        self.es.close()
        return self.nc


def run(nc, in_maps):
    res = run_bass_kernel_spmd(nc, in_maps, core_ids=list(range(NCORES)))
    return res.results


def build_A():
    kb = KB()
    w = kb.dram("ada_w", [2, 4096, 3072], F32, "ExternalInput")
    bia = kb.dram("ada_b", [2, 3072], F32, "ExternalInput")
    cv = kb.dram("cv", [128, 32, 3], F32, "ExternalInput")
    out = kb.dram("mods", [2, 3, 3072], F32, "ExternalOutput")
    s = kb.sbuf("s", [128, 32, 3], F32)
    bt = kb.sbuf("bt", [3, 2, 3072], F32)
    res = kb.sbuf("res", [3, 2, 3072], F32)
    wt = [kb.sbuf(f"wt{i}", [128, 32, 512], F32) for i in range(2)]
    ps = [kb.psum(f"ps{i}", [128, 512], F32) for i in range(2)]
    kb.load(s, s[:], cv, cv.ap())
    kb.load(bt, bt[:], bia, bia.ap().partition_broadcast(3))
    kb.op("act", lambda h: h.activation(s[:], s[:], AF.Silu), reads=[s], writes=[s])
    it = 0
    for l in range(2):
        wv = w.ap()[l].rearrange("(k p) n -> p k n", p=128)
        for cb in range(6):
            t = wt[it % 2]; p = ps[it % 2]
            kb.load(t, t[:], w, wv[:, :, cb * 512:(cb + 1) * 512], eng=("sp" if it % 2 == 0 else "act"))
            for k in range(32):
                kb.mm(p, p[0:3, :], s, s[:, k, :], t, t[:, k, :], k == 0, k == 31)
            kb.op("dve", lambda h: h.tensor_tensor(res[:, l, cb * 512:(cb + 1) * 512], p[0:3, :], bt[:, l, cb * 512:(cb + 1) * 512], ALU.add),
                  reads=[p, bt], writes=[res])
            it += 1
    kb.store(out, out.ap().rearrange("l r n -> r l n"), res, res[:])
    return kb.finish()


def launch_A(inp):
    nc = build_A()
    cvec = np.concatenate([inp["c"], inp["c_ctx"][None]], 0).astype(np.float32)
    cv = np.ascontiguousarray(cvec.T.reshape(32, 128, 3).transpose(1, 0, 2))
    maps = []
    for c in range(NCORES):
        maps.append({"ada_w": np.ascontiguousarray(inp["ada_w"][:, :, c * 3072:(c + 1) * 3072]),
                     "ada_b": np.ascontiguousarray(inp["ada_b"][:, c * 3072:(c + 1) * 3072]),
                     "cv": cv})
    r = run(nc, maps)
    mods = np.concatenate([r[c]["mods"] for c in range(NCORES)], axis=2)
    return mods.reshape(2, 3, 6, D)


class Arena:
    def __init__(self, kb, name, nbytes):
        self.kb = kb
        self.t = kb.es.enter_context(kb.nc.sbuf_tensor(name, [128, nbytes // 2], BF16))
        self.nbytes = nbytes

    def view(self, name, off, shape, dtype, prev=()):
        n = 1
        for s in shape[1:]: n *= s
        esz = 2 if dtype == BF16 else 4
        assert off % 4 == 0 and off + n * esz <= self.nbytes, (name, off, n * esz, self.nbytes)
        ap = self.t[0:shape[0], off // 2: off // 2 + n * esz // 2]
        if esz == 4:
            ap = ap.bitcast(dtype)
        if len(shape) == 3:
            ap = ap.rearrange("p (a b) -> p a b", a=shape[1])
        if len(shape) == 4:
            ap = ap.rearrange("p (a b c) -> p a b c", a=shape[1], b=shape[2])
        b = Buf(ap, name)
        for o in prev:
            for s, v in list(o.w.items()) + list(o.r.items()):
                b.w[s] = max(b.w.get(s, 0), v)
        self.kb.bufs.append(b)
        return b


def psum_banks(kb):
    banks = []
    for i in range(8):
        b = kb.psum(f"ps{i}", [128, 512], F32)
        b.bf = b.t[:].bitcast(BF16)
        banks.append(b)
    return banks


IN = "ExternalInput"; OUT = "ExternalOutput"


def rms_rstd(kb, ss_ap, ss_buf, eps_buf, n):
    kb.op("act", lambda h: h.activation(ss_ap, ss_ap, AF.Sqrt, bias=eps_buf[:, 0:1], scale=1.0 / n), reads=[ss_buf, eps_buf], writes=[ss_buf])
    kb.op("dve", lambda h: h.reciprocal(ss_ap, ss_ap), reads=[ss_buf], writes=[ss_buf])


def build_B(stop=None):
    kb = KB()
    xo = kb.dram("xo", [1024, 4096], F32, IN)
    xe = kb.dram("xe", [512, 4096], F32, IN)
    gs = kb.dram("gs", [128, 5, 32], F32, IN)
    rowv = kb.dram("rowv", [4, 4096], F32, IN)
    w_in = kb.dram("w_in", [4096, 7168], F32, IN)
    w_out = kb.dram("w_out", [4096, 4096], F32, IN)
    ropec = kb.dram("ropec", [128, 1536], F32, IN)
    ropes = kb.dram("ropes", [128, 1536], F32, IN)
    cbf = kb.dram("cbf", [128, 3, 128], BF16, IN)
    masks = kb.dram("masks", [128, 4, 512], BF16, IN)
    identf = kb.dram("identf", [128, 128], F32, IN)
    sink = kb.dram("sink", [1, 16], F32, IN)
    sgu_ws = kb.dram("sgu_ws", [16, 128, 128], F32, IN)
    sgu_bs = kb.dram("sgu_bs", [1, 2048], F32, IN)
    sgu_g = kb.dram("sgu_g", [1, 2048], F32, IN)
    wr = kb.dram("wr", [128, 32, 16], F32, IN)
    h1 = kb.dram("h1", [1024, 4096], F32, OUT)
    bl = kb.dram("bl", [1024, 4096], BF16, OUT)
    affT = kb.dram("affT", [16, 1024], F32, OUT)
    catT = kb.dram("catT", [4096, 1024], BF16, OUT)
    d = dict(locals())
    mixer0_body(kb, d)
    if d.get('_stopped'):
        return kb.finish()
    ffn_front(kb, d['_ar'], d['_ps'], d['_dead'], d['_epsb'], xo, catT, w_out, rowv, wr, identf, h1, bl, affT)
    return kb.finish()


def rope_evac(kb, ps, rot_ps, cosb, cos_ap, sinb, sin_ap, rt, tmpb, t1, t2, outb, out_ap):
    kb.op("act", lambda h: h.activation(tmpb[:], ps[:], AF.Copy), reads=[ps], writes=[tmpb])
    kb.mm(rot_ps, rot_ps[:], rt, rt[:], tmpb, tmpb[:], True, True)
    kb.op("dve", lambda h: h.tensor_tensor(t1[:], ps[:], cos_ap, ALU.mult), reads=[ps, cosb], writes=[t1])
    kb.op("dve", lambda h: h.tensor_tensor(t2[:], rot_ps[:], sin_ap, ALU.mult), reads=[rot_ps, sinb], writes=[t2])
    kb.op("dve", lambda h: h.tensor_tensor(out_ap, t1[:], t2[:], ALU.add), reads=[t1, t2], writes=[outb])


def mixer0_body(kb, d):
    xo, xe, gs, w_in, ropec, ropes, cbf, masks, sink = d["xo"], d["xe"], d["gs"], d["w_in"], d["ropec"], d["ropes"], d["cbf"], d["masks"], d["sink"]
    sgu_ws, sgu_bs, sgu_g, catT, identf = d["sgu_ws"], d["sgu_bs"], d["sgu_g"], d["catT"], d["identf"]
    ps = psum_banks(kb)
    ar = Arena(kb, "arenaB", 204 * 1024)
    KBY = 1024
    off = 184 * KBY
    gsb = ar.view("gsb", off, [128, 5, 32], F32); off += 640
    GS = ar.view("GS", off, [128, 4, 32], F32); off += 512
    cb = ar.view("cb", off, [128, 3, 128], BF16); off += 768
    epsb = ar.view("epsb", off, [128, 1], F32); off += 4
    ssb = ar.view("ssb", off, [128, 16], F32); off += 64
    mk = ar.view("mk", off, [128, 4, 512], BF16); off += 4096
    esk = ar.view("esk", off, [128, 16], F32); off += 64
    eskhl = ar.view("eskhl", off, [2, 16, 128], BF16); off += 4096
    eskf = ar.view("eskf", off, [1, 16, 128], F32); off += 8192
    assert off <= 204 * KBY
    ident = Buf(cb.t[:, 0, :], "ident"); rt = Buf(cb.t[:, 1, :], "rt"); ones = Buf(cb.t[:, 2, :], "ones")
    for b_ in (ident, rt, ones): b_.w = cb.w; b_.r = cb.r
    kb.load(gsb, gsb[:], gs, gs.ap())
    kb.load(cb, cb[:], cbf, cbf.ap())
    kb.load(mk, mk[:], masks, masks.ap())
    kb.op("dve", lambda h: h.memset(epsb[:], 1e-6), writes=[epsb])
    kb.op("dve", lambda h: h.scalar_tensor_tensor(GS[:, 0, :], gsb[:, 1, :], 1.0, gsb[:, 0, :], ALU.add, ALU.mult), reads=[gsb], writes=[GS])
    kb.op("dve", lambda h: h.tensor_copy(GS[:, 1, :], gsb[:, 2, :]), reads=[gsb], writes=[GS])
    kb.op("dve", lambda h: h.scalar_tensor_tensor(GS[:, 2, :], gsb[:, 3, :], 1.0, gsb[:, 0, :], ALU.add, ALU.mult), reads=[gsb], writes=[GS])
    kb.op("dve", lambda h: h.tensor_copy(GS[:, 3, :], gsb[:, 4, :]), reads=[gsb], writes=[GS])
    kb.load(esk, esk[0:1, :], sink, sink.ap())
    kb.op("act", lambda h: h.activation(esk[0:1, :], esk[0:1, :], AF.Exp), reads=[esk], writes=[esk])
    kb.op("dve", lambda h: h.tensor_copy(eskf[0:1, :, :], esk[0:1, :].unsqueeze(2).to_broadcast([1, 16, 128])), reads=[esk], writes=[eskf])
    kb.op("dve", lambda h: h.tensor_copy(eskhl[0:1, :, :], eskf[0:1, :, :]), reads=[eskf], writes=[eskhl])
    kb.op("dve", lambda h: h.tensor_tensor(eskf[0:1, :, :], eskf[0:1, :, :], eskhl[0:1, :, :], ALU.subtract), reads=[eskf, eskhl], writes=[eskf])
    lo_tmp = ar.view("lo_tmp", 180 * KBY, [1, 16, 128], BF16)
    kb.op("dve", lambda h: h.tensor_copy(lo_tmp[0:1, :, :], eskf[0:1, :, :]), reads=[eskf], writes=[lo_tmp])
    kb.dma("sp", lambda h: h.dma_start(out=eskhl[1:2, :, :], in_=lo_tmp[0:1, :, :]), reads=[lo_tmp], writes=[eskhl], sembuf=eskhl)

    if d.get('stop') == 'C':
        d['_stopped'] = True
        return
    aT = ar.view("aT", 0, [128, 32, 1024], BF16)
    aTx = ar.view("aTx", 64 * KBY, [128, 32, 512], BF16)
    wA = ar.view("wA", 96 * KBY, [128, 32, 512], BF16)
    xt = [ar.view("xt0", 128 * KBY, [128, 4096], F32), ar.view("xt1", 144 * KBY, [128, 4096], F32)]
    xn = ar.view("xn", 160 * KBY, [128, 4096], BF16)

    def norm_tile(i, src, row0, dst, col0, gi):
        x_ = xt[i % 2]
        kb.load(x_, x_[:], src, src.ap()[row0:row0 + 128, :])
        kb.op("act", lambda h: h.activation(xn[:], x_[:], AF.Square, accum_out=ssb[:, 0:1]), reads=[x_], writes=[xn, ssb])
        rms_rstd(kb, ssb[:, 0:1], ssb, epsb, 4096)
        kb.op("act", lambda h: h.activation(xn[:], x_[:], AF.Copy, scale=ssb[:, 0:1]), reads=[x_, ssb], writes=[xn])
        for bk in range(4):
            p = ps[bk]
            for j in range(8):
                k = bk * 8 + j
                kb.op("pe", lambda h: h.transpose(p.bf[:, j * 128:(j + 1) * 128], xn[:, k * 128:(k + 1) * 128], ident[:]),
                      reads=[xn, ident], writes=[p], sig=(j == 7))
            for j in range(8):
                k = bk * 8 + j
                o_ap = dst[:, k, col0:col0 + 128]
                i_ap = p.bf[:, j * 128:(j + 1) * 128]
                if bk % 2 == 0:
                    kb.op("act", lambda h: h.activation(o_ap, i_ap, AF.Identity, bias=GS[:, gi + 1, k:k + 1], scale=GS[:, gi, k:k + 1]),
                          reads=[p, GS], writes=[dst])
                else:
                    kb.op("dve", lambda h: h.tensor_scalar(o_ap, i_ap, GS[:, gi, k:k + 1], GS[:, gi + 1, k:k + 1], ALU.mult, ALU.add),
                          reads=[p, GS], writes=[dst])
    for i in range(4):
        norm_tile(i, xe, i * 128, aTx, i * 128, 0 if i < 2 else 2)
    for i in range(8):
        norm_tile(i, xo, i * 128, aT, i * 128, 0)

    if d.get('stop') == 'B1':
        d['_stopped'] = True
        return
    r6 = 128 * KBY
    cosb = ar.view("cosb", r6, [128, 1536], F32, prev=xt + [xn]); sinb = ar.view("sinb", r6 + 6 * KBY, [128, 1536], F32, prev=xt + [xn])
    kT = ar.view("kT", r6 + 12 * KBY, [128, 4, 1536], BF16, prev=xt + [xn])
    V = ar.view("V", r6 + 24 * KBY, [128, 12, 512], BF16, prev=xt + [xn])
    tmpb = ar.view("tmpb", 164 * KBY, [128, 512], BF16, prev=[xn])
    t1 = ar.view("t1", 165 * KBY, [128, 512], F32, prev=[xn]); t2 = ar.view("t2", 167 * KBY, [128, 512], F32, prev=[xn])
    kb.load(cosb, cosb[:], ropec, ropec.ap())
    kb.load(sinb, sinb[:], ropes, ropes.ap())
    wv_in = w_in.ap().rearrange("(k p) n -> p k n", p=128)

    def wload(t, c0, eng="pool"):
        kb.dma(eng, lambda h: h.dma_start(out=t[:], in_=wv_in[:, :, c0:c0 + 512]), reads=[w_in], writes=[t], sembuf=t)

    def tok_rhs(tt):
        return (aT, lambda k: aT[:, k, tt * 512:(tt + 1) * 512]) if tt < 2 else (aTx, lambda k: aTx[:, k, :])

    def tok_lhs(t128):
        return (aT, lambda k: aT[:, k, t128 * 128:(t128 + 1) * 128]) if t128 < 8 else (aTx, lambda k: aTx[:, k, (t128 - 8) * 128:(t128 - 7) * 128])

    wload(wA, 2048)
    it = 0
    for hk in range(4):
        for tt in range(3):
            p = ps[4 + it % 2]; it += 1
            ab, af = tok_rhs(tt)
            for k in range(32):
                kb.mm(p, p[:], wA, wA[:, k, hk * 128:(hk + 1) * 128], ab, af(k), k == 0, k == 31)
            if d.get('stop') == 'B15a':
                d['_stopped'] = True
                return
            rope_evac(kb, p, ps[6], cosb, cosb[:, tt * 512:(tt + 1) * 512], sinb, sinb[:, tt * 512:(tt + 1) * 512], rt, tmpb, t1, t2,
                      kT, kT[:, hk, tt * 512:(tt + 1) * 512])
            if d.get('stop') == 'B15b':
                d['_stopped'] = True
                return
    if d.get('stop') == 'B15c':
        d['_stopped'] = True
        return
    wload(wA, 2560)
    for t128 in range(12):
        p = ps[4 + t128 % 2]
        ab, af = tok_lhs(t128)
        for k in range(32):
            kb.mm(p, p[:], ab, af(k), wA, wA[:, k, :], k == 0, k == 31)
        kb.op("act", lambda h: h.activation(V[:, t128, :], p[:], AF.Copy), reads=[p], writes=[V])

    if d.get('stop') == 'B15':
        d['_stopped'] = True
        return
    wB = ar.view("wB", 64 * KBY, [128, 32, 512], BF16, prev=[aTx])
    wbufs = [wA, wB]
    qT = ar.view("qT", 169 * KBY, [128, 4, 1024], BF16)
    Pt = [ar.view(f"P{c}", 177 * KBY + c * 1024, [128, 512], BF16, prev=[lo_tmp]) for c in range(5)]
    Oh = ar.view("Oh", 172 * KBY + 10 * KBY, [128, 4, 1024], BF16) if False else None
    Oh = [ar.view(f"Oh{g}", 120 * KBY + g * 2 * KBY, [128, 1024], BF16) for g in range(4)] if False else None
    wi = 0
    nxt = wbufs[wi % 2]; wload(nxt, 0)
    ohst = ar.view("ohst", 182 * KBY, [128, 4, 128], BF16, prev=[lo_tmp])
    for hk in range(4):
        wq = wbufs[wi % 2]; wi += 1
        if hk < 3:
            wload(wbufs[wi % 2], (hk + 1) * 512)
        for g in range(4):
            for tt in range(2):
                p = ps[4 + (g * 2 + tt) % 2]
                for k in range(32):
                    kb.mm(p, p[:], wq, wq[:, k, g * 128:(g + 1) * 128], aT, aT[:, k, tt * 512:(tt + 1) * 512], k == 0, k == 31)
                rope_evac(kb, p, ps[6], cosb, cosb[:, tt * 512:(tt + 1) * 512], sinb, sinb[:, tt * 512:(tt + 1) * 512], rt, tmpb, t1, t2,
                          qT, qT[:, g, tt * 512:(tt + 1) * 512])
        for j in range(8):
            prev_c = ((j - 1) * 128, j - 1, 0) if j >= 1 else (1024, 8, 2)
            next_c = ((j + 1) * 128, j + 1, 1) if j <= 6 else (1152, 9, 3)
            chunks = [prev_c, (j * 128, j, None), next_c, (1280, 10, None), (1408, 11, None)]
            q_ap = qT[:, :, j * 128:(j + 1) * 128]
            for c, (ko, vt, mi) in enumerate(chunks):
                sp_ = ps[c % 4]
                kb.mm(sp_, sp_[:], kT, kT[:, hk, ko:ko + 128], qT, q_ap, True, True)
                kb.op("act", lambda h: h.activation(Pt[c][:], sp_[:], AF.Exp, scale=128 ** -0.5), reads=[sp_], writes=[Pt[c]])
                if mi is not None:
                    kb.op("dve", lambda h: h.tensor_tensor(Pt[c][:], Pt[c][:], mk[:, mi, :], ALU.mult), reads=[Pt[c], mk], writes=[Pt[c]])
            o_ps = ps[4 + j % 2]; d_ps = ps[6 + j % 2]
            for c, (ko, vt, mi) in enumerate(chunks):
                kb.mm(o_ps, o_ps[:], V, V[:, vt, hk * 128:(hk + 1) * 128], Pt[c], Pt[c][:], c == 0, c == 4)
            for c in range(5):
                kb.mm(d_ps, d_ps[:], ones, ones[:], Pt[c], Pt[c][:], c == 0, False)
            kb.mm(d_ps, d_ps[:], ones, ones[0:2, :], eskhl, eskhl[0:2, hk * 4:(hk + 1) * 4, :], False, True)
            kb.op("dve", lambda h: h.reciprocal(t1[:], d_ps[:]), reads=[d_ps], writes=[t1])
            kb.op("dve", lambda h: h.tensor_tensor(ohst[:], o_ps[:], t1[:], ALU.mult), reads=[o_ps, t1], writes=[ohst])
            kb.store(catT, catT.ap()[hk * 512:(hk + 1) * 512, j * 128:(j + 1) * 128].rearrange("(g d) t -> d g t", g=4), ohst, ohst[:])

    if d.get('stop') == 'B2':
        d['_stopped'] = True
        return
    r6v = [cosb, sinb, kT, V, qT, tmpb, t1, t2] + Pt
    zg = ar.view("zg", r6, [128, 512], F32, prev=r6v); sq = ar.view("sq", r6 + 2 * KBY, [128, 512], F32, prev=r6v)
    zn = ar.view("zn", r6 + 4 * KBY, [128, 8, 512], BF16, prev=r6v)
    uT = ar.view("uT", r6 + 12 * KBY, [128, 4, 1024], BF16, prev=r6v)
    wsT = ar.view("wsT", r6 + 20 * KBY, [128, 16, 128], BF16, prev=r6v)
    bsb = ar.view("bsb", r6 + 24 * KBY, [128, 2048], F32, prev=r6v)
    sgb = ar.view("sgb", r6 + 32 * KBY, [128, 2048], F32, prev=r6v)
    wsf = ar.view("wsf", r6 + 40 * KBY, [128, 128], F32, prev=r6v)
    st4 = ar.view("st4", r6 + 41 * KBY, [128, 8], F32, prev=r6v)
    Sst = ar.view("Sst", r6 + 42 * KBY, [128, 512], BF16, prev=r6v)
    idf = ar.view("idf", r6 + 43 * KBY, [128, 128], F32, prev=r6v)
    kb.load(bsb, bsb[:], sgu_bs, sgu_bs.ap()[0, :].partition_broadcast(128))
    kb.load(sgb, sgb[:], sgu_g, sgu_g.ap()[0, :].partition_broadcast(128))
    kb.load(idf, idf[:], identf, identf.ap())
    for g in range(16):
        kb.load(wsf, wsf[:], sgu_ws, sgu_ws.ap()[g])
        p = ps[g % 2]
        kb.op("pe", lambda h: h.transpose(p[:, 0:128], wsf[:], idf[:]), reads=[wsf, idf], writes=[p])
        kb.op("act", lambda h: h.activation(wsT[:, g, :], p[:, 0:128], AF.Copy), reads=[p], writes=[wsT])
    for cbk in range(4):
        wz = wbufs[wi % 2]; wi += 1
        wu = wbufs[wi % 2]; wi += 1
        wload(wz, 5120 + cbk * 512)
        wload(wu, 3072 + cbk * 512)
        for tq in range(8):
            p = ps[tq % 2]
            for k in range(32):
                kb.mm(p, p[:], aT, aT[:, k, tq * 128:(tq + 1) * 128], wz, wz[:, k, :], k == 0, k == 31)
            kb.op("act", lambda h: h.activation(zg[:], p[:], AF.Gelu), reads=[p], writes=[zg])
            zg3 = zg[:].rearrange("p (g c) -> p g c", g=4)
            kb.op("dve", lambda h: h.tensor_reduce(st4[:, 0:4], zg3, AX.X, ALU.add), reads=[zg], writes=[st4])
            kb.op("pool", lambda h: h.tensor_tensor(sq[:], zg[:], zg[:], ALU.mult), reads=[zg], writes=[sq])
            kb.op("dve", lambda h: h.tensor_reduce(st4[:, 4:8], sq[:].rearrange("p (g c) -> p g c", g=4), AX.X, ALU.add), reads=[sq], writes=[st4])
            kb.op("dve", lambda h: h.tensor_scalar(st4[:, 0:4], st4[:, 0:4], 1.0 / 128, None, ALU.mult), reads=[st4], writes=[st4])
            kb.op("dve", lambda h: h.tensor_tensor(sq[:, 0:4], st4[:, 0:4], st4[:, 0:4], ALU.mult), reads=[st4], writes=[sq])
            kb.op("dve", lambda h: h.scalar_tensor_tensor(st4[:, 4:8], st4[:, 4:8], 1.0 / 128, sq[:, 0:4], ALU.mult, ALU.subtract), reads=[st4, sq], writes=[st4])
            kb.op("act", lambda h: h.activation(st4[:, 4:8], st4[:, 4:8], AF.Sqrt, bias=epsb[:, 0:1], scale=1.0), reads=[st4, epsb], writes=[st4])
            kb.op("dve", lambda h: h.reciprocal(st4[:, 4:8], st4[:, 4:8]), reads=[st4], writes=[st4])
            for g4 in range(4):
                kb.op("dve", lambda h: h.tensor_scalar(zg[:, g4 * 128:(g4 + 1) * 128], zg[:, g4 * 128:(g4 + 1) * 128], st4[:, g4:g4 + 1], st4[:, 4 + g4:5 + g4],
                                                        ALU.subtract, ALU.mult), reads=[zg, st4], writes=[zg])
            kb.op("pool", lambda h: h.tensor_tensor(zn[:, tq, :], zg[:], sgb[:, cbk * 512:(cbk + 1) * 512], ALU.mult), reads=[zg, sgb], writes=[zn])
        for g4 in range(4):
            for tt in range(2):
                p = ps[2 + (g4 * 2 + tt) % 2]
                for k in range(32):
                    kb.mm(p, p[:], wu, wu[:, k, g4 * 128:(g4 + 1) * 128], aT, aT[:, k, tt * 512:(tt + 1) * 512], k == 0, k == 31)
                kb.op("act", lambda h: h.activation(uT[:, g4, tt * 512:(tt + 1) * 512], p[:], AF.Gelu), reads=[p], writes=[uT])
        for g4 in range(4):
            g = cbk * 4 + g4
            for half in range(2):
                p = ps[4 + half]
                for ch in range(4):
                    tq = half * 4 + ch
                    kb.op("pe", lambda h: h.matmul(p[:, ch * 128:(ch + 1) * 128], zn[:, tq, g4 * 128:(g4 + 1) * 128], wsT[:, g, :], start=True, stop=True),
                          reads=[zn, wsT], writes=[p], sig=(ch == 3))
                p3 = p[:].rearrange("p (c q) -> p c q", c=4)
                kb.op("dve", lambda h: h.tensor_tensor(sq[:].rearrange("p (c q) -> p c q", c=4), p3,
                                                        bsb[:, g * 128:(g + 1) * 128].unsqueeze(1).to_broadcast([128, 4, 128]), ALU.add),
                      reads=[p, bsb], writes=[sq])
                kb.op("dve", lambda h: h.tensor_tensor(Sst[:], sq[:], uT[:, g4, half * 512:(half + 1) * 512], ALU.mult), reads=[sq, uT], writes=[Sst])
                kb.store(catT, catT.ap()[2048 + g * 128:2048 + (g + 1) * 128, half * 512:(half + 1) * 512], Sst, Sst[:])
    if d.get('stop') == 'B3':
        d['_stopped'] = True
        return
    d["_ar"] = ar; d["_ps"] = ps; d["_dead"] = [aT, aTx, wA, wB, zg, sq, zn, uT, wsT, bsb, sgb, wsf, st4, Sst, idf, xt[0], xt[1], xn, ohst, qT, kT, V, cosb, sinb, t1, t2, tmpb] + Pt
    d["_epsb"] = epsb


def ffn_front(kb, ar, ps, dead, epsb, xres, catT, w_out, rowv, wr, identf, h1, bl, affT):
    KBY = 1024
    cT = ar.view("cT", 0, [128, 32, 1024], BF16, prev=dead)
    wo = [ar.view("wo0", 64 * KBY, [128, 32, 512], BF16, prev=dead), ar.view("wo1", 96 * KBY, [128, 32, 512], BF16, prev=dead)]
    r6 = 128 * KBY
    m2b = ar.view("m2b", r6, [128, 4096], F32, prev=dead)
    xs = [ar.view(f"xs{i}", r6 + 16 * KBY + i * 2 * KBY, [128, 512], F32, prev=dead) for i in range(2)]
    hs = [ar.view(f"hs{i}", r6 + 20 * KBY + i * 2 * KBY, [128, 512], F32, prev=dead) for i in range(2)]
    ssq = ar.view("ssq", r6 + 24 * KBY, [128, 8, 8], F32, prev=dead)
    junk = ar.view("junk", r6 + 25 * KBY, [128, 512], BF16, prev=dead)
    ssum = ar.view("ssum", r6 + 26 * KBY, [128, 8], F32, prev=dead)
    ones16 = ar.view("ones16", r6 + 26 * KBY + 64, [16, 16], F32, prev=dead)
    ex = ar.view("ex", r6 + 27 * KBY, [16, 128], F32, prev=dead)
    affs = ar.view("affs", r6 + 28 * KBY, [16, 1024], F32, prev=dead)
    wrb = ar.view("wrb", r6 + 32 * KBY, [128, 32, 16], F32, prev=dead)
    idf = ar.view("idf2", r6 + 34 * KBY, [128, 128], F32, prev=dead)
    kb.load(cT, cT[:], catT, catT.ap().rearrange("(k p) t -> p k t", p=128))
    kb.load(m2b, m2b[:], rowv, rowv.ap()[0, :].partition_broadcast(128))
    kb.load(wrb, wrb[:], wr, wr.ap())
    kb.load(idf, idf[:], identf, identf.ap())
    kb.op("dve", lambda h: h.memset(ones16[:], 1.0), writes=[ones16])
    wov = w_out.ap().rearrange("(k p) n -> p k n", p=128)

    def wload(t, c0):
        kb.dma("pool", lambda h: h.dma_start(out=t[:], in_=wov[:, :, c0:c0 + 512]), reads=[w_out], writes=[t], sembuf=t)
    wload(wo[0], 0)
    it = 0
    for ct in range(8):
        w = wo[ct % 2]
        if ct < 7:
            wload(wo[(ct + 1) % 2], (ct + 1) * 512)
        for tq in range(8):
            p = ps[it % 4]; x_ = xs[it % 2]; h_ = hs[it % 2]; it += 1
            kb.load(x_, x_[:], xres, xres.ap()[tq * 128:(tq + 1) * 128, ct * 512:(ct + 1) * 512])
            for k in range(32):
                kb.mm(p, p[:], cT, cT[:, k, tq * 128:(tq + 1) * 128], w, w[:, k, :], k == 0, k == 31)
            kb.op("dve", lambda h: h.tensor_tensor(h_[:], p[:], m2b[:, ct * 512:(ct + 1) * 512], ALU.mult), reads=[p, m2b], writes=[h_])
            kb.op("dve", lambda h: h.tensor_tensor(h_[:], h_[:], x_[:], ALU.add), reads=[h_, x_], writes=[h_])
            kb.op("act", lambda h: h.activation(junk[:], h_[:], AF.Square, accum_out=ssq[:, tq, ct:ct + 1]), reads=[h_], writes=[junk, ssq])
            kb.store(h1, h1.ap()[tq * 128:(tq + 1) * 128, ct * 512:(ct + 1) * 512], h_, h_[:])
    kb.op("dve", lambda h: h.tensor_reduce(ssum[:], ssq[:], AX.X, ALU.add), reads=[ssq], writes=[ssum])
    rms_rstd(kb, ssum[:], ssum, epsb, 4096)
    G4 = ar.view("G4", 0, [128, 4096], F32, prev=[cT]); S3 = ar.view("S3", 16 * KBY, [128, 4096], F32, prev=[cT])
    gf = ar.view("gf", 32 * KBY, [128, 4096], F32, prev=[cT]); m4 = ar.view("m4", 48 * KBY, [128, 4096], F32, prev=[cT])
    kb.load(gf, gf[:], rowv, rowv.ap()[1, :].partition_broadcast(128))
    kb.load(m4, m4[:], rowv, rowv.ap()[2, :].partition_broadcast(128))
    kb.load(S3, S3[:], rowv, rowv.ap()[3, :].partition_broadcast(128))
    kb.op("dve", lambda h: h.scalar_tensor_tensor(G4[:], m4[:], 1.0, gf[:], ALU.add, ALU.mult), reads=[m4, gf], writes=[G4])
    ht = ar.view("ht", 64 * KBY, [128, 4096], F32, prev=wo); bt = ar.view("bt", 80 * KBY, [128, 4096], F32, prev=wo)
    bb = ar.view("bb", 96 * KBY, [128, 4096], BF16, prev=wo); bT = ar.view("bT", 104 * KBY, [128, 32, 128], F32, prev=wo)
    for tq in range(8):
        kb.load(ht, ht[:], h1, h1.ap()[tq * 128:(tq + 1) * 128, :])
        kb.op("act", lambda h: h.activation(bt[:], ht[:], AF.Copy, scale=ssum[:, tq:tq + 1]), reads=[ht, ssum], writes=[bt])
        kb.op("dve", lambda h: h.tensor_tensor(bt[:], bt[:], G4[:], ALU.mult), reads=[bt, G4], writes=[bt])
        kb.op("pool", lambda h: h.tensor_tensor(bt[:], bt[:], S3[:], ALU.add), reads=[bt, S3], writes=[bt])
        kb.op("act", lambda h: h.activation(bb[:], bt[:], AF.Copy), reads=[bt], writes=[bb])
        kb.store(bl, bl.ap()[tq * 128:(tq + 1) * 128, :], bb, bb[:])
        for bk in range(8):
            p = ps[bk]
            for j in range(4):
                k = bk * 4 + j
                kb.op("pe", lambda h: h.transpose(p[:, j * 128:(j + 1) * 128], bt[:, k * 128:(k + 1) * 128], idf[:]), reads=[bt, idf], writes=[p], sig=(j == 3))
            o_ap = bT[:, bk * 4:(bk + 1) * 4, :]
            if bk % 2 == 0:
                kb.op("act", lambda h: h.activation(o_ap, p[:].rearrange("p (a b) -> p a b", a=4), AF.Copy), reads=[p], writes=[bT])
            else:
                kb.op("dve", lambda h: h.tensor_copy(o_ap, p[:].rearrange("p (a b) -> p a b", a=4)), reads=[p], writes=[bT])
        lg = ps[tq % 2]
        for k in range(32):
            kb.mm(lg, lg[0:16, 0:128], wrb, wrb[:, k, :], bT, bT[:, k, :], k == 0, k == 31)
        kb.op("act", lambda h: h.activation(ex[:], lg[0:16, 0:128], AF.Exp), reads=[lg], writes=[ex])
        sm = ps[2 + tq % 2]
        kb.mm(sm, sm[0:16, 0:128], ones16, ones16[:], ex, ex[:], True, True)
        kb.op("dve", lambda h: h.reciprocal(affs[:, tq * 128:(tq + 1) * 128], sm[0:16, 0:128]), reads=[sm], writes=[affs])
        kb.op("dve", lambda h: h.tensor_tensor(affs[:, tq * 128:(tq + 1) * 128], affs[:, tq * 128:(tq + 1) * 128], ex[:], ALU.mult), reads=[ex, affs], writes=[affs])
    kb.store(affT, affT.ap(), affs, affs[:])


def fm(v):
    return np.ascontiguousarray(np.asarray(v, np.float32).reshape(32, 128).T)


def consts_B(q):
    ident = np.eye(128, dtype=np.float32)
    R = np.zeros((128, 128), np.float32)
    for half in (0, 64):
        for i in range(32):
            R[half + i, half + i + 32] = -1.0
            R[half + i + 32, half + i] = 1.0
    cbf = np.stack([ident, R.T, np.ones((128, 128), np.float32)], 1).astype(NPBF)
    kk = np.arange(128)[:, None]; qq = np.arange(128)[None, :]
    m0 = (kk >= qq).astype(np.float32); m1 = (kk <= qq).astype(np.float32)
    ms = np.stack([m0, m1, m0 * (1.0 if q > 0 else 0.0), m1 * (1.0 if q < 3 else 0.0)], 0)
    masks = np.ascontiguousarray(np.tile(ms[:, :, None, :], (1, 1, 4, 1)).reshape(4, 128, 512).transpose(1, 0, 2)).astype(NPBF)
    t0 = 1024 * q
    tok = np.concatenate([np.arange(t0, t0 + 1024), np.arange(t0 - 128, t0), np.arange(t0 + 1024, t0 + 1152)])
    row = (tok // 64).astype(np.float32); col = (tok % 64).astype(np.float32)
    inv = (10000.0 ** (-np.arange(0, 64, 2, dtype=np.float32) / 64)).astype(np.float32)
    ang = np.zeros((128, 1280), np.float32)
    for d in range(128):
        pos = row if d < 64 else col
        ang[d] = pos * inv[d % 32]
    cosT = np.concatenate([np.cos(ang), np.ones((128, 256), np.float32)], 1).astype(np.float32)
    sinT = np.concatenate([np.sin(ang), np.zeros((128, 256), np.float32)], 1).astype(np.float32)
    return cbf, masks, cosT, sinT, ident


def launch_B(inp, mods, stop=None):
    nc = build_B(stop)
    maps = []
    x = inp["x"]; ctx = inp["ctx"]
    w_in = np.ascontiguousarray(inp["attn_sgu_w_in"][0]); w_out = np.ascontiguousarray(inp["attn_sgu_w_out"][0])
    wr = np.ascontiguousarray(np.asarray(inp["router_w"][0], np.float32).reshape(32, 128, 16).transpose(1, 0, 2))
    for c in range(NCORES):
        b, q = c // 4, c % 4
        t0 = 1024 * q
        z = np.zeros((128, D), np.float32)
        hp = x[b, t0 - 128:t0] if q > 0 else z
        hn = x[b, t0 + 1024:t0 + 1152] if q < 3 else z
        cbf, masks, cosT, sinT, ident = consts_B(q)
        gs = np.stack([fm(inp["norm_mix_g"][0]), fm(mods[0, b, 1]), fm(mods[0, b, 0]), fm(mods[0, 2, 1]), fm(mods[0, 2, 0])], 1)
        rowv = np.stack([mods[0, b, 2], inp["norm_ffn_g"][0], mods[0, b, 4], mods[0, b, 3]], 0).astype(np.float32)
        maps.append({
            "xo": np.ascontiguousarray(x[b, t0:t0 + 1024]), "xe": np.ascontiguousarray(np.concatenate([hp, hn, ctx[b]], 0)),
            "gs": np.ascontiguousarray(gs), "rowv": np.ascontiguousarray(rowv), "w_in": w_in, "w_out": w_out,
            "ropec": cosT, "ropes": sinT, "cbf": cbf, "masks": masks, "identf": ident,
            "sink": np.asarray(inp["attn_sink"][0], np.float32).reshape(1, 16),
            "sgu_ws": np.ascontiguousarray(inp["sgu_w_s"][0]), "sgu_bs": np.asarray(inp["sgu_b_s"][0], np.float32).reshape(1, 2048),
            "sgu_g": np.asarray(inp["sgu_norm_g"][0], np.float32).reshape(1, 2048), "wr": wr})
    r = run(nc, maps)
    return r


def build_C():
    kb = KB()
    affd = kb.dram("aff", [32, 4096], F32, IN)
    bld = kb.dram("bl", [8192, 4096], BF16, IN)
    wg = kb.dram("wg", [2, 4096, 1024], F32, IN)
    wu = kb.dram("wu", [2, 4096, 1024], F32, IN)
    wd = kb.dram("wd", [2, 1024, 4096], F32, IN)
    seld = kb.dram("sel", [32, 4], F32, IN)
    iotad = kb.dram("iota", [128, 512], F32, IN)
    rowidd = kb.dram("rowid", [128, 64], F32, IN)
    identd = kb.dram("ident", [128, 128], BF16, IN)
    ysd = kb.dram("ys", [4, 513, 4096], BF16, OUT)
    slotd = kb.dram("slotm", [32, 4096], F32, OUT)
    ps = psum_banks(kb)
    ar = Arena(kb, "arenaC", 204 * 1024)
    KBY = 1024
    aff = ar.view("aff", 0, [32, 4096], F32)
    msk = ar.view("msk", 16 * KBY, [32, 4096], F32)
    cum = ar.view("cum", 32 * KBY, [32, 4096], F32)
    onesr = ar.view("onesr", 48 * KBY, [32, 4096], F32)
    sm = ar.view("sm", 64 * KBY, [32, 16], F32)
    sel = ar.view("sel", 64 * KBY + 64, [32, 4], F32)
    kb.load(aff, aff[:], affd, affd.ap())
    kb.load(sel, sel[:], seld, seld.ap())
    lo, hi, mid, cnt, ge, tmp = (sm[:, i:i + 1] for i in range(6))
    kb.op("dve", lambda h: h.memset(sm[:], 0.0), writes=[sm])
    kb.op("dve", lambda h: h.memset(sm[:, 1:2], 1.0), writes=[sm])
    kb.op("dve", lambda h: h.memset(onesr[:], 1.0), writes=[onesr])
    for it in range(30):
        kb.op("dve", lambda h: h.scalar_tensor_tensor(mid, lo, 1.0, hi, ALU.mult, ALU.add), reads=[sm], writes=[sm])
        kb.op("dve", lambda h: h.tensor_scalar(mid, mid, 0.5, None, ALU.mult), reads=[sm], writes=[sm])
        kb.op("dve", lambda h: h.tensor_scalar(msk[:], aff[:], mid, None, ALU.is_gt), reads=[aff, sm], writes=[msk])
        kb.op("dve", lambda h: h.tensor_reduce(cnt, msk[:], AX.X, ALU.add), reads=[msk], writes=[sm])
        kb.op("dve", lambda h: h.tensor_scalar(ge, cnt, 511.5, None, ALU.is_gt), reads=[sm], writes=[sm])
        kb.op("dve", lambda h: h.tensor_tensor(tmp, mid, lo, ALU.subtract), reads=[sm], writes=[sm])
        kb.op("dve", lambda h: h.scalar_tensor_tensor(lo, tmp, ge, lo, ALU.mult, ALU.add), reads=[sm], writes=[sm])
        kb.op("dve", lambda h: h.tensor_tensor(tmp, hi, mid, ALU.subtract), reads=[sm], writes=[sm])
        kb.op("dve", lambda h: h.scalar_tensor_tensor(hi, tmp, ge, mid, ALU.mult, ALU.add), reads=[sm], writes=[sm])
    kb.op("dve", lambda h: h.tensor_scalar(msk[:], aff[:], lo, None, ALU.is_gt), reads=[aff, sm], writes=[msk])
    kb.op("dve", lambda h: h.tensor_tensor_scan(cum[:], onesr[:], msk[:], 0.0, ALU.mult, ALU.add), reads=[onesr, msk], writes=[cum])
    kb.op("dve", lambda h: h.tensor_scalar(cum[:], cum[:], -513.0, None, ALU.add), reads=[cum], writes=[cum])
    kb.op("dve", lambda h: h.tensor_tensor(cum[:], cum[:], msk[:], ALU.mult), reads=[cum, msk], writes=[cum])
    kb.op("dve", lambda h: h.tensor_scalar(cum[:], cum[:], 512.0, 512.0, ALU.add, ALU.min), reads=[cum], writes=[cum])
    kb.store(slotd, slotd.ap(), cum, cum[:])

    r1 = 65 * KBY
    stT = ar.view("stT", r1, [128, 32, 8], F32)
    iota = ar.view("iota", r1 + 1 * KBY, [128, 512], F32)
    rowid = ar.view("rowid", r1 + 3 * KBY, [128, 64], F32)
    ident = ar.view("identc", r1 + 4 * KBY, [128, 128], BF16)
    tv = ar.view("tv", r1 + 5 * KBY, [128, 32, 2], F32)
    oh = [ar.view(f"oh{i}", r1 + 6 * KBY + i * 2 * KBY, [128, 512], F32) for i in range(2)]
    idxf = ar.view("idxf", r1 + 10 * KBY, [128, 4, 2], F32)
    idxi = [ar.view(f"idxi{j}", r1 + 10 * KBY + 64 + j * 16, [128, 4], I32) for j in range(4)]
    gat = [ar.view(f"gat{j}", r1 + 10 * KBY + 128 + j * 16, [128, 4], F32) for j in range(4)]
    zrow = ar.view("zrow", r1 + 11 * KBY, [1, 4096], BF16)
    kb.load(iota, iota[:], iotad, iotad.ap())
    kb.load(rowid, rowid[:], rowidd, rowidd.ap())
    kb.load(ident, ident[:], identd, identd.ap())
    kb.op("dve", lambda h: h.memset(zrow[:], 0.0), writes=[zrow])
    for j in range(4):
        kb.store(ysd, ysd.ap()[j, 512:513, :], zrow, zrow[:])
    for ti in range(32):
        p = ps[ti % 2]
        kb.mm(p, p[:, 0:4], cum, cum[:, ti * 128:(ti + 1) * 128], sel, sel[:], True, True)
        kb.mm(p, p[:, 4:8], aff, aff[:, ti * 128:(ti + 1) * 128], sel, sel[:], True, True)
        kb.op("act", lambda h: h.activation(stT[:, ti, :], p[:, 0:8], AF.Copy), reads=[p], writes=[stT])
    for j in range(4):
        b = j % 2
        kb.op("dve", lambda h: h.tensor_copy(tv[:, :, 0], rowid[:, b * 32:(b + 1) * 32]), reads=[rowid], writes=[tv])
        kb.op("dve", lambda h: h.tensor_copy(tv[:, :, 1], stT[:, :, 4 + j]), reads=[stT], writes=[tv])
        ip = ps[2 + j % 2]
        for ti in range(32):
            o_ = oh[ti % 2]
            kb.op("dve", lambda h: h.tensor_scalar(o_[:], iota[:], stT[:, ti, j:j + 1], None, ALU.is_equal), reads=[iota, stT], writes=[o_])
            for sc in range(4):
                kb.op("pe", lambda h: h.matmul(ip[:, sc * 2:sc * 2 + 2], o_[:, sc * 128:(sc + 1) * 128], tv[:, ti, :], start=(ti == 0 and sc == 0), stop=(ti == 31),
                                               skip_group_check=True),
                      reads=[o_, tv], writes=[ip], sig=(sc == 3))
        kb.op("dve", lambda h: h.tensor_copy(idxf[:], ip[:, 0:8].rearrange("p (a b) -> p a b", a=4)), reads=[ip], writes=[idxf])
        kb.op("dve", lambda h: h.tensor_copy(idxi[j][:], idxf[:, :, 0]), reads=[idxf], writes=[idxi[j]])
        kb.op("dve", lambda h: h.tensor_copy(gat[j][:], idxf[:, :, 1]), reads=[idxf], writes=[gat[j]])

    route_dead = [aff, msk, cum, onesr, stT, iota, tv, oh[0], oh[1], rowid]
    xs = ar.view("xs", 84 * KBY, [128, 4096], BF16)
    xsT = [ar.view(f"xsT{b}", 92 * KBY + b * 32 * KBY, [128, 32, 512], BF16) for b in range(2)]
    actT = [ar.view(f"actT{b}", 156 * KBY + b * 8 * KBY, [128, 8, 512], BF16) for b in range(2)]
    wb = [ar.view("wb0", 0, [128, 32, 512], BF16, prev=route_dead), ar.view("wb1", 32 * KBY, [128, 32, 512], BF16, prev=route_dead),
          ar.view("wb2", 172 * KBY, [128, 32, 512], BF16)]
    wbd = [ar.t[:, 0:16384].rearrange("p (k n) -> p k n", k=8), ar.t[:, 16384:32768].rearrange("p (k n) -> p k n", k=8),
           ar.t[:, 86 * 1024:86 * 1024 + 16384].rearrange("p (k n) -> p k n", k=8)]
    sgt = ar.view("sgt", r1, [128, 512], F32, prev=route_dead)
    yst = [ar.view(f"yst{i}", r1 + 2 * KBY + i * KBY, [128, 512], BF16, prev=route_dead) for i in range(2)]
    wi = 0
    for el in range(2):
        for b in range(2):
            j = el * 2 + b
            for sc in range(4):
                kb.dma("pool", lambda h: h.indirect_dma_start(out=xs[:], out_offset=None, in_=bld.ap(),
                                                              in_offset=bass.IndirectOffsetOnAxis(ap=idxi[j][:, sc:sc + 1], axis=0)),
                       reads=[bld, idxi[j]], writes=[xs], sembuf=xs)
                for bk in range(4):
                    p = ps[4 + bk]
                    for q in range(8):
                        k = bk * 8 + q
                        kb.op("pe", lambda h: h.transpose(p.bf[:, q * 128:(q + 1) * 128], xs[:, k * 128:(k + 1) * 128], ident[:]), reads=[xs, ident], writes=[p], sig=(q == 7))
                    o_ap = xsT[b][:, bk * 8:(bk + 1) * 8, sc * 128:(sc + 1) * 128]
                    i_ap = p.bf[:].rearrange("p (a b) -> p a b", a=8)
                    if bk % 2 == 0:
                        kb.op("act", lambda h: h.activation(o_ap, i_ap, AF.Copy), reads=[p], writes=[xsT[b]])
                    else:
                        kb.op("dve", lambda h: h.tensor_copy(o_ap, i_ap), reads=[p], writes=[xsT[b]])
        wgv = wg.ap()[el].rearrange("(k p) n -> p k n", p=128)
        wuv = wu.ap()[el].rearrange("(k p) n -> p k n", p=128)
        wdv = wd.ap()[el].rearrange("(k p) n -> p k n", p=128)
        it = 0
        for hf in range(2):
            gi = wi % 3; wi += 1
            ui = wi % 3; wi += 1
            kb.dma("pool", lambda h: h.dma_start(out=wb[gi][:], in_=wgv[:, :, hf * 512:(hf + 1) * 512]), reads=[wg], writes=[wb[gi]], sembuf=wb[gi])
            kb.dma("pool", lambda h: h.dma_start(out=wb[ui][:], in_=wuv[:, :, hf * 512:(hf + 1) * 512]), reads=[wu], writes=[wb[ui]], sembuf=wb[ui])
            for b in range(2):
                for f4 in range(4):
                    gp = ps[(it * 2) % 4]; up = ps[(it * 2 + 1) % 4]; it += 1
                    for k in range(32):
                        kb.mm(gp, gp[:], wb[gi], wb[gi][:, k, f4 * 128:(f4 + 1) * 128], xsT[b], xsT[b][:, k, :], k == 0, k == 31)
                    for k in range(32):
                        kb.mm(up, up[:], wb[ui], wb[ui][:, k, f4 * 128:(f4 + 1) * 128], xsT[b], xsT[b][:, k, :], k == 0, k == 31)
                    kb.op("act", lambda h: h.activation(sgt[:], gp[:], AF.Silu), reads=[gp], writes=[sgt])
                    kb.op("dve", lambda h: h.tensor_tensor(actT[b][:, hf * 4 + f4, :], sgt[:], up[:], ALU.mult), reads=[sgt, up], writes=[actT[b]])
        it = 0
        for dq in range(2):
            di = wi % 3; wi += 1
            kb.dma("pool", lambda h: h.dma_start(out=wbd[di], in_=wdv[:, :, dq * 2048:(dq + 1) * 2048]), reads=[wd], writes=[wb[di]], sembuf=wb[di])
            for b in range(2):
                j = el * 2 + b
                for sc in range(4):
                    for c4 in range(4):
                        yp = ps[4 + it % 4]; ys_ = yst[it % 2]; it += 1
                        for k in range(8):
                            kb.mm(yp, yp[:], actT[b], actT[b][:, k, sc * 128:(sc + 1) * 128], wb[di], wbd[di][:, k, c4 * 512:(c4 + 1) * 512], k == 0, k == 7)
                        kb.op("act", lambda h: h.activation(ys_[:], yp[:], AF.Copy, scale=gat[j][:, sc:sc + 1]), reads=[yp, gat[j]], writes=[ys_])
                        c0 = dq * 2048 + c4 * 512
                        kb.store(ysd, ysd.ap()[j, sc * 128:(sc + 1) * 128, c0:c0 + 512], ys_, ys_[:])
    return kb.finish()


def launch_C(inp, layer, affT_all, bl_all):
    nc = build_C()
    iota = np.tile(np.arange(512, dtype=np.float32)[None], (128, 1))
    rowid = (np.arange(64, dtype=np.float32)[None, :] * 128 + np.arange(128, dtype=np.float32)[:, None]).astype(np.float32)
    ident = np.eye(128, dtype=np.float32).astype(NPBF)
    maps = []
    for c in range(NCORES):
        sel = np.zeros((32, 4), np.float32)
        for el in range(2):
            for b in range(2):
                sel[b * 16 + 2 * c + el, el * 2 + b] = 1.0
        maps.append({"aff": affT_all, "bl": bl_all,
                     "wg": np.ascontiguousarray(inp["expert_w_gate"][layer, 2 * c:2 * c + 2]),
                     "wu": np.ascontiguousarray(inp["expert_w_up"][layer, 2 * c:2 * c + 2]),
                     "wd": np.ascontiguousarray(inp["expert_w_down"][layer, 2 * c:2 * c + 2]),
                     "sel": sel, "iota": iota, "rowid": rowid, "ident": ident})
    r = run(nc, maps)
    ys_all = np.zeros((2, 16, 513, D), NPBF)
    for c in range(NCORES):
        for el in range(2):
            for b in range(2):
                ys_all[b, 2 * c + el] = r[c]["ys"][el * 2 + b]
    return ys_all, r[0]["slotm"]


def build_D(final):
    kb = KB()
    hin = kb.dram("hin", [1024, 4096], F32, IN)
    ysf = kb.dram("ysf", [16 * 513, 4096], BF16, IN)
    slotd = kb.dram("slot", [16, 1024], F32, IN)
    rows = kb.dram("rows", [2, 4096], F32, IN)
    cf = kb.dram("cf", [128, 32], F32, IN)
    identd = kb.dram("ident", [128, 128], BF16, IN)
    if final:
        outd = kb.dram("out", [1024, 4096], F32, OUT)
    else:
        gsd = kb.dram("gs", [128, 3, 32], F32, IN)
        h2d = kb.dram("h2", [1024, 4096], F32, OUT)
        aTd = kb.dram("aT1", [4096, 1024], BF16, OUT)
    ps = psum_banks(kb)
    ar = Arena(kb, "arenaD", 204 * 1024)
    KBY = 1024
    G = [ar.view(f"G{i}", i * 8 * KBY, [128, 4096], BF16) for i in range(4)]
    hi_ = ar.view("hi", 32 * KBY, [128, 4096], F32)
    h2t = ar.view("h2t", 48 * KBY, [128, 4096], F32)
    m5b = ar.view("m5b", 64 * KBY, [128, 4096], F32)
    xn = ar.view("xn", 80 * KBY, [128, 4096], BF16)
    big = ar.view("big", 88 * KBY, [128, 32, 1024], BF16) if not final else ar.view("gfin", 88 * KBY, [128, 4096], F32)
    slot = ar.view("slot", 152 * KBY, [16, 1024], F32)
    cfb = ar.view("cfb", 156 * KBY, [128, 32], F32)
    ident = ar.view("identd", 156 * KBY + 128, [128, 128], BF16)
    posf = ar.view("posf", 157 * KBY, [128, 16], F32)
    posi = ar.view("posi", 157 * KBY + 64, [128, 16], I32)
    ssb = ar.view("ssb", 157 * KBY + 128, [128, 8], F32)
    epsb = ar.view("epsb", 157 * KBY + 160, [128, 1], F32)
    GS = ar.view("GS", 158 * KBY, [128, 2, 32], F32)
    gsb = ar.view("gsb", 158 * KBY + 256, [128, 3, 32], F32)
    kb.load(slot, slot[:], slotd, slotd.ap())
    kb.load(cfb, cfb[:], cf, cf.ap())
    kb.load(ident, ident[:], identd, identd.ap())
    kb.load(m5b, m5b[:], rows, rows.ap()[0, :].partition_broadcast(128))
    kb.op("dve", lambda h: h.memset(epsb[:], 1e-6), writes=[epsb])
    if final:
        kb.load(big, big[:], rows, rows.ap()[1, :].partition_broadcast(128))
    else:
        kb.load(gsb, gsb[:], gsd, gsd.ap())
        kb.op("dve", lambda h: h.scalar_tensor_tensor(GS[:, 0, :], gsb[:, 1, :], 1.0, gsb[:, 0, :], ALU.add, ALU.mult), reads=[gsb], writes=[GS])
        kb.op("dve", lambda h: h.tensor_copy(GS[:, 1, :], gsb[:, 2, :]), reads=[gsb], writes=[GS])
    gi = 0
    for tq in range(8):
        pp = ps[0]
        kb.mm(pp, pp[:, 0:16], slot, slot[0:16, tq * 128:(tq + 1) * 128], cfb, cfb[0:16, 0:16], True, True)
        kb.op("dve", lambda h: h.tensor_tensor(posf[:], pp[:, 0:16], cfb[:, 16:32], ALU.add), reads=[pp, cfb], writes=[posf])
        kb.op("dve", lambda h: h.tensor_copy(posi[:], posf[:]), reads=[posf], writes=[posi])
        kb.load(hi_, hi_[:], hin, hin.ap()[tq * 128:(tq + 1) * 128, :])
        for e in range(16):
            g_ = G[gi % 4]; gi += 1
            kb.dma("pool", lambda h: h.indirect_dma_start(out=g_[:], out_offset=None, in_=ysf.ap(),
                                                          in_offset=bass.IndirectOffsetOnAxis(ap=posi[:, e:e + 1], axis=0)),
                   reads=[ysf, posi], writes=[g_], sembuf=g_)
            for ct in range(8):
                kb.mm(ps[ct], ps[ct][:], ident, ident[:], g_, g_[:, ct * 512:(ct + 1) * 512], e == 0, e == 15, sig=(ct == 7 or e == 15))
        for ct in range(8):
            sl = slice(ct * 512, (ct + 1) * 512)
            kb.op("dve", lambda h: h.tensor_tensor(h2t[:, sl], ps[ct][:], m5b[:, sl], ALU.mult), reads=[ps[ct], m5b], writes=[h2t])
        kb.op("dve", lambda h: h.tensor_tensor(h2t[:], h2t[:], hi_[:], ALU.add), reads=[h2t, hi_], writes=[h2t])
        kb.op("act", lambda h: h.activation(xn[:], h2t[:], AF.Square, accum_out=ssb[:, 0:1]), reads=[h2t], writes=[xn, ssb])
        rms_rstd(kb, ssb[:, 0:1], ssb, epsb, 4096)
        if final:
            kb.op("act", lambda h: h.activation(hi_[:], h2t[:], AF.Copy, scale=ssb[:, 0:1]), reads=[h2t, ssb], writes=[hi_])
            kb.op("dve", lambda h: h.tensor_tensor(hi_[:], hi_[:], big[:], ALU.mult), reads=[hi_, big], writes=[hi_])
            kb.store(outd, outd.ap()[tq * 128:(tq + 1) * 128, :], hi_, hi_[:])
        else:
            kb.store(h2d, h2d.ap()[tq * 128:(tq + 1) * 128, :], h2t, h2t[:])
            kb.op("act", lambda h: h.activation(xn[:], h2t[:], AF.Copy, scale=ssb[:, 0:1]), reads=[h2t, ssb], writes=[xn])
            for bk in range(4):
                p = ps[bk]
                for j in range(8):
                    k = bk * 8 + j
                    kb.op("pe", lambda h: h.transpose(p.bf[:, j * 128:(j + 1) * 128], xn[:, k * 128:(k + 1) * 128], ident[:]), reads=[xn, ident], writes=[p], sig=(j == 7))
                for j in range(8):
                    k = bk * 8 + j
                    o_ap = big[:, k, tq * 128:(tq + 1) * 128]; i_ap = p.bf[:, j * 128:(j + 1) * 128]
                    if bk % 2 == 0:
                        kb.op("act", lambda h: h.activation(o_ap, i_ap, AF.Identity, bias=GS[:, 1, k:k + 1], scale=GS[:, 0, k:k + 1]), reads=[p, GS], writes=[big])
                    else:
                        kb.op("dve", lambda h: h.tensor_scalar(o_ap, i_ap, GS[:, 0, k:k + 1], GS[:, 1, k:k + 1], ALU.mult, ALU.add), reads=[p, GS], writes=[big])
    if not final:
        kb.store(aTd, aTd.ap().rearrange("(k p) t -> p k t", p=128), big, big[:])
    return kb.finish()


def launch_D(inp, final, h_in, ys_all, slotm, mods, layer):
    nc = build_D(final)
    cf = np.zeros((128, 32), np.float32)
    cf[0:16, 0:16] = np.eye(16, dtype=np.float32)
    cf[:, 16:32] = (np.arange(16, dtype=np.float32) * 513)[None, :]
    ident = np.eye(128, dtype=np.float32).astype(NPBF)
    maps = []
    for c in range(NCORES):
        b, q = c // 4, c % 4
        m = {"hin": np.ascontiguousarray(h_in[b, q * 1024:(q + 1) * 1024]), "ysf": ys_all[b].reshape(16 * 513, D),
             "slot": np.ascontiguousarray(slotm[b * 16:(b + 1) * 16, q * 1024:(q + 1) * 1024]),
             "rows": np.stack([mods[layer, b, 5], np.asarray(inp["final_norm_g"], np.float32)], 0).astype(np.float32),
             "cf": cf, "ident": ident}
        if not final:
            m["gs"] = np.ascontiguousarray(np.stack([fm(inp["norm_mix_g"][1]), fm(mods[1, b, 1]), fm(mods[1, b, 0])], 1))
        maps.append(m)
    return run(nc, maps)


def build_F():
    kb = KB()
    xres = kb.dram("xres", [1024, 4096], F32, IN)
    catT = kb.dram("catT", [4096, 1024], BF16, IN)
    w_out = kb.dram("w_out", [4096, 4096], F32, IN)
    rowv = kb.dram("rowv", [4, 4096], F32, IN)
    wr = kb.dram("wr", [128, 32, 16], F32, IN)
    identf = kb.dram("identf", [128, 128], F32, IN)
    h1 = kb.dram("h1", [1024, 4096], F32, OUT)
    bl = kb.dram("bl", [1024, 4096], BF16, OUT)
    affT = kb.dram("affT", [16, 1024], F32, OUT)
    ps = psum_banks(kb)
    ar = Arena(kb, "arenaF", 204 * 1024)
    epsb = ar.view("epsb", 200 * 1024, [128, 1], F32)
    kb.op("dve", lambda h: h.memset(epsb[:], 1e-6), writes=[epsb])
    ffn_front(kb, ar, ps, [], epsb, xres, catT, w_out, rowv, wr, identf, h1, bl, affT)
    return kb.finish()


def launch_F(inp, mods, h2, finT_all):
    nc = build_F()
    w_out = np.ascontiguousarray(inp["hyena_w_out"][0])
    wr = np.ascontiguousarray(np.asarray(inp["router_w"][1], np.float32).reshape(32, 128, 16).transpose(1, 0, 2))
    ident = np.eye(128, dtype=np.float32)
    maps = []
    for c in range(NCORES):
        b, q = c // 4, c % 4
        rowv = np.stack([mods[1, b, 2], inp["norm_ffn_g"][1], mods[1, b, 4], mods[1, b, 3]], 0).astype(np.float32)
        maps.append({"xres": np.ascontiguousarray(h2[b, q * 1024:(q + 1) * 1024]),
                     "catT": np.ascontiguousarray(finT_all[b][:, q * 1024:(q + 1) * 1024]),
                     "w_out": w_out, "rowv": np.ascontiguousarray(rowv), "wr": wr, "identf": ident})
    return run(nc, maps)


def gather_rows(r, key):
    return np.stack([np.concatenate([r[b * 4 + q][key] for q in range(4)], 0) for b in range(2)], 0)


def hy_consts():
    N = 8192
    P = np.arange(256)[:, None]; FP = np.arange(256)[None, :]
    w = 2 * np.pi * P * FP / 256
    W256cat = np.stack([np.concatenate([np.cos(w[h * 128:(h + 1) * 128]), -np.sin(w[h * 128:(h + 1) * 128])], 1) for h in range(2)], 1)
    m = np.arange(128); a_of = m // 4; c_of = m % 4
    tw = 2 * np.pi * a_of[:, None] * np.arange(256)[None, :] / N
    TW = np.concatenate([np.cos(tw), -np.sin(tw), np.cos(tw)], 1)
    ang32 = 2 * np.pi * a_of[:, None] * a_of[None, :] / 32
    same = (c_of[:, None] == c_of[None, :]).astype(np.float64)
    M32 = np.stack([np.cos(ang32) * same, -np.sin(ang32) * same], 1)
    Vre = np.cos(ang32) * same; Vim = np.sin(ang32) * same
    Vcat = np.stack([np.concatenate([Vre, Vim], 1), np.concatenate([-Vim, Vre], 1)], 1)
    T2 = np.zeros((128, 2, 3, 128)); U = np.zeros((128, 2, 2, 128))
    for jc in range(2):
        fp = jc * 128 + np.arange(128)
        t2 = 2 * np.pi * fp[:, None] * a_of[None, :] / N
        T2[:, jc, 0] = np.cos(t2); T2[:, jc, 1] = np.sin(t2); T2[:, jc, 2] = np.cos(t2)
        u = 2 * np.pi * fp[:, None] * np.arange(128)[None, :] / 256
        U[:, jc, 0] = np.cos(u) / N; U[:, jc, 1] = -np.sin(u) / N
    return (W256cat.astype(NPBF), TW.astype(np.float32), M32.astype(NPBF), Vcat.astype(NPBF), T2.astype(np.float32), U.astype(NPBF))


def build_E():
    kb = KB()
    aTf = kb.dram("aTf", [2, 4096, 4098], BF16, IN)
    wind = kb.dram("win", [4096, 1536], F32, IN)
    cwd = kb.dram("cw", [128, 12, 4], F32, IN)
    hbd = kb.dram("hb", [128, 4], F32, IN)
    featd = kb.dram("feat", [2, 33, 4096], F32, IN)
    mlpd = kb.dram("mlp", [64, 200], F32, IN)
    w3d = kb.dram("w3", [64, 2, 512], F32, IN)
    t01d = kb.dram("t01", [128, 2, 32], F32, IN)
    deld = kb.dram("delt", [128, 512], F32, IN)
    m0d = kb.dram("m0", [128, 1], F32, IN)
    c_w256 = kb.dram("c_w256", [128, 2, 512], BF16, IN)
    c_tw = kb.dram("c_tw", [128, 768], F32, IN)
    c_m32 = kb.dram("c_m32", [128, 2, 128], BF16, IN)
    c_vcat = kb.dram("c_vcat", [128, 2, 256], BF16, IN)
    c_t2 = kb.dram("c_t2", [128, 2, 3, 128], F32, IN)
    c_u = kb.dram("c_u", [128, 2, 2, 128], BF16, IN)
    c_id = kb.dram("c_id", [128, 128], BF16, IN)
    Khd = kb.dram("Kh", [128, 128, 768], F32, OUT)
    find = kb.dram("finT", [512, 2, 4096], BF16, OUT)
    ps = psum_banks(kb)
    ar = Arena(kb, "arenaE", 204 * 1024)
    KBY = 1024
    o = 160 * KBY
    def cv(name, shape, dt):
        nonlocal o
        n = 1
        for s_ in shape[1:]: n *= s_
        b_ = ar.view(name, o, shape, dt); o += ((n * (2 if dt == BF16 else 4) + 3) // 4) * 4
        return b_
    w256 = cv("w256", [128, 2, 512], BF16); tw = cv("tw", [128, 768], F32); m32 = cv("m32", [128, 2, 128], BF16)
    vcat = cv("vcat", [128, 2, 256], BF16); t2c = cv("t2c", [128, 2, 384], F32); uc = cv("uc", [128, 4, 128], BF16)
    ident = cv("identE", [128, 128], BF16); cwb = cv("cwb", [128, 12, 4], F32); hbb = cv("hbb", [128, 4], F32)
    rn = cv("rn", [128, 4], F32); m0 = cv("m0", [128, 1], F32); t01 = cv("t01", [128, 2, 32], F32)
    hpi = cv("hpi", [128, 1], F32); onesb = cv("onesb", [128, 1], BF16)
    ta01 = cv("ta01", [128, 512], F32); ta23 = cv("ta23", [128, 512], F32)
    Bp = cv("Bp", [128, 768], BF16); Yh = cv("Yh", [128, 512], BF16)
    Bp2 = [Bp, cv("Bp1", [128, 768], BF16)]; Yh2 = [Yh, cv("Yh1", [128, 512], BF16)]
    tb01 = cv("tb01", [128, 512], F32); tb23 = cv("tb23", [128, 512], F32); tc01 = cv("tc01", [128, 256], F32); tc23 = cv("tc23", [128, 256], F32)
    Cp = cv("Cp", [128, 2, 2, 512], BF16)
    Kb = [cv(f"Kb{i}", [128, 768], F32) for i in range(2)]
    assert o <= 204 * KBY, o
    for (dst, src) in ((w256, c_w256), (tw, c_tw), (m32, c_m32), (vcat, c_vcat), (ident, c_id), (cwb, cwd), (hbb, hbd), (m0, m0d), (t01, t01d)):
        kb.load(dst, dst[:], src, src.ap())
    kb.load(t2c, t2c[:], c_t2, c_t2.ap().rearrange("p a b c -> p a (b c)"))
    kb.load(uc, uc[:], c_u, c_u.ap().rearrange("p a b c -> p (a b) c"))
    kb.op("dve", lambda h: h.memset(hpi[:], math.pi / 2), writes=[hpi])
    kb.op("dve", lambda h: h.memset(onesb[:], 1.0), writes=[onesb])

    ktok = ar.view("ktok", 32 * KBY, [128, 2, 128, 128], BF16)
    r0 = 96 * KBY
    feat = ar.view("feat", r0, [33, 4096], F32)
    hid1 = ar.view("hid1", r0 + 16 * KBY, [64, 4096], F32)
    hid2 = [ar.view(f"hid2{i}", r0 + 32 * KBY + i * 16 * KBY, [64, 4096], F32) for i in range(2)]
    zreg = 0
    mlp = ar.view("mlp", zreg, [64, 200], F32)
    w3 = ar.view("w3", zreg + 1 * KBY, [64, 2, 512], F32)
    delt = ar.view("delt", zreg + 5 * KBY, [128, 512], F32)
    dec = ar.view("dec", zreg + 7 * KBY, [128, 512], F32)
    kf = ar.view("kf", zreg + 9 * KBY, [128, 512], F32)
    kab = ar.view("kab", zreg + 11 * KBY, [128, 512], BF16)
    s1 = ar.view("s1", zreg + 12 * KBY, [64, 512], F32); s2 = ar.view("s2", zreg + 14 * KBY, [64, 512], F32); sa = ar.view("sa", zreg + 16 * KBY, [64, 512], F32)
    sx = ar.view("sx", zreg + 18 * KBY, [64, 512], F32)
    kb.load(mlp, mlp[:], mlpd, mlpd.ap()); kb.load(w3, w3[:], w3d, w3d.ap()); kb.load(delt, delt[:], deld, deld.ap())

    def sin_layer(src_ps, bcol, fcol, dst_ap, dstb):
        kb.op("dve", lambda h: h.tensor_scalar(sx[:], src_ps[0:64, :], mlp[:, bcol:bcol + 1], mlp[:, fcol:fcol + 1], ALU.add, ALU.mult), reads=[src_ps, mlp], writes=[sx])
        kb.op("act", lambda h: h.activation(s1[:], sx[:], AF.Sin, scale=0.5), reads=[sx], writes=[s1])
        kb.op("act", lambda h: h.activation(sa[:], sx[:], AF.Abs, scale=0.5), reads=[sx], writes=[sa])
        kb.op("act", lambda h: h.activation(s2[:], sa[:], AF.Sin, bias=hpi[0:64, 0:1], scale=-1.0), reads=[sa, hpi], writes=[s2])
        kb.op("dve", lambda h: h.scalar_tensor_tensor(dst_ap, s1[:], 2.0, s2[:], ALU.mult, ALU.mult), reads=[s1, s2], writes=[dstb])

    for half in range(2):
        kb.load(feat, feat[:], featd, featd.ap()[half])
        for tt in range(8):
            p = ps[tt % 2]
            kb.mm(p, p[0:64, :], mlp, mlp[0:33, 0:64], feat, feat[:, tt * 512:(tt + 1) * 512], True, True)
            sin_layer(p, 128, 130, hid1[:, tt * 512:(tt + 1) * 512], hid1)
        for tt in range(8):
            p = ps[2 + tt % 2]
            kb.mm(p, p[0:64, :], mlp, mlp[:, 64:128], hid1, hid1[:, tt * 512:(tt + 1) * 512], True, True)
            sin_layer(p, 129, 131, hid2[half][:, tt * 512:(tt + 1) * 512], hid2[half])
    nps = ps[7]
    first = True
    for half in range(2):
        h3 = hid2[half][:].rearrange("k (p a) -> k a p", a=32)
        for a in range(32):
            p = ps[4 + a % 2]
            kb.mm(p, p[:], hid2[half], h3[:, a, :], w3, w3[:, half, :], True, True)
            kb.op("act", lambda h: h.activation(dec[:], delt[:], AF.Exp, scale=t01[:, half, a:a + 1]), reads=[delt, t01], writes=[dec])
            kb.op("dve", lambda h: h.tensor_tensor(kf[:], p[:], dec[:], ALU.mult), reads=[p, dec], writes=[kf])
            if half == 1 and a == 0:
                kb.op("dve", lambda h: h.tensor_scalar(kf[:], kf[:], m0[:, 0:1], None, ALU.mult), reads=[kf, m0], writes=[kf])
            kb.op("act", lambda h: h.activation(ktok[:, half, :, a * 4:(a + 1) * 4], kf[:].rearrange("p (g c) -> p g c", c=4), AF.Copy), reads=[kf], writes=[ktok])
            kb.op("act", lambda h: h.activation(kab[:], kf[:], AF.Abs), reads=[kf], writes=[kab])
            for cc in range(4):
                last = (half == 1 and a == 31)
                kb.op("pe", lambda h: h.matmul(nps[:, cc:cc + 1], kab[:, cc * 128:(cc + 1) * 128], onesb[:], start=first, stop=last, skip_group_check=True),
                      reads=[kab, onesb], writes=[nps], sig=(cc == 3))
                first = False
    kb.op("dve", lambda h: h.reciprocal(rn[:], nps[:, 0:4]), reads=[nps], writes=[rn])

    cnt = {"g": 0}

    def fft_fwd(srcb, src_aps):
        i = cnt["g"]; cnt["g"] += 1
        p1 = ps[i % 2]; p2 = ps[2 + i % 2]
        for hh, ap_ in enumerate(src_aps):
            kb.mm(p1, p1[:], srcb, ap_, w256, w256[:, hh, :], hh == 0, hh == len(src_aps) - 1)
        kb.op("dve", lambda h: h.tensor_tensor(ta01[:], p1[:], tw[:, 0:512], ALU.mult), reads=[p1, tw], writes=[ta01])
        kb.op("dve", lambda h: h.tensor_tensor(ta23[:], p1[:], tw[:, 256:768], ALU.mult), reads=[p1, tw], writes=[ta23])
        kb.op("dve", lambda h: h.tensor_tensor(Bp[:, 256:512], ta01[:, 0:256], ta01[:, 256:512], ALU.subtract), reads=[ta01], writes=[Bp])
        kb.op("dve", lambda h: h.tensor_tensor(ta23[:, 0:256], ta23[:, 0:256], ta23[:, 256:512], ALU.add), reads=[ta23], writes=[ta23])
        kb.op("act", lambda h: h.activation(Bp[:, 512:768], ta23[:, 0:256], AF.Copy), reads=[ta23], writes=[Bp])
        kb.op("act", lambda h: h.activation(Bp[:, 0:256], ta23[:, 0:256], AF.Copy, scale=-1.0), reads=[ta23], writes=[Bp])
        kb.mm(p2, p2[:], m32, m32[:, 0, :], Bp, Bp[:, 256:768], True, False)
        kb.mm(p2, p2[:], m32, m32[:, 1, :], Bp, Bp[:, 0:512], False, True)
        return p2

    for g in range(128):
        p2 = fft_fwd(ktok, [ktok[:, hh, g, :] for hh in range(2)])
        kbuf = Kb[g % 2]
        kb.op("act", lambda h: h.activation(kbuf[:, 0:512], p2[:], AF.Copy), reads=[p2], writes=[kbuf])
        kb.op("act", lambda h: h.activation(kbuf[:, 512:768], p2[:, 0:256], AF.Copy), reads=[p2], writes=[kbuf])
        kb.store(Khd, Khd.ap()[g], kbuf, kbuf[:])

    filt_dead = [ktok, feat, hid1, hid2[0], hid2[1], mlp, w3, delt, dec, kf, kab, s1, s2, sa, sx]
    zT = ar.view("zT", 0, [128, 4, 4096], BF16, prev=filt_dead)
    atile = [ar.view(f"at{i}", 32 * KBY + i * 32 * KBY, [128, 32, 512], BF16, prev=filt_dead) for i in range(2)]
    ztok = ar.view("ztok", 32 * KBY, [128, 128, 128], BF16, prev=filt_dead)
    ytok = ar.view("ytok", 64 * KBY, [128, 32, 512], BF16, prev=filt_dead)
    ztok.w = atile[0].w; ztok.r = atile[0].r; ytok.w = atile[1].w; ytok.r = atile[1].r
    yT = ar.view("yT", 96 * KBY, [128, 4, 4096], BF16, prev=filt_dead)
    wp = ar.view("wp", 128 * KBY, [128, 32, 512], BF16, prev=filt_dead)
    ct_ = [ar.view(f"ct{i}", 96 * KBY + i * 2 * KBY, [128, 512], F32, prev=filt_dead) for i in range(2)]
    cx = [ar.view(f"cx{i}", 156 * KBY + i * 2 * KBY, [128, 512], F32) for i in range(2)] if False else None
    winv = wind.ap().rearrange("(k p) n -> p k n", p=128)
    tiles = [(510 * i, 510) for i in range(8)] + [(4080, 16)]
    for b in range(2):
        xdead = [ztok, ytok] if b > 0 else []
        for part in (1, 2, 0):
            kb.dma("pool", lambda h: h.dma_start(out=wp[:], in_=winv[:, :, part * 512:(part + 1) * 512]), reads=[wind], writes=[wp], sembuf=wp)
            if part == 0:
                for cbk in range(4):
                    for a0 in range(0, 32, 8):
                        p = ps[4 + (a0 // 8) % 2]
                        z3 = zT[:, cbk, :].rearrange("c (p a) -> c a p", a=32)
                        for q in range(8):
                            kb.op("pe", lambda h: h.transpose(p.bf[:, q * 128:(q + 1) * 128], z3[:, a0 + q, :], ident[:]), reads=[zT, ident], writes=[p], sig=(q == 7))
                        kb.op("act", lambda h: h.activation(ztok[:, cbk * 32:(cbk + 1) * 32, a0 * 4:(a0 + 8) * 4].rearrange("p g (a c) -> p g a c", c=4), p.bf[:].rearrange("p (a g c) -> p g a c", a=8, c=4), AF.Copy),
                              reads=[p], writes=[ztok])
                kb.load(Kb[0], Kb[0][:], Khd, Khd.ap()[0])
                p2s = {}
                for i in range(130):
                    gT = i - 2; gS = i - 1
                    if 0 <= gT < 128:
                        yh = Yh2[gT % 2]
                        for jc in range(2):
                            p3 = ps[4 + jc]
                            kb.mm(p3, p3[:, 0:256], yh, yh[:, jc * 128:(jc + 1) * 128], vcat, vcat[:, 0, :], True, False)
                            kb.mm(p3, p3[:, 0:256], yh, yh[:, 256 + jc * 128:256 + (jc + 1) * 128], vcat, vcat[:, 1, :], False, True)
                    if i < 128:
                        p1 = ps[i % 2]
                        kb.mm(p1, p1[:], ztok, ztok[:, i, :], w256, w256[:, 0, :], True, True)
                    if 0 <= gS < 128:
                        if gS < 127:
                            kb.load(Kb[(gS + 1) % 2], Kb[(gS + 1) % 2][:], Khd, Khd.ap()[gS + 1])
                        kbuf = Kb[gS % 2]; p2 = p2s.pop(gS); yh = Yh2[gS % 2]
                        kb.op("dve", lambda h: h.tensor_tensor(ta01[:], p2[:], kbuf[:, 0:512], ALU.mult), reads=[p2, kbuf], writes=[ta01])
                        kb.op("dve", lambda h: h.tensor_tensor(ta23[:], p2[:], kbuf[:, 256:768], ALU.mult), reads=[p2, kbuf], writes=[ta23])
                        kb.op("dve", lambda h: h.tensor_tensor(yh[:, 0:256], ta01[:, 0:256], ta01[:, 256:512], ALU.subtract), reads=[ta01], writes=[yh])
                        kb.op("dve", lambda h: h.tensor_tensor(yh[:, 256:512], ta23[:, 0:256], ta23[:, 256:512], ALU.add), reads=[ta23], writes=[yh])
                    if i < 128:
                        bp = Bp2[i % 2]; p1 = ps[i % 2]; p2 = ps[2 + i % 2]
                        kb.op("dve", lambda h: h.tensor_tensor(tb01[:], p1[:], tw[:, 0:512], ALU.mult), reads=[p1, tw], writes=[tb01])
                        kb.op("dve", lambda h: h.tensor_tensor(tb23[:], p1[:], tw[:, 256:768], ALU.mult), reads=[p1, tw], writes=[tb23])
                        kb.op("dve", lambda h: h.tensor_tensor(bp[:, 256:512], tb01[:, 0:256], tb01[:, 256:512], ALU.subtract), reads=[tb01], writes=[bp])
                        kb.op("dve", lambda h: h.tensor_tensor(tb23[:, 0:256], tb23[:, 0:256], tb23[:, 256:512], ALU.add), reads=[tb23], writes=[tb23])
                        kb.op("act", lambda h: h.activation(bp[:, 512:768], tb23[:, 0:256], AF.Copy), reads=[tb23], writes=[bp])
                        kb.op("act", lambda h: h.activation(bp[:, 0:256], tb23[:, 0:256], AF.Copy, scale=-1.0), reads=[tb23], writes=[bp])
                        kb.mm(p2, p2[:], m32, m32[:, 0, :], bp, bp[:, 256:768], True, False)
                        kb.mm(p2, p2[:], m32, m32[:, 1, :], bp, bp[:, 0:512], False, True)
                        p2s[i] = p2
                    if 0 <= gT < 128:
                        g4 = gT % 4
                        for jc in range(2):
                            p3 = ps[4 + jc]
                            kb.op("dve", lambda h: h.tensor_tensor(tc01[:, 0:256], p3[:, 0:256], t2c[:, jc, 0:256], ALU.mult), reads=[p3, t2c], writes=[tc01])
                            kb.op("dve", lambda h: h.tensor_tensor(tc23[:, 0:256], p3[:, 0:256], t2c[:, jc, 128:384], ALU.mult), reads=[p3, t2c], writes=[tc23])
                            kb.op("dve", lambda h: h.tensor_tensor(Cp[:, jc, 0, g4 * 128:(g4 + 1) * 128], tc01[:, 0:128], tc01[:, 128:256], ALU.subtract), reads=[tc01], writes=[Cp])
                            kb.op("dve", lambda h: h.tensor_tensor(Cp[:, jc, 1, g4 * 128:(g4 + 1) * 128], tc23[:, 0:128], tc23[:, 128:256], ALU.add), reads=[tc23], writes=[Cp])
                        if g4 == 3:
                            G4 = gT // 4
                            p4 = ps[6 + G4 % 2]
                            kb.mm(p4, p4[:], uc, uc[:, 0, :], Cp, Cp[:, 0, 0, :], True, False)
                            kb.mm(p4, p4[:], uc, uc[:, 1, :], Cp, Cp[:, 0, 1, :], False, False)
                            kb.mm(p4, p4[:], uc, uc[:, 2, :], Cp, Cp[:, 1, 0, :], False, False)
                            kb.mm(p4, p4[:], uc, uc[:, 3, :], Cp, Cp[:, 1, 1, :], False, True)
                            kb.op("act", lambda h: h.activation(ytok[:, :, 16 * G4:16 * G4 + 16].rearrange("p a (g c) -> p a g c", g=4),
                                                                 p4[:].rearrange("p (g a c) -> p a g c", g=4, a=32), AF.Copy), reads=[p4], writes=[ytok])
                for cbk in range(4):
                    y3 = yT[:, cbk, :].rearrange("c (p a) -> c a p", a=32)
                    for a0 in range(0, 32, 8):
                        p = ps[(a0 // 8) % 2]
                        for q in range(8):
                            kb.op("pe", lambda h: h.transpose(p.bf[:, q * 128:(q + 1) * 128], ytok[:, a0 + q, cbk * 128:(cbk + 1) * 128], ident[:]), reads=[ytok, ident], writes=[p], sig=(q == 7))
                        kb.op("act", lambda h: h.activation(y3[:, a0:a0 + 8, :], p.bf[:].rearrange("c (a p) -> c a p", a=8), AF.Copy), reads=[p], writes=[yT])
            for ti, (t0, n) in enumerate(tiles):
                at = atile[ti % 2]
                kb.dma("sp", lambda h: h.dma_start(out=at[:, :, 0:n + 2], in_=aTf.ap()[b].rearrange("(k p) t -> p k t", p=128)[:, :, t0:t0 + n + 2]),
                       reads=[aTf], writes=[at], sembuf=at)
                for cbk in range(4):
                    p = ps[4 + cbk % 2] if part != 0 else ps[2 + cbk % 2]
                    for k in range(32):
                        kb.mm(p, p[:, 0:n + 2], wp, wp[:, k, cbk * 128:(cbk + 1) * 128], at, at[:, k, 0:n + 2], k == 0, k == 31)
                    blk = part * 4 + cbk
                    c1 = Kb[0]; c2 = Kb[1]
                    kb.op("act", lambda h: h.activation(c1[:, 0:n], p[:, 1:n + 1], AF.Identity, bias=cwb[:, blk, 3:4], scale=cwb[:, blk, 1:2]), reads=[p, cwb], writes=[c1])
                    kb.op("dve", lambda h: h.scalar_tensor_tensor(c1[:, 0:n], p[:, 0:n], cwb[:, blk, 0:1], c1[:, 0:n], ALU.mult, ALU.add), reads=[p, cwb, c1], writes=[c1])
                    zsl = zT[:, cbk, t0:t0 + n]
                    if part == 1:
                        kb.op("dve", lambda h: h.scalar_tensor_tensor(zsl, p[:, 2:n + 2], cwb[:, blk, 2:3], c1[:, 0:n], ALU.mult, ALU.add), reads=[p, cwb, c1], writes=[zT])
                    elif part == 2:
                        kb.op("dve", lambda h: h.scalar_tensor_tensor(c1[:, 0:n], p[:, 2:n + 2], cwb[:, blk, 2:3], c1[:, 0:n], ALU.mult, ALU.add), reads=[p, cwb, c1], writes=[c1])
                        kb.op("dve", lambda h: h.tensor_tensor(zsl, zsl, c1[:, 0:n], ALU.mult), reads=[zT, c1], writes=[zT])
                    else:
                        kb.op("dve", lambda h: h.scalar_tensor_tensor(c1[:, 0:n], p[:, 2:n + 2], cwb[:, blk, 2:3], c1[:, 0:n], ALU.mult, ALU.add), reads=[p, cwb, c1], writes=[c1])
                        kb.op("dve", lambda h: h.tensor_scalar(c2[:, 0:n], zsl, hbb[:, cbk:cbk + 1], None, ALU.mult), reads=[zT, hbb], writes=[c2])
                        kb.op("dve", lambda h: h.scalar_tensor_tensor(c2[:, 0:n], yT[:, cbk, t0:t0 + n], rn[:, cbk:cbk + 1], c2[:, 0:n], ALU.mult, ALU.add), reads=[yT, rn, c2], writes=[c2])
                        fo = Yh if False else None
                        kb.op("dve", lambda h: h.tensor_tensor(Bp[:, 0:n], c2[:, 0:n], c1[:, 0:n], ALU.mult), reads=[c1, c2], writes=[Bp])
                        kb.store(find, find.ap()[cbk * 128:(cbk + 1) * 128, b, t0:t0 + n], Bp, Bp[:, 0:n])
    return kb.finish()


def launch_E(inp, aT1_all):
    nc = build_E()
    W256cat, TW, M32, Vcat, T2, U = hy_consts()
    aTf = np.zeros((2, 4096, 4098), NPBF)
    aTf[:, :, 1:4097] = aT1_all
    Lm = 4096
    pos = np.arange(Lm, dtype=np.float32)
    def feats(posv):
        t01 = posv / (Lm - 1)
        bands = np.linspace(1e-4, 15, 16, dtype=np.float32)
        ang = (2.0 * math.pi / Lm) * posv[:, None] * bands[None, :]
        return np.concatenate([t01[:, None], np.cos(ang), -np.sin(ang)], -1).astype(np.float32)
    jrev = (4096 - np.arange(Lm)).astype(np.float32); jrev[0] = 0.0
    feat = np.ascontiguousarray(np.stack([feats(pos).T, feats(jrev).T], 0))
    t01 = np.zeros((128, 2, 32), np.float32)
    ii = (32 * np.arange(128)[:, None] + np.arange(32)[None, :]).astype(np.float32)
    t01[:, 0, :] = -ii / (Lm - 1)
    t01[:, 1, :] = -(4096 - ii) / (Lm - 1)
    deltas = np.abs(np.linspace(math.log(1e-2) / 1.5, math.log(1e-2) / 0.3, 4096, dtype=np.float32))
    m0 = np.ones((128, 1), np.float32); m0[0, 0] = 0.0
    mlp = np.zeros((64, 200), np.float32)
    mlp[:33, 0:64] = inp["hyena_filt_w1"][0]; mlp[:, 64:128] = inp["hyena_filt_w2"][0]
    mlp[:, 128] = inp["hyena_filt_b1"][0]; mlp[:, 129] = inp["hyena_filt_b2"][0]
    mlp[:, 130] = inp["hyena_filt_freq"][0][0]; mlp[:, 131] = inp["hyena_filt_freq"][0][1]
    ident = np.eye(128, dtype=np.float32).astype(NPBF)
    w_in = inp["hyena_w_in"][0]; cwt = np.asarray(inp["hyena_conv_w"][0], np.float32); cbt = np.asarray(inp["hyena_conv_b"][0], np.float32)
    w3full = np.asarray(inp["hyena_filt_w3"][0], np.float32); hbias = np.asarray(inp["hyena_bias"][0], np.float32)
    maps = []
    for c in range(NCORES):
        C0 = 512 * c
        cols = np.concatenate([np.arange(part * 4096 + C0, part * 4096 + C0 + 512) for part in range(3)])
        cw = np.zeros((128, 12, 4), np.float32)
        for blk in range(12):
            cc = cols[blk * 128:(blk + 1) * 128]
            cw[:, blk, 0:3] = cwt[:, cc].T
            cw[:, blk, 3] = cbt[cc]
        maps.append({"aTf": aTf, "win": np.ascontiguousarray(w_in[:, cols]), "cw": cw,
                     "hb": np.ascontiguousarray(hbias[C0:C0 + 512].reshape(4, 128).T), "feat": feat, "mlp": mlp,
                     "w3": np.ascontiguousarray(np.stack([w3full[:, C0:C0 + 512], w3full[:, 4096 + C0:4096 + C0 + 512]], 1)),
                     "t01": t01, "delt": np.ascontiguousarray(np.tile(deltas[None, C0:C0 + 512], (128, 1))), "m0": m0,
                     "c_w256": W256cat, "c_tw": TW, "c_m32": M32, "c_vcat": Vcat, "c_t2": T2, "c_u": U, "c_id": ident})
    r = run(nc, maps)
    fin = np.concatenate([r[c]["finT"] for c in range(NCORES)], 0)
    return np.ascontiguousarray(fin.transpose(1, 0, 2))


def kernel(**inp):
    inp = {k: np.asarray(v) for k, v in inp.items()}
    mods = launch_A(inp)
    rB = launch_B(inp, mods)
    h1 = gather_rows(rB, "h1"); bl = gather_rows(rB, "bl")
    affT = np.concatenate([np.concatenate([rB[b * 4 + q]["affT"] for q in range(4)], 1) for b in range(2)], 0)
    ys_all, slotm = launch_C(inp, 0, np.ascontiguousarray(affT), np.ascontiguousarray(bl.reshape(8192, D)))
    rD = launch_D(inp, False, h1, ys_all, slotm, mods, 0)
    h2 = gather_rows(rD, "h2")
    aT1 = np.stack([np.concatenate([rD[b * 4 + q]["aT1"] for q in range(4)], 1) for b in range(2)], 0)
    finT = launch_E(inp, aT1)
    rF = launch_F(inp, mods, h2, finT)
    h3 = gather_rows(rF, "h1"); bl2 = gather_rows(rF, "bl")
    affT2 = np.concatenate([np.concatenate([rF[b * 4 + q]["affT"] for q in range(4)], 1) for b in range(2)], 0)
    ys2, slotm2 = launch_C(inp, 1, np.ascontiguousarray(affT2), np.ascontiguousarray(bl2.reshape(8192, D)))
    rH = launch_D(inp, True, h3, ys2, slotm2, mods, 1)
    return gather_rows(rH, "out").astype(np.float32)
```
